# Optimizing a Trainium2 kernel written in Bass

```python
import math
import jax
import jax.numpy as jnp
from jax import lax
import numpy as np

D_MODEL = 1024
BATCH = 16
SEQ = 2048
DEPTH = 4

CTX_LEN = 256
GRID_W = 64
EPS = 1e-6

RW_HEAD = 64
RW_WIDTH = D_MODEL // 2
RW_HEADS = RW_WIDTH // RW_HEAD
RW_W_LORA = 64
RW_A_LORA = 64
RW_G_LORA = 128
RW_GN_EPS = 64e-5
RW_SIZES = (RW_WIDTH, RW_WIDTH, RW_WIDTH, RW_W_LORA, RW_A_LORA, RW_G_LORA)
RW_IN = 3 * RW_WIDTH + RW_W_LORA + RW_A_LORA + RW_G_LORA

ML_WIDTH = D_MODEL // 2
ML_HEADS = 4
ML_DV = ML_WIDTH // ML_HEADS
ML_DK = ML_DV // 2
ML_QK = ML_HEADS * ML_DK
ML_CHUNK = 64
ML_SIZES = (2 * ML_QK, ML_WIDTH, ML_WIDTH, 4 * ML_HEADS)
ML_IN = 2 * ML_QK + 2 * ML_WIDTH + 4 * ML_HEADS

MB_WIDTH = D_MODEL
MB_HEADDIM = 64
MB_HEADS = MB_WIDTH // MB_HEADDIM
MB_GROUPS = 4
MB_HPG = MB_HEADS // MB_GROUPS
MB_STATE = 128
MB_BC = MB_GROUPS * MB_STATE
MB_XBC = MB_WIDTH + 2 * MB_BC
MB_CHUNK = 64
MB_SIZES = (MB_WIDTH, MB_XBC, 2 * MB_HEADS)
MB_IN = MB_WIDTH + MB_XBC + 2 * MB_HEADS

CONV_W = 5
N_BRANCH = 3
IN_SIZES = (RW_IN, ML_IN, MB_IN, N_BRANCH * D_MODEL)
IN_W = RW_IN + ML_IN + MB_IN + N_BRANCH * D_MODEL

N_EXPERTS = 16
EXPERT_FF = 1024
CAPACITY_FACTOR = 2

kernel_name = 'hybrid_rwkv7_mlstm_ssd_ecmoe_dit'


def _split(u, sizes):
    idx = [int(i) for i in np.cumsum(sizes)[:-1]]
    return jnp.split(u, idx, axis=-1)


def _chunks(t, L):
    B_, T = t.shape[:2]
    return jnp.moveaxis(t.reshape(B_, T // L, L, *t.shape[2:]), 1, 0)


def _unchunk(t):
    t = jnp.moveaxis(t, 0, 1)
    return t.reshape(t.shape[0], t.shape[1] * t.shape[2], *t.shape[3:])


def rmsnorm(x, g):
    xf = x.astype(jnp.float32)
    y = xf * lax.rsqrt(jnp.mean(xf * xf, axis=-1, keepdims=True) + EPS)
    return (y * g).astype(x.dtype)


def to_col_major(x):
    B_, T, D_ = x.shape
    rows = T // GRID_W
    return x.reshape(B_, rows, GRID_W, D_).transpose(0, 2, 1, 3).reshape(B_, T, D_)


def from_col_major(x):
    B_, T, D_ = x.shape
    rows = T // GRID_W
    return x.reshape(B_, GRID_W, rows, D_).transpose(0, 2, 1, 3).reshape(B_, T, D_)


def token_shift(u):
    up = jnp.pad(u, ((0, 0), (1, 1), (0, 0)))
    return 0.5 * (up[:, :-2] + up[:, 2:])


def dwconv(x, w, b):
    pad = w.shape[0] // 2
    y = lax.conv_general_dilated(x, w.astype(x.dtype)[:, None, :], window_strides=(1,),
                                 padding=[(pad, pad)], dimension_numbers=('NWC', 'WIO', 'NWC'),
                                 feature_group_count=x.shape[-1])
    return y + b


def two_way(run, ctx_dirs, lat_dirs, state0):
    y_ctx, y_lat = 0.0, 0.0
    for d in range(2):
        rev = (lambda t: jnp.flip(t, axis=1)) if d == 1 else (lambda t: t)
        yc, st = run(tuple(rev(t) for t in ctx_dirs[d]), state0)
        yl, _ = run(tuple(rev(t) for t in lat_dirs[d]), st)
        y_ctx = y_ctx + rev(yc)
        y_lat = y_lat + rev(yl)
    return y_ctx, y_lat


def rwkv_run(inputs, S0):
    def step(S, inp):
        r, w, k, v, kk, a = inp
        sa = jnp.einsum('bhij,bhj->bhi', S, -kk)
        S = S * w[:, :, None, :] + sa[..., None] * (kk * a)[:, :, None, :] + v[..., None] * k[:, :, None, :]
        return S, jnp.einsum('bhij,bhj->bhi', S, r)
    S, ys = lax.scan(step, S0, tuple(jnp.moveaxis(t, 1, 0) for t in inputs))
    return jnp.moveaxis(ys, 0, 1), S


def rwkv_prep(u, mu, w0, w2, a0, a2, g2, k_k, k_a):
    u = u.astype(jnp.float32)
    u = u + mu * (token_shift(u) - u)
    r, k, v, xw, xa, xg = _split(u, RW_SIZES)
    B_, T = u.shape[:2]
    heads = lambda t: t.reshape(B_, T, RW_HEADS, RW_HEAD)
    g = jax.nn.sigmoid(xg) @ g2
    kk = heads(k * k_k)
    kk = kk * lax.rsqrt(jnp.sum(kk * kk, axis=-1, keepdims=True) + 1e-12)
    tw = jnp.tanh(xw)
    dirs = []
    for d in range(2):
        w_log = -jax.nn.softplus(-(w0[d] + tw @ w2[d])) - 0.5
        a = jax.nn.sigmoid(a0[d] + xa @ a2[d])
        kd = k * (1.0 + (a - 1.0) * k_a)
        dirs.append((heads(r), heads(jnp.exp(-jnp.exp(w_log))), heads(kd), heads(v), kk, heads(a)))
    return dirs, g


def rwkv_post(y, dirs, g, r_k, ln_w, ln_b):
    B_, T = y.shape[:2]
    mean = jnp.mean(y, axis=-1, keepdims=True)
    var = jnp.mean(jnp.square(y - mean), axis=-1, keepdims=True)
    y = (y - mean) * lax.rsqrt(var + RW_GN_EPS)
    r, v = dirs[0][0], dirs[0][3]
    rk = r_k.reshape(RW_HEADS, RW_HEAD)
    bonus = (jnp.sum(r * dirs[0][2] * rk, axis=-1, keepdims=True)
             + jnp.sum(r * dirs[1][2] * rk, axis=-1, keepdims=True)) * v
    out = y.reshape(B_, T, RW_WIDTH) * ln_w + ln_b + bonus.reshape(B_, T, RW_WIDTH)
    return out * g


def rwkv_mixer(uc, ul, p, need_ctx):
    mu, w0, w2, a0, a2, g2, k_k, k_a, r_k, ln_w, ln_b = p
    dc, gc = rwkv_prep(uc, mu, w0, w2, a0, a2, g2, k_k, k_a)
    dl, gl = rwkv_prep(ul, mu, w0, w2, a0, a2, g2, k_k, k_a)
    s0 = jnp.zeros((ul.shape[0], RW_HEADS, RW_HEAD, RW_HEAD), jnp.float32)
    yc, yl = two_way(rwkv_run, dc, dl, s0)
    out_l = rwkv_post(yl, dl, gl, r_k, ln_w, ln_b)
    out_c = rwkv_post(yc, dc, gc, r_k, ln_w, ln_b) if need_ctx else None
    return out_c, out_l


def mlstm_run(inputs, state0):
    L = ML_CHUNK
    mask = jnp.tril(jnp.ones((L, L), dtype=bool))

    def step(carry, inp):
        C, n, m = carry
        qc, kc, vc, ic, fc = inp
        b = jnp.cumsum(fc, axis=1)
        Dm = b[:, :, None, :] - b[:, None, :, :] + ic[:, None, :, :]
        Dm = jnp.where(mask[None, :, :, None], Dm, -jnp.inf)
        inter = b + m[:, None, :]
        m_t = jnp.maximum(inter, jnp.max(Dm, axis=2))
        s = jnp.einsum('bthd,bshd->btsh', qc, kc) * jnp.exp(Dm - m_t[:, :, None, :])
        g_inter = jnp.exp(inter - m_t)
        num = jnp.einsum('btsh,bshv->bthv', s, vc) + g_inter[..., None] * jnp.einsum('bthd,bhdv->bthv', qc, C)
        den = jnp.sum(s, axis=2) + g_inter * jnp.einsum('bthd,bhd->bth', qc, n)
        h = num / jnp.maximum(jnp.abs(den), jnp.exp(-m_t))[..., None]
        bL = b[:, -1]
        wl = bL[:, None, :] - b + ic
        m_new = jnp.maximum(bL + m, jnp.max(wl, axis=1))
        ws = jnp.exp(wl - m_new[:, None, :])
        decay = jnp.exp(bL + m - m_new)
        C = decay[..., None, None] * C + jnp.einsum('bsh,bshd,bshv->bhdv', ws, kc, vc)
        n = decay[..., None] * n + jnp.einsum('bsh,bshd->bhd', ws, kc)
        return (C, n, m_new), h

    carry, hs = lax.scan(step, state0, tuple(_chunks(t, L) for t in inputs))
    return _unchunk(hs), carry


def mlstm_prep(u, conv_w, conv_b, gate_b):
    u = u.astype(jnp.float32)
    qk0, v, o, gates = _split(u, ML_SIZES)
    qk = jax.nn.silu(dwconv(qk0, conv_w, conv_b))
    q, k = jnp.split(qk, 2, axis=-1)
    B_, T = u.shape[:2]
    q = q.reshape(B_, T, ML_HEADS, ML_DK)
    k = k.reshape(B_, T, ML_HEADS, ML_DK) * (ML_DK ** -0.5)
    v = v.reshape(B_, T, ML_HEADS, ML_DV)
    gates = (gates + gate_b).reshape(B_, T, 2, 2, ML_HEADS)
    dirs = [(q, k, v, gates[:, :, d, 0], jax.nn.log_sigmoid(gates[:, :, d, 1])) for d in range(2)]
    return dirs, o


def mlstm_post(h, o, norm_g):
    B_, T = h.shape[:2]
    h = h * lax.rsqrt(jnp.mean(h * h, axis=-1, keepdims=True) + EPS)
    return h.reshape(B_, T, ML_WIDTH) * norm_g * jax.nn.sigmoid(o)


def mlstm_mixer(uc, ul, p, need_ctx):
    conv_w, conv_b, gate_b, norm_g = p
    dc, oc = mlstm_prep(uc, conv_w, conv_b, gate_b)
    dl, ol = mlstm_prep(ul, conv_w, conv_b, gate_b)
    B_ = ul.shape[0]
    st0 = (jnp.zeros((B_, ML_HEADS, ML_DK, ML_DV), jnp.float32),
           jnp.zeros((B_, ML_HEADS, ML_DK), jnp.float32),
           jnp.zeros((B_, ML_HEADS), jnp.float32))
    hc, hl = two_way(mlstm_run, dc, dl, st0)
    out_l = mlstm_post(hl, ol, norm_g)
    out_c = mlstm_post(hc, oc, norm_g) if need_ctx else None
    return out_c, out_l


def ssd_run(inputs, h0):
    L = MB_CHUNK
    mask = jnp.tril(jnp.ones((L, L), dtype=bool))

    def step(h, inp):
        xc, bc, cc, dtc, ac = inp
        acum = jnp.cumsum(ac, axis=1)
        seg = acum[:, :, None] - acum[:, None, :]
        decay = jnp.exp(jnp.where(mask[None, :, :, None, None], seg, -jnp.inf))
        cb = jnp.einsum('btgn,bsgn->btsg', cc, bc)
        M = cb[..., None] * decay * dtc[:, None]
        y = jnp.einsum('btsgj,bsgjp->btgjp', M, xc)
        y = y + jnp.exp(acum)[..., None] * jnp.einsum('btgn,bgjpn->btgjp', cc, h)
        aL = acum[:, -1]
        wgt = jnp.exp(aL[:, None] - acum) * dtc
        h = jnp.exp(aL)[..., None, None] * h + jnp.einsum('bsgj,bsgjp,bsgn->bgjpn', wgt, xc, bc)
        return h, y

    h, ys = lax.scan(step, h0, tuple(_chunks(t, L) for t in inputs))
    return _unchunk(ys), h


def mamba_prep(u, conv_w, conv_b, dt_bias, a_log):
    u = u.astype(jnp.float32)
    z, xbc, dt_raw = _split(u, MB_SIZES)
    xbc = jax.nn.silu(dwconv(xbc, conv_w, conv_b))
    xs, Bm, Cm = _split(xbc, (MB_WIDTH, MB_BC, MB_BC))
    B_, T = u.shape[:2]
    xs = xs.reshape(B_, T, MB_GROUPS, MB_HPG, MB_HEADDIM)
    Bm = Bm.reshape(B_, T, MB_GROUPS, MB_STATE)
    Cm = Cm.reshape(B_, T, MB_GROUPS, MB_STATE)
    dt_raw = dt_raw.reshape(B_, T, 2, MB_GROUPS, MB_HPG)
    dirs = []
    for d in range(2):
        dt = jax.nn.softplus(dt_raw[:, :, d] + dt_bias[d].reshape(MB_GROUPS, MB_HPG))
        A = -jnp.exp(a_log[d].astype(jnp.float32)).reshape(MB_GROUPS, MB_HPG)
        dirs.append((xs, Bm, Cm, dt, dt * A))
    return dirs, z, xs


def mamba_post(y, z, xs, D, norm_g):
    B_, T = y.shape[:2]
    gw = MB_WIDTH // MB_GROUPS
    y = y + D.reshape(MB_GROUPS, MB_HPG)[..., None] * xs
    y = y.reshape(B_, T, MB_GROUPS, gw) * jax.nn.silu(z).reshape(B_, T, MB_GROUPS, gw)
    y = y * lax.rsqrt(jnp.mean(y * y, axis=-1, keepdims=True) + EPS)
    return y.reshape(B_, T, MB_WIDTH) * norm_g


def mamba_mixer(uc, ul, p, need_ctx):
    conv_w, conv_b, dt_bias, a_log, D, norm_g = p
    dc, zc, xc = mamba_prep(uc, conv_w, conv_b, dt_bias, a_log)
    dl, zl, xl = mamba_prep(ul, conv_w, conv_b, dt_bias, a_log)
    h0 = jnp.zeros((ul.shape[0], MB_GROUPS, MB_HPG, MB_HEADDIM, MB_STATE), jnp.float32)
    yc, yl = two_way(ssd_run, dc, dl, h0)
    out_l = mamba_post(yl, zl, xl, D, norm_g)
    out_c = mamba_post(yc, zc, xc, D, norm_g) if need_ctx else None
    return out_c, out_l


def token_mixer(hc, hl, w_in, rw_p, ml_p, mb_p, p_a, p_b, p_c, w_out, need_ctx):
    rc, mc, bc, gc = _split(hc @ w_in, IN_SIZES)
    rl, mll, bl, gl = _split(hl @ w_in, IN_SIZES)
    ya = rwkv_mixer(rc, rl, rw_p, need_ctx)
    yb = mlstm_mixer(mc, mll, ml_p, need_ctx)
    yc = mamba_mixer(bc, bl, mb_p, need_ctx)
    dt = hl.dtype

    def merge(y_a, y_b, y_c, gates):
        ga, gb, gcc = jnp.split(jax.nn.sigmoid(gates), N_BRANCH, axis=-1)
        m = ga * (y_a.astype(dt) @ p_a) + gb * (y_b.astype(dt) @ p_b) + gcc * (y_c.astype(dt) @ p_c)
        return m @ w_out

    out_l = merge(ya[1], yb[1], yc[1], gl)
    out_c = merge(ya[0], yb[0], yc[0], gc) if need_ctx else None
    return out_c, out_l


def expert_choice_ffn(h, router_w, e_wg, e_wu, e_wd):
    B_, n, D_ = h.shape
    cap = CAPACITY_FACTOR * n // N_EXPERTS
    aff = jax.nn.softmax((h @ router_w).astype(jnp.float32), axis=-1)
    gate, idx = lax.top_k(jnp.swapaxes(aff, 1, 2), cap)
    xe = jax.vmap(lambda hb, ib: hb[ib])(h, idx)
    a = jnp.einsum('becd,edf->becf', xe, e_wg)
    u = jnp.einsum('becd,edf->becf', xe, e_wu)
    y = jnp.einsum('becf,efd->becd', jax.nn.silu(a) * u, e_wd) * gate[..., None].astype(h.dtype)
    return jax.vmap(lambda ib, yb: jnp.zeros((n, D_), yb.dtype).at[ib.reshape(-1)].add(yb.reshape(-1, D_)))(idx, y)


def setup_inputs(seed: int = 0) -> dict:
    key = jax.random.key(seed)
    ks = iter(jax.random.split(key, 64))
    L, D = DEPTH, D_MODEL

    def nrm(shape, scale):
        return scale * jax.random.normal(next(ks), shape, jnp.float32)

    def uni(shape, lo, hi):
        return jax.random.uniform(next(ks), shape, jnp.float32, minval=lo, maxval=hi)

    x = nrm((BATCH, SEQ, D), 1.0)
    c = nrm((BATCH, D), 1.0)
    ctx = nrm((BATCH, CTX_LEN, D), 1.0)
    c_ctx = nrm((D,), 1.0)
    ada_w = nrm((L, D, 6 * D), 0.5 * D ** -0.5)
    ada_b = nrm((L, 6 * D), 0.02)
    norm1_g = 1.0 + nrm((L, D), 0.02)
    norm2_g = 1.0 + nrm((L, D), 0.02)
    w_in = nrm((L, D, IN_W), D ** -0.5)
    rw_mu = uni((L, RW_IN), 0.0, 1.0)
    rw_w0 = uni((L, 2, RW_WIDTH), -6.0, 1.0)
    rw_w2 = nrm((L, 2, RW_W_LORA, RW_WIDTH), 0.1)
    rw_a0 = nrm((L, 2, RW_WIDTH), 0.1)
    rw_a2 = nrm((L, 2, RW_A_LORA, RW_WIDTH), 0.1)
    rw_g2 = nrm((L, RW_G_LORA, RW_WIDTH), RW_G_LORA ** -0.5)
    rw_kk = 0.85 + nrm((L, RW_WIDTH), 0.05)
    rw_ka = 1.0 + nrm((L, RW_WIDTH), 0.05)
    rw_rk = nrm((L, RW_WIDTH), 0.1)
    rw_ln_w = 1.0 + nrm((L, RW_WIDTH), 0.02)
    rw_ln_b = nrm((L, RW_WIDTH), 0.02)
    ml_conv_w = nrm((L, CONV_W, 2 * ML_QK), CONV_W ** -0.5)
    ml_conv_b = nrm((L, 2 * ML_QK), 0.02)
    gate_base = jnp.stack([jnp.zeros((ML_HEADS,), jnp.float32), jnp.linspace(3.0, 6.0, ML_HEADS)])
    ml_gate_b = (gate_base[None, None] + nrm((L, 2, 2, ML_HEADS), 0.1)).reshape(L, 4 * ML_HEADS)
    ml_norm_g = 1.0 + nrm((L, ML_WIDTH), 0.02)
    mb_conv_w = nrm((L, CONV_W, MB_XBC), CONV_W ** -0.5)
    mb_conv_b = nrm((L, MB_XBC), 0.02)
    dt0 = jnp.exp(uni((L, 2, MB_HEADS), math.log(1e-3), math.log(1e-1)))
    mb_dt_bias = dt0 + jnp.log(-jnp.expm1(-dt0))
    mb_a_log = jnp.log(uni((L, 2, MB_HEADS), 1.0, 16.0))
    mb_d = 1.0 + nrm((L, MB_HEADS), 0.1)
    mb_norm_g = 1.0 + nrm((L, MB_WIDTH), 0.02)
    p_a = nrm((L, RW_WIDTH, D), RW_WIDTH ** -0.5)
    p_b = nrm((L, ML_WIDTH, D), ML_WIDTH ** -0.5)
    p_c = nrm((L, MB_WIDTH, D), MB_WIDTH ** -0.5)
    w_out = nrm((L, D, D), D ** -0.5)
    router_w = nrm((L, D, N_EXPERTS), D ** -0.5)
    e_wg = nrm((L, N_EXPERTS, D, EXPERT_FF), D ** -0.5)
    e_wu = nrm((L, N_EXPERTS, D, EXPERT_FF), D ** -0.5)
    e_wd = nrm((L, N_EXPERTS, EXPERT_FF, D), EXPERT_FF ** -0.5)
    final_g = 1.0 + nrm((D,), 0.02)
    return {'x': x, 'c': c, 'ctx': ctx, 'c_ctx': c_ctx, 'ada_w': ada_w, 'ada_b': ada_b,
            'norm1_g': norm1_g, 'norm2_g': norm2_g, 'w_in': w_in,
            'rw_mu': rw_mu, 'rw_w0': rw_w0, 'rw_w2': rw_w2, 'rw_a0': rw_a0, 'rw_a2': rw_a2,
            'rw_g2': rw_g2, 'rw_kk': rw_kk, 'rw_ka': rw_ka, 'rw_rk': rw_rk,
            'rw_ln_w': rw_ln_w, 'rw_ln_b': rw_ln_b,
            'ml_conv_w': ml_conv_w, 'ml_conv_b': ml_conv_b, 'ml_gate_b': ml_gate_b, 'ml_norm_g': ml_norm_g,
            'mb_conv_w': mb_conv_w, 'mb_conv_b': mb_conv_b, 'mb_dt_bias': mb_dt_bias,
            'mb_a_log': mb_a_log, 'mb_d': mb_d, 'mb_norm_g': mb_norm_g,
            'p_a': p_a, 'p_b': p_b, 'p_c': p_c, 'w_out': w_out,
            'router_w': router_w, 'e_wg': e_wg, 'e_wu': e_wu, 'e_wd': e_wd, 'final_g': final_g}


def reference(x, c, ctx, c_ctx, ada_w, ada_b, norm1_g, norm2_g, w_in,
              rw_mu, rw_w0, rw_w2, rw_a0, rw_a2, rw_g2, rw_kk, rw_ka, rw_rk, rw_ln_w, rw_ln_b,
              ml_conv_w, ml_conv_b, ml_gate_b, ml_norm_g,
              mb_conv_w, mb_conv_b, mb_dt_bias, mb_a_log, mb_d, mb_norm_g,
              p_a, p_b, p_c, w_out, router_w, e_wg, e_wu, e_wd, final_g):
    for l in range(DEPTH):
        last = l == DEPTH - 1
        col_major = l % 2 == 1
        mod_l = jax.nn.silu(c) @ ada_w[l] + ada_b[l]
        mod_c = jax.nn.silu(c_ctx) @ ada_w[l] + ada_b[l]
        sh1, sc1, g1, sh2, sc2, g2 = jnp.split(mod_l[:, None, :], 6, axis=-1)
        csh1, csc1, cg1, csh2, csc2, cg2 = jnp.split(mod_c, 6, axis=-1)

        hl = rmsnorm(x, norm1_g[l]) * (1.0 + sc1) + sh1
        hc = rmsnorm(ctx, norm1_g[l]) * (1.0 + csc1) + csh1
        if col_major:
            hl = to_col_major(hl)
        rw_p = (rw_mu[l], rw_w0[l], rw_w2[l], rw_a0[l], rw_a2[l], rw_g2[l],
                rw_kk[l], rw_ka[l], rw_rk[l], rw_ln_w[l], rw_ln_b[l])
        ml_p = (ml_conv_w[l], ml_conv_b[l], ml_gate_b[l], ml_norm_g[l])
        mb_p = (mb_conv_w[l], mb_conv_b[l], mb_dt_bias[l], mb_a_log[l], mb_d[l], mb_norm_g[l])
        oc, ol = token_mixer(hc, hl, w_in[l], rw_p, ml_p, mb_p, p_a[l], p_b[l], p_c[l], w_out[l],
                             need_ctx=not last)
        if col_major:
            ol = from_col_major(ol)
        x = x + g1 * ol

        hl = rmsnorm(x, norm2_g[l]) * (1.0 + sc2) + sh2
        x = x + g2 * expert_choice_ffn(hl, router_w[l], e_wg[l], e_wu[l], e_wd[l])

        if not last:
            ctx = ctx + cg1 * oc
            hc = rmsnorm(ctx, norm2_g[l]) * (1.0 + csc2) + csh2
            ctx = ctx + cg2 * expert_choice_ffn(hc, router_w[l], e_wg[l], e_wu[l], e_wd[l])
    return rmsnorm(x, final_g)
```

```python
import numpy as np
from contextlib import ExitStack
import concourse.bass as bass
import concourse.mybir as mybir
from concourse.bass_utils import run_bass_kernel_spmd

F32 = mybir.dt.float32
AF = mybir.ActivationFunctionType
ALU = mybir.AluOpType
AX = mybir.AxisListType
NDS = 8


class V:
    __slots__ = ("buf", "ap")

    def __init__(s, buf, ap):
        s.buf = buf
        s.ap = ap

    def __getitem__(s, idx):
        return V(s.buf, s.ap[idx])

    def re(s, pat, **kw):
        return V(s.buf, s.ap.rearrange(pat, **kw))

    def bc(s, shape):
        return V(s.buf, s.ap.to_broadcast(list(shape)))

    def pbc(s, n):
        return V(s.buf, s.ap.partition_broadcast(n))


class Buf:
    __slots__ = ("t", "w", "r", "psum")

    def __init__(s, t, psum=False):
        s.t = t
        s.w = None
        s.r = {}
        s.psum = psum

    def __getitem__(s, idx):
        return V(s, s.t[idx])

    def v(s):
        return V(s, s.t[:])


class KB:
    def __init__(s, nc, es):
        s.nc = nc
        s.es = es
        s.eng = {"pe": nc.tensor, "dve": nc.vector, "act": nc.scalar, "pool": nc.gpsimd, "sp": nc.sync}
        s.semobj = {}
        s.cnt = {}
        for e in ("pe", "dve", "act", "pool"):
            s.semobj[e] = es.enter_context(nc.semaphore("s_" + e))
            s.cnt[e] = 0
        s.dq = ("sp", "pool", "act")
        s.dma_rr = {q: 0 for q in s.dq}
        s.dma_val = {}
        for q in s.dq:
            for i in range(NDS):
                s.semobj[(q, i)] = es.enter_context(nc.semaphore("d_%s%d" % (q, i)))
                s.dma_val[(q, i)] = 0
        s.waited = {e: {} for e in s.eng}
        s.nins = 0

    def sb(s, name, shape, dt=F32):
        return Buf(s.es.enter_context(s.nc.sbuf_tensor(name, list(shape), dt)))

    def ps(s, name, shape, dt=F32):
        return Buf(s.es.enter_context(s.nc.psum_tensor(name, list(shape), dt)), psum=True)

    def dram(s, name, shape, dt=F32, kind="Internal"):
        return Buf(s.nc.dram_tensor(name, list(shape), dt, kind=kind).ap())

    def _waits(s, e, reads, writes, is_dma):
        deps = {}

        def need(ev):
            if ev is None:
                return
            k, val, src = ev
            if src == "pe" and e == "pe" and not is_dma:
                return
            if deps.get(k, 0) < val:
                deps[k] = val

        for x in reads:
            need(x.buf.w)
            if x.buf.psum:
                for k, (val, src) in x.buf.r.items():
                    if src != e:
                        need((k, val, src))
        for x in writes:
            need(x.buf.w)
            for k, (val, src) in x.buf.r.items():
                need((k, val, src))
        w = s.waited[e]
        for k, val in deps.items():
            if w.get(k, 0) < val:
                s.eng[e].wait_ge(s.semobj[k], val)
                w[k] = val
                s.nins += 1

    def _post(s, ev, reads, writes):
        k, val, src = ev
        for x in reads:
            x.buf.r[k] = (val, src)
        for x in writes:
            x.buf.w = ev
            x.buf.r = {}

    def op(s, e, fn, reads, writes):
        s._waits(e, reads, writes, False)
        ins = fn()
        s.cnt[e] += 1
        ins.then_inc(s.semobj[e], 1)
        s.nins += 1
        s._post((e, s.cnt[e], e), reads, writes)

    def dma(s, out, in_, q="sp", **kw):
        s._waits(q, [in_], [out], True)
        slot = s.dma_rr[q]
        s.dma_rr[q] = (slot + 1) % NDS
        k = (q, slot)
        prev = s.dma_val[k]
        if s.waited[q].get(k, 0) < prev:
            s.eng[q].wait_ge(s.semobj[k], prev)
            s.waited[q][k] = prev
        ins = s.eng[q].dma_start(out=out.ap, in_=in_.ap, **kw)
        ins.then_inc(s.semobj[k], 16)
        s.nins += 1
        s.dma_val[k] = prev + 16
        s._post((k, prev + 16, "dma"), [in_], [out])

    def finish(s, bufs):
        for b in bufs:
            ev = b.w
            if ev is not None:
                k, val, _ = ev
                s.eng["sp"].wait_ge(s.semobj[k], val)
        for k, val in s.dma_val.items():
            if val:
                s.eng["sp"].wait_ge(s.semobj[k], val)

    def mm(s, out, lhsT, rhs, start=True, stop=True):
        s.op("pe", lambda: s.nc.tensor.matmul(out.ap, lhsT.ap, rhs.ap, start=start, stop=stop),
             [lhsT, rhs], [out])

    def tr(s, out, in_, ident):
        s.op("pe", lambda: s.nc.tensor.transpose(out.ap, in_.ap, ident.ap), [in_, ident], [out])

    def act(s, out, in_, func, bias=None, scale=1.0, accum=None):
        rd = [in_]
        kw = {}
        if isinstance(bias, V):
            rd.append(bias)
            kw["bias"] = bias.ap
        elif bias is not None:
            kw["bias"] = bias
        if isinstance(scale, V):
            rd.append(scale)
            kw["scale"] = scale.ap
        else:
            kw["scale"] = scale
        wr = [out]
        if accum is not None:
            wr.append(accum)
            kw["accum_out"] = accum.ap
        s.op("act", lambda: s.nc.scalar.activation(out=out.ap, in_=in_.ap, func=func, **kw), rd, wr)

    def tt(s, out, a, b, op, e="dve"):
        s.op(e, lambda: s.eng[e].tensor_tensor(out.ap, a.ap, b.ap, op), [a, b], [out])

    def ts(s, out, a, s1, s2=None, op0=ALU.mult, op1=None, accum=None, e="dve"):
        rd = [a]
        a1 = s1.ap if isinstance(s1, V) else s1
        a2 = s2.ap if isinstance(s2, V) else s2
        if isinstance(s1, V):
            rd.append(s1)
        if isinstance(s2, V):
            rd.append(s2)
        kw = {}
        if op1 is not None:
            kw["op1"] = op1
        wr = [out]
        if accum is not None:
            kw["accum_out"] = accum.ap
            wr.append(accum)
        s.op(e, lambda: s.eng[e].tensor_scalar(out.ap, a.ap, a1, a2, op0, **kw), rd, wr)

    def stt(s, out, a, sc, b, op0, op1, e="dve"):
        rd = [a, b]
        a1 = sc.ap if isinstance(sc, V) else sc
        if isinstance(sc, V):
            rd.append(sc)
        s.op(e, lambda: s.eng[e].scalar_tensor_tensor(out.ap, a.ap, a1, b.ap, op0, op1), rd, [out])

    def copy(s, out, in_, e="dve"):
        if e == "act":
            s.act(out, in_, AF.Copy)
        else:
            s.op(e, lambda: s.eng[e].tensor_copy(out.ap, in_.ap), [in_], [out])

    def memset(s, out, val, e="dve"):
        s.op(e, lambda: s.eng[e].memset(out.ap, val), [], [out])

    def recip(s, out, in_):
        s.op("dve", lambda: s.nc.vector.reciprocal(out.ap, in_.ap), [in_], [out])

    def reduce(s, out, in_, op, axis=AX.X):
        s.op("dve", lambda: s.nc.vector.tensor_reduce(out.ap, in_.ap, axis, op), [in_], [out])


def _kb_barrier(s):
    for e in s.eng:
        w = s.waited[e]
        for k in ("pe", "dve", "act", "pool"):
            if w.get(k, 0) < s.cnt[k]:
                s.eng[e].wait_ge(s.semobj[k], s.cnt[k])
                w[k] = s.cnt[k]
                s.nins += 1
        for k, val in s.dma_val.items():
            if val and w.get(k, 0) < val:
                s.eng[e].wait_ge(s.semobj[k], val)
                w[k] = val
                s.nins += 1


def _kb_init_arena(s, n):
    s.arena = s.es.enter_context(s.nc.sbuf_tensor("arena", [128, n], F32))
    s.aoff = 0
    s.an = n


def _kb_A(s, shape):
    n = int(np.prod(shape[1:]))
    assert s.aoff + n <= s.an, ("arena overflow", s.aoff, n, s.an)
    ap = s.arena[0:shape[0], s.aoff:s.aoff + n]
    s.aoff += n
    if len(shape) == 3:
        ap = ap.rearrange("p (a b) -> p a b", a=shape[1])
    elif len(shape) == 4:
        ap = ap.rearrange("p (a b c) -> p a b c", a=shape[1], b=shape[2])
    return Buf(ap)


def _kb_release(s, mark=0):
    s.barrier()
    s.aoff = mark


def _kb_scan(s, out, d0, d1, init, op0, op1):
    s.op("dve", lambda: s.nc.vector.tensor_tensor_scan(out.ap, d0.ap, d1.ap, init, op0, op1), [d0, d1], [out])


KB.barrier = _kb_barrier
KB.init_arena = _kb_init_arena
KB.A = _kb_A
KB.release = _kb_release
KB.scan = _kb_scan

D = 1024
T = 2304
NT = 18
EPS = 1e-6
GROUPS = [(0, 512), (512, 512), (1024, 512), (1536, 512), (2048, 256)]
SEGS = [(0, 256), (256, 2304)]
C_MLQK, C_MLV, C_MLO, C_MLG, C_Z, C_XBC, C_DT, C_G = 1792, 2304, 2816, 3328, 3344, 4368, 6416, 6448
CHUNK_ORDER = {0: list(range(18)), 1: [1, 0] + list(range(17, 1, -1))}

CP_OFF = {}
_c = 0
for _n, _w in [("mu", 14), ("w0_0", 4), ("w0_1", 4), ("a0_0", 4), ("a0_1", 4), ("kk", 4), ("ka", 4), ("rk", 4),
               ("lnw", 4), ("lnb", 4)] + [("mlcw%d" % k, 4) for k in range(5)] + [("mlcb", 4), ("mlng", 4)] + \
              [("mbcw%d" % k, 16) for k in range(5)] + [("mbcb", 16), ("mbng", 8), ("om", 14), ("hm", 14)]:
    CP_OFF[_n] = _c
    _c += _w
NCP = _c
RP_N1, RP_N2, RP_MLGB, RP_DTB, RP_ALOG, RP_MBD, NRP = 0, 1024, 2048, 2064, 2096, 2128, 3152
CS_ID, CS_UI, CS_US, CS_LI, CS_LS, CS_ONE, CS_BLK, CS_IOTA, CS_RST, NCS = 0, 128, 256, 384, 512, 640, 768, 896, 1152, 1152 + 2304


def host_consts():
    p = np.arange(128)[:, None]
    f = np.arange(128)[None, :]
    c = np.zeros((128, NCS), np.float32)
    c[:, CS_ID:CS_ID + 128] = (p == f)
    c[:, CS_UI:CS_UI + 128] = (p <= f)
    c[:, CS_US:CS_US + 128] = (p < f)
    c[:, CS_LI:CS_LI + 128] = (p >= f)
    c[:, CS_LS:CS_LS + 128] = (p > f)
    c[:, CS_ONE:CS_ONE + 128] = 1.0
    c[:, CS_BLK:CS_BLK + 128] = (p // 64 == f // 64)
    c[:, CS_IOTA:CS_IOTA + 256] = np.arange(1, 257)[None, :]
    rst = np.ones(2304, np.float32)
    rst[::128] = 0.0
    c[:, CS_RST:CS_RST + 2304] = rst[None, :]
    return c


def host_colpack(P, l):
    cp = np.zeros((128, NCP), np.float32)

    def put(name, vec):
        v = np.asarray(vec, np.float32).reshape(-1, 128).T
        cp[:, CP_OFF[name]:CP_OFF[name] + v.shape[1]] = v

    put("mu", P["rw_mu"][l])
    for d in range(2):
        put("w0_%d" % d, P["rw_w0"][l, d])
        put("a0_%d" % d, P["rw_a0"][l, d])
    put("kk", P["rw_kk"][l]); put("ka", P["rw_ka"][l]); put("rk", P["rw_rk"][l])
    put("lnw", P["rw_ln_w"][l]); put("lnb", P["rw_ln_b"][l])
    for k in range(5):
        put("mlcw%d" % k, P["ml_conv_w"][l, k])
        put("mbcw%d" % k, P["mb_conv_w"][l, k])
    put("mlcb", P["ml_conv_b"][l]); put("mlng", P["ml_norm_g"][l])
    put("mbcb", P["mb_conv_b"][l]); put("mbng", P["mb_norm_g"][l])
    return cp


def host_rowpack(P, l):
    r = np.zeros((NRP,), np.float32)
    r[RP_N1:RP_N1 + 1024] = P["norm1_g"][l]
    r[RP_N2:RP_N2 + 1024] = P["norm2_g"][l]
    r[RP_MLGB:RP_MLGB + 16] = P["ml_gate_b"][l]
    r[RP_DTB:RP_DTB + 32] = P["mb_dt_bias"][l].reshape(-1)
    r[RP_ALOG:RP_ALOG + 32] = P["mb_a_log"][l].reshape(-1)
    r[RP_MBD:RP_MBD + 1024] = np.repeat(P["mb_d"][l], 64)
    return r

RW_CUT = 0


def build(NB=2, NL=4, dbg=(), stop=None, LW=4, EW=16):
    nc = bass.Bass("TRN2", target_bir_lowering=False)
    es = ExitStack()
    with es:
        kb = KB(nc, es)

        def din(name, shape):
            return kb.dram(name, shape, kind="ExternalInput")

        def scratch(name, shape):
            return kb.dram(name, shape, kind=("ExternalOutput" if name in dbg else "Internal"))

        x_in = din("x", [NB, 2048, D]); ctx_in = din("ctx", [NB, 256, D])
        ccol = din("ccol", [NB, 128, 8]); cctx = din("cctx", [128, 8])
        ada_w = din("ada_w", [LW, D, 6 * D]); ada_b = din("ada_b", [LW, 6 * D]); w_in = din("w_in", [LW, D, 9520])
        rw_w2 = din("rw_w2", [LW, 2, 64, 512]); rw_a2 = din("rw_a2", [LW, 2, 64, 512]); rw_g2 = din("rw_g2", [LW, 128, 512])
        p_a = din("p_a", [LW, 512, D]); p_b = din("p_b", [LW, 512, D]); p_c = din("p_c", [LW, D, D]); w_out = din("w_out", [LW, D, D])
        router_w = din("router_w", [LW, D, 16])
        e_wg = din("e_wg", [LW, EW, D, D]); e_wu = din("e_wu", [LW, EW, D, D]); e_wd = din("e_wd", [LW, EW, D, D])
        final_g = din("final_g", [1, D]); colp = din("colp", [LW, 128, NCP]); rowp = din("rowp", [LW, NRP])
        cst = din("cst", [128, NCS])
        out = kb.dram("out", [NB, 2048, D], kind="ExternalOutput")
        XS = scratch("XS", [NB, T, D]); MOD = scratch("MOD", [2, 6 * D]); UF = scratch("UF", [9520, T])
        VML = scratch("VML", [T, 512]); ZT = scratch("ZT", [T, 1024]); SMALL = scratch("SMALL", [T, 48])
        GG = scratch("GG", [512, T]); YA = scratch("YA", [512, T]); YB = scratch("YB", [512, T]); YC = scratch("YC", [1024, T])
        H2 = scratch("H2", [T, D])

        PS = [kb.ps("ps%d" % i, [128, 1024]) for i in range(4)]
        CST = kb.sb("CST", [128, NCS]); CP = kb.sb("CP", [128, NCP])
        kb.init_arena(45000)
        kb.dma(CST.v(), cst.v())
        ident = CST[:, CS_ID:CS_ID + 128]
        ones = CST[:, CS_ONE:CS_ONE + 128]
        blk = CST[:, CS_BLK:CS_BLK + 128]
        state = {"ev": 0}

        def evac(o, i, func=None):
            if func is not None:
                kb.act(o, i, func)
                return
            state["ev"] += 1
            if state["ev"] % 2:
                kb.act(o, i, AF.Copy)
            else:
                kb.copy(o, i)

        def xrows(b, l, i):
            if i < 2:
                return [(slice(0, 128), XS[b, i * 128:(i + 1) * 128, :])]
            j = i - 2
            if l % 2 == 0:
                return [(slice(0, 128), XS[b, 256 + j * 128:256 + (j + 1) * 128, :])]
            lat = XS[b, 256:T, :].re("(r c) d -> c r d", c=64)
            return [(slice(cc * 32, (cc + 1) * 32), lat[4 * j + cc]) for cc in range(4)]

        def load_x(b, l, i, xt, q="sp"):
            for ps_, src in xrows(b, l, i):
                kb.dma(xt[ps_, :], src, q=q)

        def store_x(b, l, i, xt, q="pool"):
            for ps_, dst in xrows(b, l, i):
                kb.dma(dst, xt[ps_, :], q=q)

        def rmsnorm(xt, h, gsc, sh, junk, ssq, rstd):
            kb.act(junk, xt, AF.Square, accum=ssq)
            kb.ts(rstd, ssq, 1.0 / D, EPS, op0=ALU.mult, op1=ALU.add)
            kb.act(rstd, rstd, AF.Sqrt)
            kb.recip(rstd, rstd)
            kb.stt(h, xt, rstd, gsc, ALU.mult, ALU.mult)
            if sh is not None:
                kb.tt(h, h, sh, ALU.add)

        def transpose_tile(h, dst_fn):
            for half in range(2):
                p = PS[half]
                for kk in range(4):
                    k = half * 4 + kk
                    kb.tr(p[:, kk * 128:(kk + 1) * 128], h[:, k * 128:(k + 1) * 128], ident)
                evac(dst_fn(half * 4), p[:, 0:512].re("p (k n) -> p k n", k=4))

        def phase_mod(b, l):
            kb.release(0)
            cc = kb.A([128, 8, 2]); modsb = kb.A([2, 6 * D]); bias = kb.A([2, 6 * D])
            W = [kb.A([128, 8, 512]) for _ in range(2)]
            kb.dma(cc[:, :, 0], ccol[b], allow_slow_non_contiguous=True)
            kb.dma(cc[:, :, 1], cctx.v(), allow_slow_non_contiguous=True)
            kb.act(cc.v(), cc.v(), AF.Silu)
            kb.dma(bias.v(), V(ada_b, ada_b.t[l, :].partition_broadcast(2)))
            for n in range(12):
                wb = W[n % 2]
                kb.dma(wb.v(), ada_w[l, :, n * 512:(n + 1) * 512].re("(k p) n -> p k n", p=128), q=("sp" if n % 2 else "pool"))
                ps = PS[n % 2][0:2, 0:512]
                for k in range(8):
                    kb.mm(ps, cc[:, k, :], wb[:, k, :], start=(k == 0), stop=(k == 7))
                kb.tt(modsb[:, n * 512:(n + 1) * 512], ps, bias[:, n * 512:(n + 1) * 512], ALU.add)
            kb.dma(MOD.v(), modsb.v())
            kb.dma(CP.v(), colp[l])
            om = CP_OFF["om"]; hm = CP_OFF["hm"]; mu = CP_OFF["mu"]
            kb.ts(CP[:, om:om + 14], CP[:, mu:mu + 14], -1.0, 1.0, op0=ALU.mult, op1=ALU.add)
            kb.ts(CP[:, hm:hm + 14], CP[:, mu:mu + 14], 0.5, None, op0=ALU.mult)

        def mod_tiles(l, which, sc_off, sh_off, g_off):
            gsc = kb.A([128, D]); sh = kb.A([128, D]); grow = kb.A([128, D])
            kb.dma(grow.v(), V(rowp, rowp.t[l, g_off:g_off + D].partition_broadcast(128)))
            kb.dma(gsc.v(), V(MOD, MOD.t[which, sc_off:sc_off + D].partition_broadcast(128)))
            kb.dma(sh.v(), V(MOD, MOD.t[which, sh_off:sh_off + D].partition_broadcast(128)))
            kb.stt(gsc.v(), gsc.v(), 1.0, grow.v(), ALU.add, ALU.mult)
            return gsc, sh

        def phase_in(b, l):
            kb.release(0)
            hT = kb.A([128, 8, T])
            m0 = kb.aoff
            gs = {}
            for which in (0, 1):
                gs[which] = mod_tiles(l, which, 1024, 0, RP_N1)
            xts = [kb.A([128, D]) for _ in range(2)]; hs = [kb.A([128, D]) for _ in range(2)]
            junk = kb.A([128, D]); ssq = kb.A([128, 1]); rstd = kb.A([128, 1])
            for i in range(NT):
                xt = xts[i % 2]; h = hs[i % 2]
                load_x(b, l, i, xt)
                gsc, sh = gs[1 if i < 2 else 0]
                rmsnorm(xt.v(), h.v(), gsc.v(), sh.v(), junk.v(), ssq.v(), rstd.v())
                transpose_tile(h, lambda k0: hT[:, k0:k0 + 4, i * 128:(i + 1) * 128])
            kb.release(m0)
            W = [kb.A([128, 8, 512]) for _ in range(2)]
            RB = [kb.A([128, T]) for _ in range(2)]; RS = kb.A([128, T]); RO = [kb.A([128, T]) for _ in range(2)]
            TO = [kb.A([128, 1024]) for _ in range(2)]
            cnt = {"w": 0, "m": 0, "p": 0}

            def load_w(c0, ncol):
                wb = W[cnt["w"] % 2]
                q = "sp" if cnt["w"] % 2 else "pool"
                cnt["w"] += 1
                kb.dma(wb[:, :, 0:ncol], w_in[l, :, c0:c0 + ncol].re("(k p) n -> p k n", p=128), q=q)
                return wb

            def nextps():
                cnt["p"] += 1
                return PS[cnt["p"] % 4]

            for (g0, gn, kind) in [(0, 1792, "rw"), (C_MLQK, 512, "copy"), (C_MLO, 512, "sig"), (C_XBC, 2048, "copy"), (C_G, 3072, "sig")]:
                for c0 in range(g0, g0 + gn, 512):
                    ncol = min(512, g0 + gn - c0)
                    wb = load_w(c0, ncol)
                    for m in range(ncol // 128):
                        row0 = c0 + m * 128
                        rb = RB[cnt["m"] % 2]; ro = RO[cnt["m"] % 2]
                        cnt["m"] += 1
                        dst = rb if kind == "rw" else ro
                        for (t0, tw) in GROUPS:
                            ps = nextps()[:, 0:tw]
                            for k in range(8):
                                kb.mm(ps, wb[:, k, m * 128:(m + 1) * 128], hT[:, k, t0:t0 + tw], start=(k == 0), stop=(k == 7))
                            evac(dst[:, t0:t0 + tw], ps, AF.Sigmoid if kind == "sig" else None)
                        if kind == "rw":
                            ti = row0 // 128
                            for (a, e_) in SEGS:
                                kb.tt(RS[:, a + 1:e_ - 1], rb[:, a:e_ - 2], rb[:, a + 2:e_], ALU.add)
                                kb.copy(RS[:, a:a + 1], rb[:, a + 1:a + 2])
                                kb.copy(RS[:, e_ - 1:e_], rb[:, e_ - 2:e_ - 1])
                            kb.ts(ro.v(), rb.v(), CP[:, CP_OFF["om"] + ti:CP_OFF["om"] + ti + 1], None, op0=ALU.mult)
                            kb.stt(ro.v(), RS.v(), CP[:, CP_OFF["hm"] + ti:CP_OFF["hm"] + ti + 1], ro.v(), ALU.mult, ALU.add)
                            if ti == 12:
                                kb.act(ro[0:64, :], ro[0:64, :], AF.Tanh)
                            if ti == 13:
                                kb.act(ro.v(), ro.v(), AF.Sigmoid)
                        kb.dma(UF[row0:row0 + 128, :], ro.v(), q="sp")
            for (c0, ncol, dstD, func) in [(C_MLV, 512, VML, None), (C_Z, 512, ZT, AF.Silu), (C_Z + 512, 512, ZT, AF.Silu)]:
                wb = load_w(c0, ncol)
                dc0 = (c0 - C_Z) if dstD is ZT else 0
                for i in range(NT):
                    ps = nextps()[:, 0:512]
                    for k in range(8):
                        kb.mm(ps, hT[:, k, i * 128:(i + 1) * 128], wb[:, k, :], start=(k == 0), stop=(k == 7))
                    to = TO[i % 2]
                    evac(to[:, 0:512], ps, func)
                    kb.dma(dstD[i * 128:(i + 1) * 128, dc0:dc0 + 512], to[:, 0:512], q="sp")
            wb = W[cnt["w"] % 2]; cnt["w"] += 1
            kb.dma(wb[:, :, 0:16], w_in[l, :, C_MLG:C_MLG + 16].re("(k p) n -> p k n", p=128))
            kb.dma(wb[:, :, 16:48], w_in[l, :, C_DT:C_DT + 32].re("(k p) n -> p k n", p=128))
            for i in range(NT):
                ps = nextps()[:, 0:48]
                for k in range(8):
                    kb.mm(ps, hT[:, k, i * 128:(i + 1) * 128], wb[:, k, 0:48], start=(k == 0), stop=(k == 7))
                to = TO[i % 2]
                evac(to[:, 0:48], ps)
                kb.dma(SMALL[i * 128:(i + 1) * 128, :], to[:, 0:48], q="sp")

        PHASES = {"mod": phase_mod, "in": phase_in}

        MASK4 = [kb.sb("mask4_%d" % d, [128, 2, 4, 128]) for d in range(2)]
        MST2 = [kb.sb("mst2_%d" % d, [128, 2, 128]) for d in range(2)]
        ID2 = kb.sb("id2", [128, 2, 128])
        TRI = {0: (CS_UI, CS_US, CS_LS), 1: (CS_LI, CS_LS, CS_US)}
        for d in range(2):
            inc, stc, stT = TRI[d]
            for h in range(2):
                for q in range(4):
                    src = stc if q < 2 else inc
                    kb.copy(MASK4[d][:, h, q, :], CST[:, src:src + 128])
                kb.copy(MST2[d][:, h, :], CST[:, stT:stT + 128])
        for h in range(2):
            kb.copy(ID2[:, h, :], ident)
        pcnt = {"p": 0}

        def nextps():
            pcnt["p"] += 1
            return PS[pcnt["p"] % 4]

        def bc3(v2, n):
            c = v2.ap.shape[1]
            return V(v2.buf, v2.ap.rearrange("p (c o) -> p c o", o=1).to_broadcast([v2.ap.shape[0], c, n]))

        def phase_rwkv(b, l):
            kb.release(0)
            RST = CST[:, CS_RST:CS_RST + T]
            TWXA = kb.A([128, T]); kb.dma(TWXA.v(), UF[1536:1664, :])
            W2A2 = kb.A([128, 2, 512])
            for d in range(2):
                kb.dma(W2A2[0:64, d, :], rw_w2[l, d]); kb.dma(W2A2[64:128, d, :], rw_a2[l, d])
            m1 = kb.aoff
            SG = kb.A([128, T]); kb.dma(SG.v(), UF[1664:1792, :])
            G2 = kb.A([128, 512]); kb.dma(G2.v(), rw_g2[l])
            ro = kb.A([128, T])
            for m in range(4):
                for (t0, tw) in GROUPS:
                    ps = nextps()[:, 0:tw]
                    kb.mm(ps, G2[:, m * 128:(m + 1) * 128], SG[:, t0:t0 + tw])
                    evac(ro[:, t0:t0 + tw], ps)
                kb.dma(GG[m * 128:(m + 1) * 128, :], ro.v())
            if RW_CUT == 1:
                return
            for pp in range(4 if RW_CUT == 0 else 1):
                kb.release(m1)
                Rr = kb.A([128, T]); Kk = kb.A([128, T]); Vv = kb.A([128, T]); KK = kb.A([128, T])
                TMP = kb.A([128, T]); TMP2 = kb.A([128, T]); VT = kb.A([128, 18, 128]); YACC = kb.A([128, T]); BON = kb.A([128, T])
                LWt = kb.A([128, T]); BB = kb.A([128, T]); KD = kb.A([128, T]); Rt = kb.A([128, T]); KKt = kb.A([128, T])
                BtW = kb.A([128, T]); KDtW = kb.A([128, T])
                CW = TMP; E = TMP2
                TOT = kb.A([128, 18]); ETOT = kb.A([128, 18]); S = kb.A([128, 64]); SP = kb.A([128, 2, 64])
                AM = kb.A([128, 2, 4, 128]); MTa = kb.A([128, 2, 128]); Pm = kb.A([128, 2, 128])
                MB = [(kb.A([128, 2, 128]), kb.A([128, 2, 128])) for _ in range(2)]
                Xn = kb.A([128, 128]); Ut = kb.A([128, 128]); BKt = kb.A([128, 256])
                kb.dma(Rr.v(), UF[pp * 128:(pp + 1) * 128, :])
                kb.dma(Kk.v(), UF[512 + pp * 128:512 + (pp + 1) * 128, :], q="pool")
                kb.dma(Vv.v(), UF[1024 + pp * 128:1024 + (pp + 1) * 128, :])

                def col(name, i=pp):
                    return CP[:, CP_OFF[name] + i:CP_OFF[name] + i + 1]

                kb.ts(KK.v(), Kk.v(), col("kk"), None, op0=ALU.mult)
                kb.act(TMP.v(), KK.v(), AF.Square)
                for (t0, tw) in GROUPS:
                    ps = nextps()[:, 0:tw]
                    kb.mm(ps, blk, TMP[:, t0:t0 + tw])
                    kb.ts(TMP2[:, t0:t0 + tw], ps, 1e-12, None, op0=ALU.add)
                kb.act(TMP2.v(), TMP2.v(), AF.Sqrt); kb.recip(TMP2.v(), TMP2.v())
                kb.tt(KK.v(), KK.v(), TMP2.v(), ALU.mult)
                for c in range(18):
                    ps = nextps()[:, 0:128]
                    kb.tr(ps, Vv[:, c * 128:(c + 1) * 128], ident)
                    evac(VT[:, c, :], ps)
                if RW_CUT == 2:
                    return
                for d in range(2):
                    for (t0, tw) in GROUPS:
                        ps = nextps()[:, 0:tw]
                        kb.mm(ps, W2A2[0:64, d, pp * 128:(pp + 1) * 128], TWXA[0:64, t0:t0 + tw])
                        kb.act(LWt[:, t0:t0 + tw], ps, AF.Sigmoid, bias=col("w0_%d" % d))
                        ps = nextps()[:, 0:tw]
                        kb.mm(ps, W2A2[64:128, d, pp * 128:(pp + 1) * 128], TWXA[64:128, t0:t0 + tw])
                        kb.act(BB[:, t0:t0 + tw], ps, AF.Sigmoid, bias=col("a0_%d" % d))
                    kb.ts(LWt.v(), LWt.v(), -float(np.exp(-0.5)), None, op0=ALU.mult)
                    kb.ts(KD.v(), BB.v(), -1.0, col("ka"), op0=ALU.add, op1=ALU.mult)
                    kb.stt(KD.v(), KD.v(), 1.0, Kk.v(), ALU.add, ALU.mult)
                    if d == 0:
                        kb.stt(BON.v(), KD.v(), col("rk"), Rr.v(), ALU.mult, ALU.mult)
                    else:
                        kb.stt(E.v(), KD.v(), col("rk"), Rr.v(), ALU.mult, ALU.mult)
                        kb.tt(BON.v(), BON.v(), E.v(), ALU.add)
                    kb.tt(BB.v(), BB.v(), KK.v(), ALU.mult)
                    kb.scan(CW.v(), RST, LWt.v(), 0.0, ALU.mult, ALU.add)
                    CW3 = CW.v().re("p (c t) -> p c t", t=128)
                    kb.copy(TOT.v(), CW3[:, :, 127])
                    if d == 1:
                        kb.tt(CW3, bc3(TOT.v(), 128), CW3, ALU.subtract)
                        kb.tt(CW.v(), CW.v(), LWt.v(), ALU.add)
                    kb.act(ETOT.v(), TOT.v(), AF.Exp)
                    kb.act(E.v(), CW.v(), AF.Exp)
                    kb.tt(Rt.v(), Rr.v(), E.v(), ALU.mult)
                    kb.tt(E.v(), CW.v(), LWt.v(), ALU.subtract)
                    kb.act(E.v(), E.v(), AF.Exp)
                    kb.tt(KKt.v(), KK.v(), E.v(), ALU.mult)
                    kb.act(E.v(), CW.v(), AF.Exp, scale=-1.0)
                    kb.tt(BB.v(), BB.v(), E.v(), ALU.mult)
                    kb.tt(KD.v(), KD.v(), E.v(), ALU.mult)
                    r3 = "p (c t) -> p c t"
                    kb.tt(BtW.v().re(r3, t=128), BB.v().re(r3, t=128), bc3(ETOT.v(), 128), ALU.mult)
                    kb.tt(KDtW.v().re(r3, t=128), KD.v().re(r3, t=128), bc3(ETOT.v(), 128), ALU.mult)
                    if RW_CUT == 3:
                        return
                    kb.memset(S.v(), 0.0)
                    kb.memset(SP.v(), 0.0)
                    for c in CHUNK_ORDER[d]:
                        cs = slice(c * 128, (c + 1) * 128)
                        pT = nextps()
                        kb.tr(pT[:, 0:128], BtW[:, cs], ident)
                        kb.tr(pT[:, 128:256], KDtW[:, cs], ident)
                        evac(BKt.v(), pT[:, 0:256])
                        pA = nextps(); pB = nextps()
                        for hh in range(2):
                            bs = slice(hh * 64, hh * 64 + 64)
                            o = hh * 512
                            kb.mm(pA[:, o:o + 128], BB[bs, cs], KKt[bs, cs])
                            kb.mm(pA[:, o + 128:o + 256], KD[bs, cs], KKt[bs, cs])
                            kb.mm(pA[:, o + 256:o + 384], BB[bs, cs], Rt[bs, cs])
                            kb.mm(pA[:, o + 384:o + 512], KD[bs, cs], Rt[bs, cs])
                            kb.mm(pB[:, o:o + 128], KKt[bs, cs], BB[bs, cs])
                        kb.tt(AM.v(), pA.v().re("p (h q t) -> p h q t", h=2, q=4), MASK4[d].v(), ALU.mult)
                        kb.tt(MTa.v(), pB.v().re("p (h x) -> p h x", h=2)[:, :, 0:128], MST2[d].v(), ALU.mult)
                        kb.tt(Pm.v(), ID2.v(), AM[:, :, 0, :], ALU.subtract)
                        if RW_CUT == 4:
                            return
                        Mk = AM[:, :, 0, :]; MkT = MTa.v()
                        for st in range(6):
                            if RW_CUT >= 10 and st >= RW_CUT - 10:
                                return
                            pC = nextps()
                            for hh in range(2):
                                kb.mm(pC[:, hh * 128:(hh + 1) * 128], Mk[:, hh, :], MkT[:, hh, :])
                                if st < 5:
                                    kb.mm(pC[:, 256 + hh * 128:256 + (hh + 1) * 128], MkT[:, hh, :], Mk[:, hh, :])
                            if RW_CUT == 21:
                                return
                            nMkT, nMk = MB[st % 2]
                            evac(nMkT.v(), pC[:, 0:256].re("p (h t) -> p h t", h=2))
                            if RW_CUT == 22:
                                return
                            if st < 5:
                                evac(nMk.v(), pC[:, 256:512].re("p (h t) -> p h t", h=2))
                            if RW_CUT == 9:
                                return
                            pD = nextps()
                            for hh in range(2):
                                kb.mm(pD[:, hh * 128:(hh + 1) * 128], nMkT[:, hh, :], Pm[:, hh, :])
                            kb.tt(Pm.v(), Pm.v(), pD[:, 0:256].re("p (h t) -> p h t", h=2), ALU.add)
                            Mk, MkT = nMk.v(), nMkT.v()
                        if RW_CUT == 5:
                            return
                        pX = nextps()
                        for hh in range(2):
                            hs_ = slice(hh * 64, (hh + 1) * 64)
                            kb.mm(pX[:, hs_], KKt[:, cs], SP[:, hh, :], start=True, stop=False)
                            kb.mm(pX[:, hs_], AM[:, hh, 1, :], VT[:, c, hs_], start=False, stop=True)
                        kb.ts(Xn.v(), pX[:, 0:128], -1.0, None, op0=ALU.mult)
                        pU = nextps()
                        for hh in range(2):
                            hs_ = slice(hh * 64, (hh + 1) * 64)
                            kb.mm(pU[:, hs_], Pm[:, hh, :], Xn[:, hs_])
                        evac(Ut.v(), pU[:, 0:128])
                        pY = nextps()
                        for hh in range(2):
                            bs = slice(hh * 64, hh * 64 + 64); hs_ = slice(hh * 64, (hh + 1) * 64)
                            kb.mm(pY[bs, 0:128], SP[:, hh, :], Rt[:, cs], start=True, stop=False)
                            kb.mm(pY[bs, 0:128], Ut[:, hs_], AM[:, hh, 2, :], start=False, stop=False)
                            kb.mm(pY[bs, 0:128], VT[:, c, hs_], AM[:, hh, 3, :], start=False, stop=True)
                        if d == 0:
                            evac(YACC[:, cs], pY[:, 0:128])
                        else:
                            kb.tt(YACC[:, cs], YACC[:, cs], pY[:, 0:128], ALU.add)
                        if RW_CUT == 6:
                            return
                        pS = nextps()
                        for hh in range(2):
                            bs = slice(hh * 64, hh * 64 + 64); hs_ = slice(hh * 64, (hh + 1) * 64)
                            kb.mm(pS[bs, 0:64], BKt[:, hs_], Ut[:, hs_], start=True, stop=False)
                            kb.mm(pS[bs, 0:64], BKt[:, 128 + hh * 64:128 + (hh + 1) * 64], VT[:, c, hs_], start=False, stop=True)
                        kb.stt(S.v(), S.v(), ETOT[:, c:c + 1], pS[:, 0:64], ALU.mult, ALU.add)
                        for hh in range(2):
                            kb.ts(SP[:, hh, :], S.v(), blk[:, hh * 64:hh * 64 + 1], None, op0=ALU.mult)
                for (t0, tw) in GROUPS:
                    g_ = slice(t0, t0 + tw)
                    ps = nextps()[:, 0:tw]
                    kb.mm(ps, blk, YACC[:, g_])
                    kb.stt(TMP[:, g_], ps, -1.0 / 64, YACC[:, g_], ALU.mult, ALU.add)
                kb.act(TMP2.v(), TMP.v(), AF.Square)
                for (t0, tw) in GROUPS:
                    g_ = slice(t0, t0 + tw)
                    ps = nextps()[:, 0:tw]
                    kb.mm(ps, blk, TMP2[:, g_])
                    kb.ts(LWt[:, g_], ps, 1.0 / 64, 64e-5, op0=ALU.mult, op1=ALU.add)
                kb.act(LWt.v(), LWt.v(), AF.Sqrt); kb.recip(LWt.v(), LWt.v())
                kb.tt(TMP.v(), TMP.v(), LWt.v(), ALU.mult)
                kb.ts(TMP.v(), TMP.v(), col("lnw"), col("lnb"), op0=ALU.mult, op1=ALU.add)
                for (t0, tw) in GROUPS:
                    g_ = slice(t0, t0 + tw)
                    ps = nextps()[:, 0:tw]
                    kb.mm(ps, blk, BON[:, g_])
                    kb.tt(TMP2[:, g_], ps, Vv[:, g_], ALU.mult)
                kb.tt(TMP.v(), TMP.v(), TMP2.v(), ALU.add)
                kb.dma(TMP2.v(), GG[pp * 128:(pp + 1) * 128, :])
                kb.tt(TMP.v(), TMP.v(), TMP2.v(), ALU.mult)
                kb.dma(YA[pp * 128:(pp + 1) * 128, :], TMP.v())

        PHASES["rwkv"] = phase_rwkv

        def bcmid(v2, n):
            t_ = v2.ap.shape[1]
            return V(v2.buf, v2.ap.rearrange("p (o t) -> p o t", o=1).to_broadcast([v2.ap.shape[0], n, t_]))

        def cview(off):
            return CST[:, off:off + 128]

        DIRC = {0: (CS_UI, CS_LS), 1: (CS_LI, CS_US)}

        def conv_silu(x, acc, wname, bname, ti, scale=None):
            def wc(k):
                o = CP_OFF["%s%d" % (wname, k)] + ti
                return CP[:, o:o + 1]
            bo = CP_OFF[bname] + ti
            kb.ts(acc.v(), x.v(), wc(2), CP[:, bo:bo + 1], op0=ALU.mult, op1=ALU.add)
            for k in (0, 1, 3, 4):
                off = k - 2
                for (a, e_) in SEGS:
                    lo = max(a, a - off); hi = min(e_, e_ - off)
                    kb.stt(acc[:, lo:hi], x[:, lo + off:hi + off], wc(k), acc[:, lo:hi], ALU.mult, ALU.add)
            kb.act(acc.v(), acc.v(), AF.Silu)
            if scale is not None:
                kb.ts(acc.v(), acc.v(), scale, None, op0=ALU.mult)

        def phase_mlstm(b, l):
            kb.release(0)
            HACC = kb.A([128, 18, 512])
            m0 = kb.aoff
            xin = kb.A([128, T])
            QM = [[kb.A([128, T]) for _ in range(2)] for _ in range(2)]
            KT = [kb.A([128, T]) for _ in range(2)]
            V1 = kb.A([128, 18, 4 * 129])
            GI = kb.A([128, 18, 8]); GF = kb.A([128, 18, 8]); GR = kb.A([128, 18, 16]); GB = kb.A([128, 16])
            CnS = [kb.A([128, 129]) for _ in range(4)]
            rhsF = kb.A([128, 4, 128]); Dm = kb.A([128, 4, 128]); Sc = kb.A([128, 4, 128]); TOTt = kb.A([128, 4, 129])
            KW = kb.A([128, 4, 128]); KTOK = kb.A([128, 256]); EX = kb.A([128, 12]); T12 = kb.A([128, 12])
            dn = kb.A([128, 4]); Hc = kb.A([128, 4, 128])
            acc = kb.A([128, T])
            for ti in range(4):
                kb.dma(xin.v(), UF[C_MLQK + ti * 128:C_MLQK + (ti + 1) * 128, :])
                if ti < 2:
                    conv_silu(xin, acc, "mlcw", "mlcb", ti)
                    for hh in range(2):
                        kb.ts(QM[ti][hh].v(), acc.v(), blk[:, hh * 64:hh * 64 + 1], None, op0=ALU.mult)
                else:
                    conv_silu(xin, KT[ti - 2], "mlcw", "mlcb", ti, scale=0.125)
            V14 = V1.v().re("p c (h x) -> p c h x", h=4)
            kb.memset(V1.v(), 1.0)
            for c in range(18):
                kb.dma(V14[:, c, :, 0:128], VML[c * 128:(c + 1) * 128, :].re("p (h v) -> p h v", h=4), q=("sp" if c % 2 else "pool"))
            kb.dma(GR.v(), SMALL[:, 0:16].re("(c p) g -> p c g", p=128), allow_slow_non_contiguous=True)
            kb.dma(GB.v(), V(rowp, rowp.t[l, RP_MLGB:RP_MLGB + 16].partition_broadcast(128)))
            kb.tt(GR.v(), GR.v(), bcmid(GB.v(), 18), ALU.add)
            GR4 = GR.v().re("p c (d g h) -> p c d g h", d=2, g=2)
            for d in range(2):
                kb.copy(GI[:, :, d * 4:(d + 1) * 4], GR4[:, :, d, 0, :])
                kb.act(GF[:, :, d * 4:(d + 1) * 4], GR4[:, :, d, 1, :], AF.Sigmoid)
            kb.act(GF.v(), GF.v(), AF.Ln)
            for d in range(2):
                tri, smat = DIRC[d]
                for h in range(4):
                    kb.memset(CnS[h].v(), 0.0)
                for c in CHUNK_ORDER[d]:
                    cs = slice(c * 128, (c + 1) * 128)
                    gi = GI[:, c, d * 4:(d + 1) * 4]; gf = GF[:, c, d * 4:(d + 1) * 4]
                    kb.tt(rhsF.v(), bcmid(cview(tri), 4), bc3(gf, 128), ALU.mult)
                    pSeg = nextps()
                    kb.mm(pSeg[:, 0:512], cview(smat), rhsF.v().re("p h t -> p (h t)"))
                    pB2 = nextps()
                    kb.mm(pB2[:, 0:4], cview(tri), gf)
                    kb.mm(pB2[:, 4:8], cview(smat), gf)
                    kb.mm(pB2[:, 8:12], ones, gf)
                    kb.copy(T12.v(), pB2[:, 0:12])
                    kb.tt(T12[:, 4:8], T12[:, 4:8], gi, ALU.add)
                    kb.act(EX.v(), T12.v(), AF.Exp)
                    kb.tt(Dm.v(), pSeg[:, 0:512].re("p (h t) -> p h t", h=4), bc3(gi, 128), ALU.add)
                    kb.act(Dm.v(), Dm.v(), AF.Exp)
                    kb.tt(Dm.v(), Dm.v(), bcmid(cview(tri), 4), ALU.mult)
                    pQK = nextps()
                    for h in range(4):
                        kb.mm(pQK[:, h * 128:(h + 1) * 128], KT[h // 2][:, cs], QM[h // 2][h % 2][:, cs])
                    kb.tt(Sc.v(), pQK[:, 0:512].re("p (h t) -> p h t", h=4), Dm.v(), ALU.mult)
                    pI = nextps(); pE = nextps()
                    for h in range(4):
                        o = (h // 2) * 512 + (h % 2) * 129
                        kb.mm(pI[:, o:o + 129], Sc[:, h, :], V14[:, c, h, :])
                        kb.mm(pE[:, o:o + 129], QM[h // 2][h % 2][:, cs], CnS[h].v())
                    for h in range(4):
                        o = (h // 2) * 512 + (h % 2) * 129
                        kb.ts(TOTt[:, h, :], pE[:, o:o + 129], EX[:, h:h + 1], None, op0=ALU.mult)
                    for half in range(2):
                        kb.tt(TOTt[:, 2 * half:2 * half + 2, :], TOTt[:, 2 * half:2 * half + 2, :],
                              pI[:, half * 512:half * 512 + 258].re("p (h x) -> p h x", h=2), ALU.add)
                    kb.act(dn.v(), TOTt[:, :, 128], AF.Abs)
                    kb.ts(dn.v(), dn.v(), 1.0, None, op0=ALU.max)
                    kb.recip(dn.v(), dn.v())
                    hv = HACC[:, c, :].re("p (h v) -> p h v", h=4)
                    if d == 0:
                        kb.tt(hv, TOTt[:, :, 0:128], bc3(dn.v(), 128), ALU.mult)
                    else:
                        kb.tt(Hc.v(), TOTt[:, :, 0:128], bc3(dn.v(), 128), ALU.mult)
                        kb.tt(hv, hv, Hc.v(), ALU.add)
                    pT = nextps()
                    kb.tr(pT[:, 0:128], KT[0][:, cs], ident)
                    kb.tr(pT[:, 128:256], KT[1][:, cs], ident)
                    evac(KTOK.v(), pT[:, 0:256])
                    pSt = nextps()
                    for h in range(4):
                        o = (h // 2) * 512 + (h % 2) * 129
                        kb.ts(KW[:, h, :], KTOK[:, (h // 2) * 128:(h // 2 + 1) * 128], EX[:, 4 + h:5 + h], None, op0=ALU.mult)
                        kb.mm(pSt[:, o:o + 129], KW[:, h, :], V14[:, c, h, :])
                    for h in range(4):
                        o = (h // 2) * 512 + (h % 2) * 129
                        kb.stt(CnS[h].v(), CnS[h].v(), EX[:, 8 + h:9 + h], pSt[:, o:o + 129], ALU.mult, ALU.add)
            kb.release(m0)
            YR = [kb.A([128, T]) for _ in range(4)]; SO = [kb.A([128, T]) for _ in range(4)]
            sq = kb.A([128, 4, 128]); ssq = kb.A([128, 4]); hn = kb.A([128, 4, 128])
            for h in range(4):
                kb.dma(SO[h].v(), UF[C_MLO + h * 128:C_MLO + (h + 1) * 128, :], q=("sp" if h % 2 else "pool"))
            for c in range(18):
                cs = slice(c * 128, (c + 1) * 128)
                hv = HACC[:, c, :].re("p (h v) -> p h v", h=4)
                kb.act(sq.v(), hv, AF.Square)
                kb.reduce(ssq.v(), sq.v(), ALU.add)
                kb.ts(ssq.v(), ssq.v(), 1.0 / 128, EPS, op0=ALU.mult, op1=ALU.add)
                kb.act(ssq.v(), ssq.v(), AF.Sqrt); kb.recip(ssq.v(), ssq.v())
                kb.tt(hn.v(), hv, bc3(ssq.v(), 128), ALU.mult)
                pT = nextps()
                for h in range(4):
                    kb.tr(pT[:, h * 128:(h + 1) * 128], hn[:, h, :], ident)
                for h in range(4):
                    o = CP_OFF["mlng"] + h
                    kb.stt(YR[h][:, cs], pT[:, h * 128:(h + 1) * 128], CP[:, o:o + 1], SO[h][:, cs], ALU.mult, ALU.mult)
            for h in range(4):
                kb.dma(YB[h * 128:(h + 1) * 128, :], YR[h].v())

        PHASES["mlstm"] = phase_mlstm

        XST = scratch("XST", [T, 1024]); YS0 = scratch("YS0", [T, 1024])

        def phase_ssd(b, l):
            kb.release(0)
            BT = [kb.A([128, T]) for _ in range(4)]; CT = [kb.A([128, T]) for _ in range(4)]
            m0 = kb.aoff
            xin = kb.A([128, T]); acc = kb.A([128, T]); XO = [kb.A([128, 4, 128]) for _ in range(2)]
            for ti in range(16):
                kb.dma(xin.v(), UF[C_XBC + ti * 128:C_XBC + (ti + 1) * 128, :], q=("sp" if ti % 2 else "pool"))
                if ti < 8:
                    conv_silu(xin, acc, "mbcw", "mbcb", ti)
                    for c0 in range(0, 18, 4):
                        n = min(4, 18 - c0)
                        pT = nextps()
                        for j in range(n):
                            kb.tr(pT[:, j * 128:(j + 1) * 128], acc[:, (c0 + j) * 128:(c0 + j + 1) * 128], ident)
                        xo = XO[(c0 // 4) % 2]
                        evac(xo[:, 0:n, :], pT[:, 0:n * 128].re("p (c f) -> p c f", c=n))
                        kb.dma(XST[c0 * 128:(c0 + n) * 128, ti * 128:(ti + 1) * 128].re("(c p) f -> p c f", p=128), xo[:, 0:n, :])
                elif ti < 12:
                    conv_silu(xin, BT[ti - 8], "mbcw", "mbcb", ti)
                else:
                    conv_silu(xin, CT[ti - 12], "mbcw", "mbcb", ti)
            kb.release(m0)
            DTt = kb.A([128, 18, 32]); DA = kb.A([128, 18, 32]); RB_ = kb.A([128, 32]); AN = kb.A([128, 32])
            DROW = kb.A([128, 1024])
            rhsF = kb.A([128, 16, 128]); Dm = kb.A([128, 16, 128]); Mt = kb.A([128, 16, 128]); CBs = kb.A([128, 4, 128])
            XC = kb.A([128, 16, 64]); XD = kb.A([128, 16, 64]); XW = kb.A([128, 16, 64]); Yc = kb.A([128, 16, 64])
            Y0 = kb.A([128, 16, 64]); HST = kb.A([128, 16, 64]); BK = kb.A([128, 4, 128]); tmp = kb.A([128, 1024])
            ZC = kb.A([128, 1024]); YO = kb.A([128, 8, 128]); EX = kb.A([128, 48]); ssq = kb.A([128, 4])
            kb.dma(DTt.v(), SMALL[:, 16:48].re("(c p) g -> p c g", p=128), allow_slow_non_contiguous=True)
            kb.dma(RB_.v(), V(rowp, rowp.t[l, RP_DTB:RP_DTB + 32].partition_broadcast(128)))
            kb.dma(AN.v(), V(rowp, rowp.t[l, RP_ALOG:RP_ALOG + 32].partition_broadcast(128)))
            kb.dma(DROW.v(), V(rowp, rowp.t[l, RP_MBD:RP_MBD + 1024].partition_broadcast(128)))
            kb.tt(DTt.v(), DTt.v(), bcmid(RB_.v(), 18), ALU.add)
            kb.act(DTt.v(), DTt.v(), AF.Exp)
            kb.act(DTt.v(), DTt.v(), AF.Ln, bias=1.0)
            kb.act(AN.v(), AN.v(), AF.Exp)
            kb.ts(AN.v(), AN.v(), -1.0, None, op0=ALU.mult)
            kb.tt(DA.v(), DTt.v(), bcmid(AN.v(), 18), ALU.mult)
            for d in range(2):
                tri, smat = DIRC[d]
                kb.memset(HST.v(), 0.0)
                for c in CHUNK_ORDER[d]:
                    cs = slice(c * 128, (c + 1) * 128)
                    da = DA[:, c, d * 16:(d + 1) * 16]; dt = DTt[:, c, d * 16:(d + 1) * 16]
                    kb.dma(XC.v().re("p h x -> p (h x)"), XST[c * 128:(c + 1) * 128, :])
                    kb.tt(rhsF.v(), bcmid(cview(tri), 16), bc3(da, 128), ALU.mult)
                    pS = [nextps(), nextps()]
                    for q in range(4):
                        kb.mm(pS[q // 2][:, (q % 2) * 512:(q % 2 + 1) * 512], cview(smat),
                              rhsF[:, q * 4:(q + 1) * 4, :].re("p h t -> p (h t)"))
                    pB2 = nextps()
                    kb.mm(pB2[:, 0:16], cview(tri), da)
                    kb.mm(pB2[:, 16:32], cview(smat), da)
                    kb.mm(pB2[:, 32:48], ones, da)
                    kb.act(EX.v(), pB2[:, 0:48], AF.Exp)
                    for half in range(2):
                        kb.act(Dm[:, half * 8:(half + 1) * 8, :], pS[half].v().re("p (h t) -> p h t", h=8), AF.Exp)
                    kb.tt(Dm.v(), Dm.v(), bcmid(cview(tri), 16), ALU.mult)
                    pCB = nextps()
                    for g in range(4):
                        kb.mm(pCB[:, g * 128:(g + 1) * 128], BT[g][:, cs], CT[g][:, cs])
                    evac(CBs.v(), pCB[:, 0:512].re("p (g t) -> p g t", g=4))
                    cb4 = V(CBs, CBs.t.rearrange("p g (o t) -> p g o t", o=1).to_broadcast([128, 4, 4, 128]))
                    kb.tt(Mt.v().re("p (g j) t -> p g j t", g=4), Dm.v().re("p (g j) t -> p g j t", g=4), cb4, ALU.mult)
                    kb.tt(XD.v(), XC.v(), bc3(dt, 64), ALU.mult)
                    pY = nextps(); pE = nextps()
                    for hd in range(16):
                        kb.mm(pY[:, hd * 64:(hd + 1) * 64], Mt[:, hd, :], XD[:, hd, :])
                    for hd in range(16):
                        kb.mm(pE[:, hd * 64:(hd + 1) * 64], CT[hd // 4][:, cs], HST[:, hd, :])
                    kb.tt(Yc.v(), pE.v().re("p (h x) -> p h x", h=16), bc3(EX[:, 0:16], 64), ALU.mult)
                    kb.tt(Yc.v(), Yc.v(), pY.v().re("p (h x) -> p h x", h=16), ALU.add)
                    pT = nextps()
                    for g in range(4):
                        kb.tr(pT[:, g * 128:(g + 1) * 128], BT[g][:, cs], ident)
                    evac(BK.v(), pT[:, 0:512].re("p (g t) -> p g t", g=4))
                    kb.tt(XW.v(), XD.v(), bc3(EX[:, 16:32], 64), ALU.mult)
                    pH = nextps()
                    for hd in range(16):
                        kb.mm(pH[:, hd * 64:(hd + 1) * 64], BK[:, hd // 4, :], XW[:, hd, :])
                    kb.tt(HST.v(), HST.v(), bc3(EX[:, 32:48], 64), ALU.mult)
                    kb.tt(HST.v(), HST.v(), pH.v().re("p (h x) -> p h x", h=16), ALU.add)
                    Ycf = Yc.v().re("p h x -> p (h x)")
                    if d == 0:
                        kb.dma(YS0[c * 128:(c + 1) * 128, :], Ycf, q="pool")
                        continue
                    kb.dma(Y0.v().re("p h x -> p (h x)"), YS0[c * 128:(c + 1) * 128, :], q="pool")
                    kb.dma(ZC.v(), ZT[c * 128:(c + 1) * 128, :], q="pool")
                    kb.tt(Yc.v(), Yc.v(), Y0.v(), ALU.add)
                    kb.tt(tmp.v(), XC.v().re("p h x -> p (h x)"), DROW.v(), ALU.mult)
                    kb.tt(Ycf, Ycf, tmp.v(), ALU.add)
                    kb.tt(Ycf, Ycf, ZC.v(), ALU.mult)
                    kb.act(tmp.v(), Ycf, AF.Square)
                    kb.reduce(ssq.v(), tmp.v().re("p (g x) -> p g x", g=4), ALU.add)
                    kb.ts(ssq.v(), ssq.v(), 1.0 / 256, EPS, op0=ALU.mult, op1=ALU.add)
                    kb.act(ssq.v(), ssq.v(), AF.Sqrt); kb.recip(ssq.v(), ssq.v())
                    y4 = Yc.v().re("p (g j) x -> p g (j x)", g=4)
                    kb.tt(y4, y4, bc3(ssq.v(), 256), ALU.mult)
                    pTs = [nextps(), nextps()]
                    for kt in range(8):
                        kb.tr(pTs[kt // 4][:, (kt % 4) * 128:(kt % 4 + 1) * 128], Ycf[:, kt * 128:(kt + 1) * 128], ident)
                    for kt in range(8):
                        o = CP_OFF["mbng"] + kt
                        kb.ts(YO[:, kt, :], pTs[kt // 4][:, (kt % 4) * 128:(kt % 4 + 1) * 128], CP[:, o:o + 1], None, op0=ALU.mult)
                    kb.dma(YC.v().re("(k p) t -> p k t", p=128)[:, :, cs], YO.v())

        PHASES["ssd"] = phase_ssd

        def phase_merge(b, l):
            kb.release(0)
            PA = kb.A([128, 4, 1024]); PB = kb.A([128, 4, 1024]); PC = kb.A([128, 8, 1024]); WO = kb.A([128, 8, 1024])
            kb.dma(PA.v(), p_a[l].re("(k p) n -> p k n", p=128)); kb.dma(PB.v(), p_b[l].re("(k p) n -> p k n", p=128), q="pool")
            kb.dma(PC.v(), p_c[l].re("(k p) n -> p k n", p=128)); kb.dma(WO.v(), w_out[l].re("(k p) n -> p k n", p=128), q="pool")
            G1 = [kb.A([128, 1024]) for _ in range(2)]
            for which in range(2):
                kb.dma(G1[which].v(), V(MOD, MOD.t[which, 2048:3072].partition_broadcast(128)))
            YAg = kb.A([128, 4, 512]); YBg = kb.A([128, 4, 512]); YCg = kb.A([128, 8, 512])
            GT = kb.A([128, 3, 512]); mT = kb.A([128, 8, 512]); tmp = kb.A([128, 512])
            xt = kb.A([128, 1024]); ol = kb.A([128, 1024])
            for (t0, tw) in GROUPS:
                kb.dma(YAg[:, :, 0:tw], YA.v().re("(k p) t -> p k t", p=128)[:, :, t0:t0 + tw])
                kb.dma(YBg[:, :, 0:tw], YB.v().re("(k p) t -> p k t", p=128)[:, :, t0:t0 + tw], q="pool")
                kb.dma(YCg[:, :, 0:tw], YC.v().re("(k p) t -> p k t", p=128)[:, :, t0:t0 + tw])
                for dt_ in range(8):
                    ds_ = slice(dt_ * 128, (dt_ + 1) * 128)
                    for br in range(3):
                        r0 = C_G + br * 1024 + dt_ * 128
                        kb.dma(GT[:, br, 0:tw], UF[r0:r0 + 128, t0:t0 + tw], q=("pool" if br % 2 else "sp"))
                    for br, (Wt, Yg, nk) in enumerate([(PA, YAg, 4), (PB, YBg, 4), (PC, YCg, 8)]):
                        ps = nextps()[:, 0:tw]
                        for k in range(nk):
                            kb.mm(ps, Wt[:, k, ds_], Yg[:, k, 0:tw], start=(k == 0), stop=(k == nk - 1))
                        if br == 0:
                            kb.tt(mT[:, dt_, 0:tw], ps, GT[:, br, 0:tw], ALU.mult)
                        else:
                            kb.tt(tmp[:, 0:tw], ps, GT[:, br, 0:tw], ALU.mult)
                            kb.tt(mT[:, dt_, 0:tw], mT[:, dt_, 0:tw], tmp[:, 0:tw], ALU.add)
                for tile in range(tw // 128):
                    i = t0 // 128 + tile
                    load_x(b, l, i, xt)
                    for half in range(2):
                        ps = nextps()[:, 0:512]
                        for k in range(8):
                            kb.mm(ps, mT[:, k, tile * 128:(tile + 1) * 128], WO[:, k, half * 512:(half + 1) * 512],
                                  start=(k == 0), stop=(k == 7))
                        kb.tt(ol[:, half * 512:(half + 1) * 512], ps, G1[1 if i < 2 else 0][:, half * 512:(half + 1) * 512], ALU.mult)
                    kb.tt(xt.v(), xt.v(), ol.v(), ALU.add)
                    store_x(b, l, i, xt)

        PHASES["merge"] = phase_merge

        def phase_moe(b, l):
            kb.release(0)
            G2 = [kb.A([128, 1024]) for _ in range(2)]
            for which in range(2):
                kb.dma(G2[which].v(), V(MOD, MOD.t[which, 5120:6144].partition_broadcast(128)))
            mA = kb.aoff
            gs = {}
            for which in (0, 1):
                gs[which] = mod_tiles(l, which, 4096, 3072, RP_N2)
            xts = [kb.A([128, D]) for _ in range(2)]; hs = [kb.A([128, D]) for _ in range(2)]
            junk = kb.A([128, D]); ssq = kb.A([128, 1]); rstd = kb.A([128, 1])
            hTt = kb.A([128, 8, 128]); RW = kb.A([128, 8, 16]); AFFT = kb.A([16, T])
            lg = kb.A([128, 16]); mx = kb.A([128, 1]); sm = kb.A([128, 1])
            kb.dma(RW.v(), router_w[l].re("(k p) e -> p k e", p=128), allow_slow_non_contiguous=True)
            for i in range(NT):
                xt = xts[i % 2]; h = hs[i % 2]
                kb.dma(xt.v(), XS[b, i * 128:(i + 1) * 128, :])
                gsc, sh = gs[1 if i < 2 else 0]
                rmsnorm(xt.v(), h.v(), gsc.v(), sh.v(), junk.v(), ssq.v(), rstd.v())
                kb.dma(H2[i * 128:(i + 1) * 128, :], h.v(), q="pool")
                transpose_tile(h, lambda k0: hTt[:, k0:k0 + 4, :])
                ps = nextps()
                for k in range(8):
                    kb.mm(ps[:, 0:16], hTt[:, k, :], RW[:, k, :], start=(k == 0), stop=(k == 7))
                kb.reduce(mx.v(), ps[:, 0:16], ALU.max)
                kb.ts(mx.v(), mx.v(), -1.0, None, op0=ALU.mult)
                kb.act(lg.v(), ps[:, 0:16], AF.Exp, bias=mx.v(), accum=sm.v())
                kb.recip(sm.v(), sm.v())
                kb.ts(lg.v(), lg.v(), sm.v(), None, op0=ALU.mult)
                ps2 = nextps()
                kb.tr(ps2[0:16, 0:128], lg.v(), ident)
                evac(AFFT[:, i * 128:(i + 1) * 128], ps2[0:16, 0:128])
            kb.dma(AFS.v(), AFFT.v())
            for (tile0, ntile, cap) in [(0, 2, 32), (2, 16, 256)]:
                kb.release(mA)
                n = ntile * 128
                cw = min(cap, 128); nct = cap // cw
                SELG_T = kb.A([128, ntile, 16]); RANK_T = kb.A([128, ntile, 16]); MASK_T = kb.A([128, ntile, 16])
                mB = kb.aoff
                work = kb.A([16, n]); selg = kb.A([16, n]); mask = kb.A([16, n]); rank = kb.A([16, n]); zer = kb.A([16, n])
                mx8 = kb.A([16, 8])
                kb.dma(work.v(), AFS[0:16, tile0 * 128:tile0 * 128 + n])
                kb.dma(selg.v(), AFS[0:16, tile0 * 128:tile0 * 128 + n], q="pool")
                for it in range(cap // 8):
                    kb.op("dve", lambda: nc.vector.max(out=mx8.t[:], in_=work.t[:]), [work.v()], [mx8.v()])
                    kb.op("dve", lambda: nc.vector.match_replace(out=work.t[:], in_to_replace=mx8.t[:], in_values=work.t[:], imm_value=0.0),
                          [work.v(), mx8.v()], [work.v()])
                kb.tt(selg.v(), selg.v(), work.v(), ALU.subtract)
                kb.ts(mask.v(), selg.v(), 0.0, None, op0=ALU.is_gt)
                kb.memset(zer.v(), 0.0)
                kb.scan(rank.v(), mask.v(), zer.v(), 0.0, ALU.add, ALU.add)
                for i in range(ntile):
                    ps = nextps()
                    kb.tr(ps[:, 0:16], selg[:, i * 128:(i + 1) * 128], ident[0:16, 0:16])
                    kb.tr(ps[:, 16:32], rank[:, i * 128:(i + 1) * 128], ident[0:16, 0:16])
                    kb.copy(SELG_T[:, i, :], ps[:, 0:16])
                    kb.copy(RANK_T[:, i, :], ps[:, 16:32])
                kb.ts(MASK_T.v(), SELG_T.v(), 0.0, None, op0=ALU.is_gt)
                kb.release(mB)
                OUT = kb.A([128, ntile, 1024]); SelE = kb.A([128, ntile, cap]); SelGT = kb.A([128, nct, n])
                H2T = [kb.A([128, 1024]) for _ in range(2)]; W = [kb.A([128, 8, 512]) for _ in range(2)]
                xeT = kb.A([128, 8, cap]); actT = kb.A([128, 8, cap]); sa = kb.A([128, cap]); ye = kb.A([128, nct, 1024])
                SG_ = [kb.A([128, cap]) for _ in range(2)]; xt = H2T[0]
                iota = CST[:, CS_IOTA:CS_IOTA + cap]
                wcnt = {"w": 0}

                def load_w(src):
                    wb = W[wcnt["w"] % 2]
                    q = "sp" if wcnt["w"] % 2 else "pool"
                    wcnt["w"] += 1
                    kb.dma(wb.v(), src.re("(k p) n -> p k n", p=128), q=q)
                    return wb

                for e in range(16):
                    for i in range(ntile):
                        kb.ts(SelE[:, i, :], iota, RANK_T[:, i, e:e + 1], MASK_T[:, i, e:e + 1], op0=ALU.is_equal, op1=ALU.mult)
                        sg = SG_[i % 2]
                        kb.ts(sg.v(), iota, RANK_T[:, i, e:e + 1], SELG_T[:, i, e:e + 1], op0=ALU.is_equal, op1=ALU.mult)
                        ps = nextps()
                        for ct in range(nct):
                            kb.tr(ps[0:cw, ct * 128:(ct + 1) * 128], sg[:, ct * cw:(ct + 1) * cw], ident)
                        evac(SelGT[0:cw, :, i * 128:(i + 1) * 128], ps[0:cw, 0:nct * 128].re("p (c t) -> p c t", c=nct))
                    for pas in range(2):
                        for i in range(ntile):
                            h2t = H2T[i % 2]
                            kb.dma(h2t.v(), H2[(tile0 + i) * 128:(tile0 + i + 1) * 128, :], q=("sp" if i % 2 else "pool"))
                            for kk in range(4):
                                k = pas * 4 + kk
                                pg = PS[kk // 2][:, (kk % 2) * 512:(kk % 2) * 512 + cap]
                                kb.mm(pg, h2t[:, k * 128:(k + 1) * 128], SelE[:, i, :], start=(i == 0), stop=(i == ntile - 1))
                        for kk in range(4):
                            k = pas * 4 + kk
                            evac(xeT[:, k, :], PS[kk // 2][:, (kk % 2) * 512:(kk % 2) * 512 + cap])
                    for half in range(2):
                        wg = load_w(e_wg[l, e, :, half * 512:(half + 1) * 512])
                        wu = load_w(e_wu[l, e, :, half * 512:(half + 1) * 512])
                        for ft in range(4):
                            p1 = nextps()[:, 0:cap]; p2 = nextps()[:, 0:cap]
                            for k in range(8):
                                kb.mm(p1, wg[:, k, ft * 128:(ft + 1) * 128], xeT[:, k, :], start=(k == 0), stop=(k == 7))
                            for k in range(8):
                                kb.mm(p2, wu[:, k, ft * 128:(ft + 1) * 128], xeT[:, k, :], start=(k == 0), stop=(k == 7))
                            kb.act(sa.v(), p1, AF.Silu)
                            kb.tt(actT[:, half * 4 + ft, :], sa.v(), p2, ALU.mult)
                    for ch in range(2):
                        wd = load_w(e_wd[l, e, :, ch * 512:(ch + 1) * 512])
                        for ct in range(nct):
                            ps = nextps()
                            for k in range(8):
                                kb.mm(ps[0:cw, 0:512], actT[:, k, ct * cw:(ct + 1) * cw], wd[:, k, :], start=(k == 0), stop=(k == 7))
                            evac(ye[0:cw, ct, ch * 512:(ch + 1) * 512], ps[0:cw, 0:512])
                    for i in range(ntile):
                        for ch in range(2):
                            ps = nextps()
                            for ct in range(nct):
                                kb.mm(ps[:, 0:512], SelGT[0:cw, ct, i * 128:(i + 1) * 128], ye[0:cw, ct, ch * 512:(ch + 1) * 512],
                                      start=(ct == 0), stop=(ct == nct - 1))
                            o_ = OUT[:, i, ch * 512:(ch + 1) * 512]
                            if e == 0:
                                evac(o_, ps[:, 0:512])
                            else:
                                kb.tt(o_, o_, ps[:, 0:512], ALU.add)
                which = 1 if tile0 == 0 else 0
                for i in range(ntile):
                    rows = XS[b, (tile0 + i) * 128:(tile0 + i + 1) * 128, :]
                    kb.dma(xt.v(), rows)
                    kb.tt(OUT[:, i, :], OUT[:, i, :], G2[which].v(), ALU.mult)
                    kb.tt(xt.v(), xt.v(), OUT[:, i, :], ALU.add)
                    kb.dma(rows, xt.v(), q="pool")

        PHASES["moe"] = phase_moe

        AFS = scratch("AFS", [16, T])

        def phase_final():
            kb.release(0)
            gsc = kb.A([128, D]); xts = [kb.A([128, D]) for _ in range(2)]; hs = [kb.A([128, D]) for _ in range(2)]
            junk = kb.A([128, D]); ssq = kb.A([128, 1]); rstd = kb.A([128, 1])
            kb.dma(gsc.v(), V(final_g, final_g.t[0, :].partition_broadcast(128)))
            for b in range(NB):
                for j in range(16):
                    xt = xts[j % 2]; h = hs[j % 2]
                    kb.dma(xt.v(), XS[b, 256 + j * 128:256 + (j + 1) * 128, :])
                    rmsnorm(xt.v(), h.v(), gsc.v(), None, junk.v(), ssq.v(), rstd.v())
                    kb.dma(out[b, j * 128:(j + 1) * 128, :], h.v(), q="pool")

        for b in range(NB):
            kb.dma(XS[b, 0:256, :], ctx_in[b], q="sp")
            kb.dma(XS[b, 256:T, :], x_in[b], q="pool")
        seq = stop if stop is not None else ["mod", "in", "rwkv", "mlstm", "ssd", "merge", "moe"]
        for b in range(NB):
            for l in range(NL):
                for ph in seq:
                    PHASES[ph](b, l)
        if stop is None:
            phase_final()
        kb.release(0)
        kb.finish([out])
        print("instructions:", kb.nins, flush=True)
    return nc


WNAMES = ["ada_w", "ada_b", "w_in", "rw_w2", "rw_a2", "rw_g2", "p_a", "p_b", "p_c", "w_out", "router_w", "e_wg", "e_wu", "e_wd"]


def make_in_maps(inputs, ncores=8, nb=2, LW=4, EW=16):
    P = {k: np.asarray(v) for k, v in inputs.items()}
    shared = {k: np.ascontiguousarray(P[k][:LW, :EW] if k.startswith("e_w") else P[k][:LW], dtype=np.float32) for k in WNAMES}
    shared["final_g"] = np.ascontiguousarray(P["final_g"].reshape(1, D), dtype=np.float32)
    shared["colp"] = np.stack([host_colpack(P, l) for l in range(LW)])
    shared["rowp"] = np.stack([host_rowpack(P, l) for l in range(LW)])
    shared["cst"] = host_consts()
    shared["cctx"] = np.ascontiguousarray(P["c_ctx"].reshape(8, 128).T, dtype=np.float32)
    maps = []
    for c in range(ncores):
        m = dict(shared)
        sl = slice(c * nb, (c + 1) * nb)
        m["x"] = np.ascontiguousarray(P["x"][sl], dtype=np.float32)
        m["ctx"] = np.ascontiguousarray(P["ctx"][sl], dtype=np.float32)
        m["ccol"] = np.ascontiguousarray(P["c"][sl].reshape(nb, 8, 128).transpose(0, 2, 1), dtype=np.float32)
        maps.append(m)
    return maps


def kernel(**inputs):
    nc = build(NB=2, NL=4)
    maps = make_in_maps(inputs, 8, 2)
    res = run_bass_kernel_spmd(nc, maps, core_ids=list(range(8)))
    return np.concatenate([r["out"] for r in res.results], axis=0).astype(np.float32)
```

```python
import numpy as np
from contextlib import ExitStack
import concourse.bass as bass
import concourse.mybir as mybir
from concourse.bass_utils import run_bass_kernel_spmd

F32 = mybir.dt.float32
F32R = mybir.dt.float32r
BF16 = mybir.dt.bfloat16
AF = mybir.ActivationFunctionType
ALU = mybir.AluOpType
AX = mybir.AxisListType
NDS = 8


class V:
    __slots__ = ("buf", "ap")

    def __init__(s, buf, ap):
        s.buf = buf
        s.ap = ap

    def __getitem__(s, idx):
        return V(s.buf, s.ap[idx])

    def re(s, pat, **kw):
        return V(s.buf, s.ap.rearrange(pat, **kw))

    def bc(s, shape):
        return V(s.buf, s.ap.to_broadcast(list(shape)))

    def pbc(s, n):
        return V(s.buf, s.ap.partition_broadcast(n))

    def r(s):
        return V(s.buf, s.ap.bitcast(F32R))


class Buf:
    __slots__ = ("t", "w", "r", "psum")

    def __init__(s, t, psum=False):
        s.t = t
        s.w = None
        s.r = {}
        s.psum = psum

    def __getitem__(s, idx):
        return V(s, s.t[idx])

    def v(s):
        return V(s, s.t[:])


class KB:
    def __init__(s, nc, es):
        s.nc = nc
        s.es = es
        s.eng = {"pe": nc.tensor, "dve": nc.vector, "act": nc.scalar, "pool": nc.gpsimd, "sp": nc.sync}
        s.semobj = {}
        s.cnt = {}
        for e in ("pe", "dve", "act", "pool"):
            s.semobj[e] = es.enter_context(nc.semaphore("s_" + e))
            s.cnt[e] = 0
        s.dq = ("sp", "pool", "act")
        s.dma_rr = {q: 0 for q in s.dq}
        s.dma_val = {}
        for q in s.dq:
            for i in range(NDS):
                s.semobj[(q, i)] = es.enter_context(nc.semaphore("d_%s%d" % (q, i)))
                s.dma_val[(q, i)] = 0
        s.waited = {e: {} for e in s.eng}
        s.nins = 0

    def sb(s, name, shape, dt=F32):
        return Buf(s.es.enter_context(s.nc.sbuf_tensor(name, list(shape), dt)))

    def ps(s, name, shape, dt=F32):
        return Buf(s.es.enter_context(s.nc.psum_tensor(name, list(shape), dt)), psum=True)

    def dram(s, name, shape, dt=F32, kind="Internal"):
        return Buf(s.nc.dram_tensor(name, list(shape), dt, kind=kind).ap())

    def _waits(s, e, reads, writes, is_dma):
        deps = {}

        def need(ev):
            if ev is None:
                return
            k, val, src = ev
            if src == "pe" and e == "pe" and not is_dma:
                return
            if deps.get(k, 0) < val:
                deps[k] = val

        for x in reads:
            need(x.buf.w)
            if x.buf.psum:
                for k, (val, src) in x.buf.r.items():
                    if src != e:
                        need((k, val, src))
        for x in writes:
            need(x.buf.w)
            for k, (val, src) in x.buf.r.items():
                need((k, val, src))
        w = s.waited[e]
        for k, val in deps.items():
            if w.get(k, 0) < val:
                s.eng[e].wait_ge(s.semobj[k], val)
                w[k] = val
                s.nins += 1

    def _post(s, ev, reads, writes):
        k, val, src = ev
        for x in reads:
            x.buf.r[k] = (val, src)
        for x in writes:
            x.buf.w = ev
            x.buf.r = {}

    def op(s, e, fn, reads, writes):
        s._waits(e, reads, writes, False)
        ins = fn()
        s.cnt[e] += 1
        ins.then_inc(s.semobj[e], 1)
        s.nins += 1
        s._post((e, s.cnt[e], e), reads, writes)

    def dma(s, out, in_, q="sp", **kw):
        s._waits(q, [in_], [out], True)
        slot = s.dma_rr[q]
        s.dma_rr[q] = (slot + 1) % NDS
        k = (q, slot)
        prev = s.dma_val[k]
        if s.waited[q].get(k, 0) < prev:
            s.eng[q].wait_ge(s.semobj[k], prev)
            s.waited[q][k] = prev
        ins = s.eng[q].dma_start(out=out.ap, in_=in_.ap, **kw)
        ins.then_inc(s.semobj[k], 16)
        s.nins += 1
        s.dma_val[k] = prev + 16
        s._post((k, prev + 16, "dma"), [in_], [out])

    def finish(s, bufs):
        for b in bufs:
            ev = b.w
            if ev is not None:
                k, val, _ = ev
                s.eng["sp"].wait_ge(s.semobj[k], val)
        for k, val in s.dma_val.items():
            if val:
                s.eng["sp"].wait_ge(s.semobj[k], val)

    def mm(s, out, lhsT, rhs, start=True, stop=True):
        s.op("pe", lambda: s.nc.tensor.matmul(out.ap, lhsT.ap, rhs.ap, start=start, stop=stop),
             [lhsT, rhs], [out])

    def tr(s, out, in_, ident):
        s.op("pe", lambda: s.nc.tensor.transpose(out.ap, in_.ap, ident.ap), [in_, ident], [out])

    def act(s, out, in_, func, bias=None, scale=1.0, accum=None):
        rd = [in_]
        kw = {}
        if isinstance(bias, V):
            rd.append(bias)
            kw["bias"] = bias.ap
        elif bias is not None:
            kw["bias"] = bias
        if isinstance(scale, V):
            rd.append(scale)
            kw["scale"] = scale.ap
        else:
            kw["scale"] = scale
        wr = [out]
        if accum is not None:
            wr.append(accum)
            kw["accum_out"] = accum.ap
        s.op("act", lambda: s.nc.scalar.activation(out=out.ap, in_=in_.ap, func=func, **kw), rd, wr)

    def tt(s, out, a, b, op, e="dve"):
        s.op(e, lambda: s.eng[e].tensor_tensor(out.ap, a.ap, b.ap, op), [a, b], [out])

    def ts(s, out, a, s1, s2=None, op0=ALU.mult, op1=None, accum=None, e="dve"):
        rd = [a]
        a1 = s1.ap if isinstance(s1, V) else s1
        a2 = s2.ap if isinstance(s2, V) else s2
        if isinstance(s1, V):
            rd.append(s1)
        if isinstance(s2, V):
            rd.append(s2)
        kw = {}
        if op1 is not None:
            kw["op1"] = op1
        wr = [out]
        if accum is not None:
            kw["accum_out"] = accum.ap
            wr.append(accum)
        s.op(e, lambda: s.eng[e].tensor_scalar(out.ap, a.ap, a1, a2, op0, **kw), rd, wr)

    def stt(s, out, a, sc, b, op0, op1, e="dve"):
        rd = [a, b]
        a1 = sc.ap if isinstance(sc, V) else sc
        if isinstance(sc, V):
            rd.append(sc)
        s.op(e, lambda: s.eng[e].scalar_tensor_tensor(out.ap, a.ap, a1, b.ap, op0, op1), rd, [out])

    def copy(s, out, in_, e="dve"):
        if e == "act":
            s.act(out, in_, AF.Copy)
        else:
            s.op(e, lambda: s.eng[e].tensor_copy(out.ap, in_.ap), [in_], [out])

    def memset(s, out, val, e="dve"):
        s.op(e, lambda: s.eng[e].memset(out.ap, val), [], [out])

    def recip(s, out, in_):
        s.op("dve", lambda: s.nc.vector.reciprocal(out.ap, in_.ap), [in_], [out])

    def reduce(s, out, in_, op, axis=AX.X):
        s.op("dve", lambda: s.nc.vector.tensor_reduce(out.ap, in_.ap, axis, op), [in_], [out])


def _kb_barrier(s):
    for e in s.eng:
        w = s.waited[e]
        for k in ("pe", "dve", "act", "pool"):
            if w.get(k, 0) < s.cnt[k]:
                s.eng[e].wait_ge(s.semobj[k], s.cnt[k])
                w[k] = s.cnt[k]
                s.nins += 1
        for k, val in s.dma_val.items():
            if val and w.get(k, 0) < val:
                s.eng[e].wait_ge(s.semobj[k], val)
                w[k] = val
                s.nins += 1


def _kb_init_arena(s, n):
    s.arena = s.es.enter_context(s.nc.sbuf_tensor("arena", [128, n], F32))
    s.aoff = 0
    s.an = n


def _kb_A(s, shape):
    n = int(np.prod(shape[1:]))
    assert s.aoff + n <= s.an, ("arena overflow", s.aoff, n, s.an)
    ap = s.arena[0:shape[0], s.aoff:s.aoff + n]
    s.aoff += n
    if len(shape) == 3:
        ap = ap.rearrange("p (a b) -> p a b", a=shape[1])
    elif len(shape) == 4:
        ap = ap.rearrange("p (a b c) -> p a b c", a=shape[1], b=shape[2])
    return Buf(ap)


def _kb_release(s, mark=0):
    s.barrier()
    s.aoff = mark


def _kb_scan(s, out, d0, d1, init, op0, op1):
    s.op("dve", lambda: s.nc.vector.tensor_tensor_scan(out.ap, d0.ap, d1.ap, init, op0, op1), [d0, d1], [out])


def _kb_Ab(s, shape):
    n = int(np.prod(shape[1:]))
    nf = (n + 1) // 2
    assert s.aoff + nf <= s.an, ("arena overflow", s.aoff, nf, s.an)
    ap = s.arena[0:shape[0], s.aoff:s.aoff + nf].bitcast(BF16)[:, 0:n]
    s.aoff += nf
    if len(shape) == 3:
        ap = ap.rearrange("p (a b) -> p a b", a=shape[1])
    return Buf(ap)


KB.Ab = _kb_Ab
KB.barrier = _kb_barrier
KB.init_arena = _kb_init_arena
KB.A = _kb_A
KB.release = _kb_release
KB.scan = _kb_scan

D = 1024
T = 2304
NT = 18
EPS = 1e-6
GROUPS = [(0, 512), (512, 512), (1024, 512), (1536, 512), (2048, 256)]
SEGS = [(0, 256), (256, 2304)]
C_MLQK, C_MLV, C_MLO, C_MLG, C_Z, C_XBC, C_DT, C_G = 1792, 2304, 2816, 3328, 3344, 4368, 6416, 6448
CHUNK_ORDER = {0: list(range(18)), 1: [1, 0] + list(range(17, 1, -1))}

CP_OFF = {}
_c = 0
for _n, _w in [("mu", 14), ("w0_0", 4), ("w0_1", 4), ("a0_0", 4), ("a0_1", 4), ("kk", 4), ("ka", 4), ("rk", 4),
               ("lnw", 4), ("lnb", 4)] + [("mlcw%d" % k, 4) for k in range(5)] + [("mlcb", 4), ("mlng", 4)] + \
              [("mbcw%d" % k, 16) for k in range(5)] + [("mbcb", 16), ("mbng", 8), ("om", 14), ("hm", 14)]:
    CP_OFF[_n] = _c
    _c += _w
NCP = _c
RP_N1, RP_N2, RP_MLGB, RP_DTB, RP_ALOG, RP_MBD, NRP = 0, 1024, 2048, 2064, 2096, 2128, 3152
CS_ID, CS_UI, CS_US, CS_LI, CS_LS, CS_ONE, CS_BLK, CS_IOTA, CS_RST, NCS = 0, 128, 256, 384, 512, 640, 768, 896, 1152, 1152 + 2304


def host_consts():
    p = np.arange(128)[:, None]
    f = np.arange(128)[None, :]
    c = np.zeros((128, NCS), np.float32)
    c[:, CS_ID:CS_ID + 128] = (p == f)
    c[:, CS_UI:CS_UI + 128] = (p <= f)
    c[:, CS_US:CS_US + 128] = (p < f)
    c[:, CS_LI:CS_LI + 128] = (p >= f)
    c[:, CS_LS:CS_LS + 128] = (p > f)
    c[:, CS_ONE:CS_ONE + 128] = 1.0
    c[:, CS_BLK:CS_BLK + 128] = (p // 64 == f // 64)
    c[:, CS_IOTA:CS_IOTA + 256] = np.arange(1, 257)[None, :]
    rst = np.ones(2304, np.float32)
    rst[::128] = 0.0
    c[:, CS_RST:CS_RST + 2304] = rst[None, :]
    return c


def host_colpack(P, l):
    cp = np.zeros((128, NCP), np.float32)

    def put(name, vec):
        v = np.asarray(vec, np.float32).reshape(-1, 128).T
        cp[:, CP_OFF[name]:CP_OFF[name] + v.shape[1]] = v

    put("mu", P["rw_mu"][l])
    for d in range(2):
        put("w0_%d" % d, P["rw_w0"][l, d])
        put("a0_%d" % d, P["rw_a0"][l, d])
    put("kk", P["rw_kk"][l]); put("ka", P["rw_ka"][l]); put("rk", P["rw_rk"][l])
    put("lnw", P["rw_ln_w"][l]); put("lnb", P["rw_ln_b"][l])
    for k in range(5):
        put("mlcw%d" % k, P["ml_conv_w"][l, k])
        put("mbcw%d" % k, P["mb_conv_w"][l, k])
    put("mlcb", P["ml_conv_b"][l]); put("mlng", P["ml_norm_g"][l])
    put("mbcb", P["mb_conv_b"][l]); put("mbng", P["mb_norm_g"][l])
    return cp


def host_rowpack(P, l):
    r = np.zeros((NRP,), np.float32)
    r[RP_N1:RP_N1 + 1024] = P["norm1_g"][l]
    r[RP_N2:RP_N2 + 1024] = P["norm2_g"][l]
    r[RP_MLGB:RP_MLGB + 16] = P["ml_gate_b"][l]
    r[RP_DTB:RP_DTB + 32] = P["mb_dt_bias"][l].reshape(-1)
    r[RP_ALOG:RP_ALOG + 32] = P["mb_a_log"][l].reshape(-1)
    r[RP_MBD:RP_MBD + 1024] = np.repeat(P["mb_d"][l], 64)
    return r

RW_CUT = 0


def build(NB=2, NL=4, dbg=(), stop=None, LW=4, EW=16):
    nc = bass.Bass("TRN2", target_bir_lowering=False)
    es = ExitStack()
    with es:
        kb = KB(nc, es)

        def din(name, shape):
            return kb.dram(name, shape, kind="ExternalInput")

        def scratch(name, shape):
            return kb.dram(name, shape, kind=("ExternalOutput" if name in dbg else "Internal"))

        x_in = din("x", [NB, 2048, D]); ctx_in = din("ctx", [NB, 256, D])
        ccol = din("ccol", [NB, 128, 8]); cctx = din("cctx", [128, 8])
        ada_w = din("ada_w", [LW, D, 6 * D]); ada_b = din("ada_b", [LW, 6 * D]); w_in = din("w_in", [LW, D, 9520])
        rw_w2 = din("rw_w2", [LW, 2, 64, 512]); rw_a2 = din("rw_a2", [LW, 2, 64, 512]); rw_g2 = din("rw_g2", [LW, 128, 512])
        p_a = din("p_a", [LW, 512, D]); p_b = din("p_b", [LW, 512, D]); p_c = din("p_c", [LW, D, D]); w_out = din("w_out", [LW, D, D])
        router_w = din("router_w", [LW, D, 16])
        e_wg = din("e_wg", [LW, EW, D, D]); e_wu = din("e_wu", [LW, EW, D, D]); e_wd = din("e_wd", [LW, EW, D, D])
        final_g = din("final_g", [1, D]); colp = din("colp", [LW, 128, NCP]); rowp = din("rowp", [LW, NRP])
        cst = din("cst", [128, NCS])
        out = kb.dram("out", [NB, 2048, D], kind="ExternalOutput")
        XS = scratch("XS", [NB, T, D]); MOD = scratch("MOD", [2, 6 * D]); UF = scratch("UF", [9520, T])
        VML = scratch("VML", [T, 512]); ZT = scratch("ZT", [T, 1024]); SMALL = scratch("SMALL", [T, 48])
        GG = scratch("GG", [512, T]); YA = scratch("YA", [512, T]); YB = scratch("YB", [512, T]); YC = scratch("YC", [1024, T])
        H2 = scratch("H2", [T, D])

        PS = [kb.ps("ps%d" % i, [128, 1024]) for i in range(4)]
        CST = kb.sb("CST", [128, NCS]); CP = kb.sb("CP", [128, NCP])
        kb.init_arena(45000)
        kb.dma(CST.v(), cst.v())
        ident = CST[:, CS_ID:CS_ID + 128]
        ones = CST[:, CS_ONE:CS_ONE + 128]
        blk = CST[:, CS_BLK:CS_BLK + 128]
        state = {"ev": 0}

        def castb(dst, src):
            state["ev"] += 1
            if state["ev"] % 2:
                kb.act(dst, src, AF.Copy)
            else:
                kb.copy(dst, src)

        def evac(o, i, func=None):
            if func is not None:
                kb.act(o, i, func)
                return
            state["ev"] += 1
            if state["ev"] % 2:
                kb.act(o, i, AF.Copy)
            else:
                kb.copy(o, i)

        def xrows(b, l, i):
            if i < 2:
                return [(slice(0, 128), XS[b, i * 128:(i + 1) * 128, :])]
            j = i - 2
            if l % 2 == 0:
                return [(slice(0, 128), XS[b, 256 + j * 128:256 + (j + 1) * 128, :])]
            lat = XS[b, 256:T, :].re("(r c) d -> c r d", c=64)
            return [(slice(cc * 32, (cc + 1) * 32), lat[4 * j + cc]) for cc in range(4)]

        def load_x(b, l, i, xt, q="sp"):
            for ps_, src in xrows(b, l, i):
                kb.dma(xt[ps_, :], src, q=q)

        def store_x(b, l, i, xt, q="pool"):
            for ps_, dst in xrows(b, l, i):
                kb.dma(dst, xt[ps_, :], q=q)

        def rmsnorm(xt, h, gsc, sh, junk, ssq, rstd):
            kb.act(junk, xt, AF.Square, accum=ssq)
            kb.ts(rstd, ssq, 1.0 / D, EPS, op0=ALU.mult, op1=ALU.add)
            kb.act(rstd, rstd, AF.Sqrt)
            kb.recip(rstd, rstd)
            kb.stt(h, xt, rstd, gsc, ALU.mult, ALU.mult)
            if sh is not None:
                kb.tt(h, h, sh, ALU.add)

        def transpose_tile(h, dst_fn):
            for half in range(2):
                p = PS[half]
                for kk in range(4):
                    k = half * 4 + kk
                    kb.tr(p[:, kk * 128:(kk + 1) * 128], h[:, k * 128:(k + 1) * 128], ident)
                evac(dst_fn(half * 4), p[:, 0:512].re("p (k n) -> p k n", k=4))

        def phase_mod(b, l):
            kb.release(0)
            cc = kb.A([128, 8, 2]); modsb = kb.A([2, 6 * D]); bias = kb.A([2, 6 * D])
            W = [kb.A([128, 8, 512]) for _ in range(2)]
            kb.dma(cc[:, :, 0], ccol[b], allow_slow_non_contiguous=True)
            kb.dma(cc[:, :, 1], cctx.v(), allow_slow_non_contiguous=True)
            kb.act(cc.v(), cc.v(), AF.Silu)
            kb.dma(bias.v(), V(ada_b, ada_b.t[l, :].partition_broadcast(2)))
            for n in range(12):
                wb = W[n % 2]
                kb.dma(wb.v(), ada_w[l, :, n * 512:(n + 1) * 512].re("(k p) n -> p k n", p=128), q=("sp" if n % 2 else "pool"))
                ps = PS[n % 2][0:2, 0:512]
                for k in range(8):
                    kb.mm(ps, cc[:, k, :], wb[:, k, :], start=(k == 0), stop=(k == 7))
                kb.tt(modsb[:, n * 512:(n + 1) * 512], ps, bias[:, n * 512:(n + 1) * 512], ALU.add)
            kb.dma(MOD.v(), modsb.v())
            kb.dma(CP.v(), colp[l])
            om = CP_OFF["om"]; hm = CP_OFF["hm"]; mu = CP_OFF["mu"]
            kb.ts(CP[:, om:om + 14], CP[:, mu:mu + 14], -1.0, 1.0, op0=ALU.mult, op1=ALU.add)
            kb.ts(CP[:, hm:hm + 14], CP[:, mu:mu + 14], 0.5, None, op0=ALU.mult)

        def mod_tiles(l, which, sc_off, sh_off, g_off):
            gsc = kb.A([128, D]); sh = kb.A([128, D]); grow = kb.A([128, D])
            kb.dma(grow.v(), V(rowp, rowp.t[l, g_off:g_off + D].partition_broadcast(128)))
            kb.dma(gsc.v(), V(MOD, MOD.t[which, sc_off:sc_off + D].partition_broadcast(128)))
            kb.dma(sh.v(), V(MOD, MOD.t[which, sh_off:sh_off + D].partition_broadcast(128)))
            kb.stt(gsc.v(), gsc.v(), 1.0, grow.v(), ALU.add, ALU.mult)
            return gsc, sh

        def phase_in(b, l):
            kb.release(0)
            hT = kb.Ab([128, 8, T])
            m0 = kb.aoff
            gs = {}
            for which in (0, 1):
                gs[which] = mod_tiles(l, which, 1024, 0, RP_N1)
            xts = [kb.A([128, D]) for _ in range(2)]; hs = [kb.A([128, D]) for _ in range(2)]
            junk = kb.A([128, D]); ssq = kb.A([128, 1]); rstd = kb.A([128, 1])
            for i in range(NT):
                xt = xts[i % 2]; h = hs[i % 2]
                load_x(b, l, i, xt)
                gsc, sh = gs[1 if i < 2 else 0]
                rmsnorm(xt.v(), h.v(), gsc.v(), sh.v(), junk.v(), ssq.v(), rstd.v())
                transpose_tile(h, lambda k0: hT[:, k0:k0 + 4, i * 128:(i + 1) * 128])
            kb.release(m0)
            W = [kb.A([128, 8, 512]) for _ in range(2)]; WB = [kb.Ab([128, 8, 512]) for _ in range(2)]
            RB = [kb.A([128, T]) for _ in range(2)]; RS = kb.A([128, T]); RO = [kb.A([128, T]) for _ in range(2)]
            TO = [kb.A([128, 1024]) for _ in range(2)]
            cnt = {"w": 0, "m": 0, "p": 0}

            def load_w(c0, ncol):
                wb = W[cnt["w"] % 2]; wbb = WB[cnt["w"] % 2]
                q = "sp" if cnt["w"] % 2 else "pool"
                cnt["w"] += 1
                kb.dma(wb[:, :, 0:ncol], w_in[l, :, c0:c0 + ncol].re("(k p) n -> p k n", p=128), q=q)
                castb(wbb[:, :, 0:ncol], wb[:, :, 0:ncol])
                return wbb

            def nextps():
                cnt["p"] += 1
                return PS[cnt["p"] % 4]

            for (g0, gn, kind) in [(0, 1792, "rw"), (C_MLQK, 512, "copy"), (C_MLO, 512, "sig"), (C_XBC, 2048, "copy"), (C_G, 3072, "sig")]:
                for c0 in range(g0, g0 + gn, 512):
                    ncol = min(512, g0 + gn - c0)
                    wb = load_w(c0, ncol)
                    for m in range(ncol // 128):
                        row0 = c0 + m * 128
                        rb = RB[cnt["m"] % 2]; ro = RO[cnt["m"] % 2]
                        cnt["m"] += 1
                        dst = rb if kind == "rw" else ro
                        for (t0, tw) in GROUPS:
                            ps = nextps()[:, 0:tw]
                            for k in range(8):
                                kb.mm(ps, wb[:, k, m * 128:(m + 1) * 128], hT[:, k, t0:t0 + tw], start=(k == 0), stop=(k == 7))
                            evac(dst[:, t0:t0 + tw], ps, AF.Sigmoid if kind == "sig" else None)
                        if kind == "rw":
                            ti = row0 // 128
                            for (a, e_) in SEGS:
                                kb.tt(RS[:, a + 1:e_ - 1], rb[:, a:e_ - 2], rb[:, a + 2:e_], ALU.add)
                                kb.copy(RS[:, a:a + 1], rb[:, a + 1:a + 2])
                                kb.copy(RS[:, e_ - 1:e_], rb[:, e_ - 2:e_ - 1])
                            kb.ts(ro.v(), rb.v(), CP[:, CP_OFF["om"] + ti:CP_OFF["om"] + ti + 1], None, op0=ALU.mult)
                            kb.stt(ro.v(), RS.v(), CP[:, CP_OFF["hm"] + ti:CP_OFF["hm"] + ti + 1], ro.v(), ALU.mult, ALU.add)
                            if ti == 12:
                                kb.act(ro[0:64, :], ro[0:64, :], AF.Tanh)
                            if ti == 13:
                                kb.act(ro.v(), ro.v(), AF.Sigmoid)
                        kb.dma(UF[row0:row0 + 128, :], ro.v(), q="sp")
            for (c0, ncol, dstD, func) in [(C_MLV, 512, VML, None), (C_Z, 512, ZT, AF.Silu), (C_Z + 512, 512, ZT, AF.Silu)]:
                wb = load_w(c0, ncol)
                dc0 = (c0 - C_Z) if dstD is ZT else 0
                for i in range(NT):
                    ps = nextps()[:, 0:512]
                    for k in range(8):
                        kb.mm(ps, hT[:, k, i * 128:(i + 1) * 128], wb[:, k, :], start=(k == 0), stop=(k == 7))
                    to = TO[i % 2]
                    evac(to[:, 0:512], ps, func)
                    kb.dma(dstD[i * 128:(i + 1) * 128, dc0:dc0 + 512], to[:, 0:512], q="sp")
            wst = W[cnt["w"] % 2]; wb = WB[cnt["w"] % 2]; cnt["w"] += 1
            kb.dma(wst[:, :, 0:16], w_in[l, :, C_MLG:C_MLG + 16].re("(k p) n -> p k n", p=128))
            kb.dma(wst[:, :, 16:48], w_in[l, :, C_DT:C_DT + 32].re("(k p) n -> p k n", p=128))
            castb(wb[:, :, 0:48], wst[:, :, 0:48])
            for i in range(NT):
                ps = nextps()[:, 0:48]
                for k in range(8):
                    kb.mm(ps, hT[:, k, i * 128:(i + 1) * 128], wb[:, k, 0:48], start=(k == 0), stop=(k == 7))
                to = TO[i % 2]
                evac(to[:, 0:48], ps)
                kb.dma(SMALL[i * 128:(i + 1) * 128, :], to[:, 0:48], q="sp")

        PHASES = {"mod": phase_mod, "in": phase_in}

        MASK4 = [kb.sb("mask4_%d" % d, [128, 2, 4, 128]) for d in range(2)]
        MST2 = [kb.sb("mst2_%d" % d, [128, 2, 128]) for d in range(2)]
        ID2 = kb.sb("id2", [128, 2, 128])
        TRI = {0: (CS_UI, CS_US, CS_LS), 1: (CS_LI, CS_LS, CS_US)}
        for d in range(2):
            inc, stc, stT = TRI[d]
            for h in range(2):
                for q in range(4):
                    src = stc if q < 2 else inc
                    kb.copy(MASK4[d][:, h, q, :], CST[:, src:src + 128])
                kb.copy(MST2[d][:, h, :], CST[:, stT:stT + 128])
        for h in range(2):
            kb.copy(ID2[:, h, :], ident)
        pcnt = {"p": 0}

        def nextps():
            pcnt["p"] += 1
            return PS[pcnt["p"] % 4]

        def bc3(v2, n):
            c = v2.ap.shape[1]
            return V(v2.buf, v2.ap.rearrange("p (c o) -> p c o", o=1).to_broadcast([v2.ap.shape[0], c, n]))

        def phase_rwkv(b, l):
            kb.release(0)
            RST = CST[:, CS_RST:CS_RST + T]
            TWXA = kb.A([128, T]); kb.dma(TWXA.v(), UF[1536:1664, :])
            W2A2 = kb.A([128, 2, 512])
            for d in range(2):
                kb.dma(W2A2[0:64, d, :], rw_w2[l, d]); kb.dma(W2A2[64:128, d, :], rw_a2[l, d])
            m1 = kb.aoff
            SG = kb.A([128, T]); kb.dma(SG.v(), UF[1664:1792, :])
            G2 = kb.A([128, 512]); kb.dma(G2.v(), rw_g2[l])
            ro = kb.A([128, T])
            for m in range(4):
                for (t0, tw) in GROUPS:
                    ps = nextps()[:, 0:tw]
                    kb.mm(ps, G2[:, m * 128:(m + 1) * 128], SG[:, t0:t0 + tw])
                    evac(ro[:, t0:t0 + tw], ps)
                kb.dma(GG[m * 128:(m + 1) * 128, :], ro.v())
            if RW_CUT == 1:
                return
            for pp in range(4 if RW_CUT == 0 else 1):
                kb.release(m1)
                Rr = kb.A([128, T]); Kk = kb.A([128, T]); Vv = kb.A([128, T]); KK = kb.A([128, T])
                TMP = kb.A([128, T]); TMP2 = kb.A([128, T]); VT = kb.A([128, 18, 128]); YACC = kb.A([128, T]); BON = kb.A([128, T])
                LWt = kb.A([128, T]); BB = kb.A([128, T]); KD = kb.A([128, T]); Rt = kb.A([128, T]); KKt = kb.A([128, T])
                BtW = kb.A([128, T]); KDtW = kb.A([128, T])
                CW = TMP; E = TMP2
                TOT = kb.A([128, 18]); ETOT = kb.A([128, 18]); S = kb.A([128, 64]); SP = kb.A([128, 2, 64])
                AM = kb.A([128, 2, 4, 128]); MTa = kb.A([128, 2, 128]); Pm = kb.A([128, 2, 128])
                MB = [(kb.A([128, 2, 128]), kb.A([128, 2, 128])) for _ in range(2)]
                Xn = kb.A([128, 128]); Ut = kb.A([128, 128]); BKt = kb.A([128, 256])
                kb.dma(Rr.v(), UF[pp * 128:(pp + 1) * 128, :])
                kb.dma(Kk.v(), UF[512 + pp * 128:512 + (pp + 1) * 128, :], q="pool")
                kb.dma(Vv.v(), UF[1024 + pp * 128:1024 + (pp + 1) * 128, :])

                def col(name, i=pp):
                    return CP[:, CP_OFF[name] + i:CP_OFF[name] + i + 1]

                kb.ts(KK.v(), Kk.v(), col("kk"), None, op0=ALU.mult)
                kb.act(TMP.v(), KK.v(), AF.Square)
                for (t0, tw) in GROUPS:
                    ps = nextps()[:, 0:tw]
                    kb.mm(ps, blk, TMP[:, t0:t0 + tw])
                    kb.ts(TMP2[:, t0:t0 + tw], ps, 1e-12, None, op0=ALU.add)
                kb.act(TMP2.v(), TMP2.v(), AF.Sqrt); kb.recip(TMP2.v(), TMP2.v())
                kb.tt(KK.v(), KK.v(), TMP2.v(), ALU.mult)
                for c in range(18):
                    ps = nextps()[:, 0:128]
                    kb.tr(ps, Vv[:, c * 128:(c + 1) * 128], ident)
                    evac(VT[:, c, :], ps)
                if RW_CUT == 2:
                    return
                for d in range(2):
                    for (t0, tw) in GROUPS:
                        ps = nextps()[:, 0:tw]
                        kb.mm(ps, W2A2[0:64, d, pp * 128:(pp + 1) * 128], TWXA[0:64, t0:t0 + tw])
                        kb.act(LWt[:, t0:t0 + tw], ps, AF.Sigmoid, bias=col("w0_%d" % d))
                        ps = nextps()[:, 0:tw]
                        kb.mm(ps, W2A2[64:128, d, pp * 128:(pp + 1) * 128], TWXA[64:128, t0:t0 + tw])
                        kb.act(BB[:, t0:t0 + tw], ps, AF.Sigmoid, bias=col("a0_%d" % d))
                    kb.ts(LWt.v(), LWt.v(), -float(np.exp(-0.5)), None, op0=ALU.mult)
                    kb.ts(KD.v(), BB.v(), -1.0, col("ka"), op0=ALU.add, op1=ALU.mult)
                    kb.stt(KD.v(), KD.v(), 1.0, Kk.v(), ALU.add, ALU.mult)
                    if d == 0:
                        kb.stt(BON.v(), KD.v(), col("rk"), Rr.v(), ALU.mult, ALU.mult)
                    else:
                        kb.stt(E.v(), KD.v(), col("rk"), Rr.v(), ALU.mult, ALU.mult)
                        kb.tt(BON.v(), BON.v(), E.v(), ALU.add)
                    kb.tt(BB.v(), BB.v(), KK.v(), ALU.mult)
                    kb.scan(CW.v(), RST, LWt.v(), 0.0, ALU.mult, ALU.add)
                    CW3 = CW.v().re("p (c t) -> p c t", t=128)
                    kb.copy(TOT.v(), CW3[:, :, 127])
                    if d == 1:
                        kb.tt(CW3, bc3(TOT.v(), 128), CW3, ALU.subtract)
                        kb.tt(CW.v(), CW.v(), LWt.v(), ALU.add)
                    kb.act(ETOT.v(), TOT.v(), AF.Exp)
                    kb.act(E.v(), CW.v(), AF.Exp)
                    kb.tt(Rt.v(), Rr.v(), E.v(), ALU.mult)
                    kb.tt(E.v(), CW.v(), LWt.v(), ALU.subtract)
                    kb.act(E.v(), E.v(), AF.Exp)
                    kb.tt(KKt.v(), KK.v(), E.v(), ALU.mult)
                    kb.act(E.v(), CW.v(), AF.Exp, scale=-1.0)
                    kb.tt(BB.v(), BB.v(), E.v(), ALU.mult)
                    kb.tt(KD.v(), KD.v(), E.v(), ALU.mult)
                    r3 = "p (c t) -> p c t"
                    kb.tt(BtW.v().re(r3, t=128), BB.v().re(r3, t=128), bc3(ETOT.v(), 128), ALU.mult)
                    kb.tt(KDtW.v().re(r3, t=128), KD.v().re(r3, t=128), bc3(ETOT.v(), 128), ALU.mult)
                    if RW_CUT == 3:
                        return
                    kb.memset(S.v(), 0.0)
                    kb.memset(SP.v(), 0.0)
                    for c in CHUNK_ORDER[d]:
                        cs = slice(c * 128, (c + 1) * 128)
                        pT = nextps()
                        kb.tr(pT[:, 0:128], BtW[:, cs], ident)
                        kb.tr(pT[:, 128:256], KDtW[:, cs], ident)
                        evac(BKt.v(), pT[:, 0:256])
                        pA = nextps(); pB = nextps()
                        for hh in range(2):
                            bs = slice(hh * 64, hh * 64 + 64)
                            o = hh * 512
                            kb.mm(pA[:, o:o + 128], BB[bs, cs], KKt[bs, cs])
                            kb.mm(pA[:, o + 128:o + 256], KD[bs, cs], KKt[bs, cs])
                            kb.mm(pA[:, o + 256:o + 384], BB[bs, cs], Rt[bs, cs])
                            kb.mm(pA[:, o + 384:o + 512], KD[bs, cs], Rt[bs, cs])
                            kb.mm(pB[:, o:o + 128], KKt[bs, cs], BB[bs, cs])
                        kb.tt(AM.v(), pA.v().re("p (h q t) -> p h q t", h=2, q=4), MASK4[d].v(), ALU.mult)
                        kb.tt(MTa.v(), pB.v().re("p (h x) -> p h x", h=2)[:, :, 0:128], MST2[d].v(), ALU.mult)
                        kb.tt(Pm.v(), ID2.v(), AM[:, :, 0, :], ALU.subtract)
                        if RW_CUT == 4:
                            return
                        Mk = AM[:, :, 0, :]; MkT = MTa.v()
                        for st in range(6):
                            if RW_CUT >= 10 and st >= RW_CUT - 10:
                                return
                            pC = nextps()
                            for hh in range(2):
                                kb.mm(pC[:, hh * 128:(hh + 1) * 128], Mk[:, hh, :], MkT[:, hh, :])
                                if st < 5:
                                    kb.mm(pC[:, 256 + hh * 128:256 + (hh + 1) * 128], MkT[:, hh, :], Mk[:, hh, :])
                            if RW_CUT == 21:
                                return
                            nMkT, nMk = MB[st % 2]
                            evac(nMkT.v(), pC[:, 0:256].re("p (h t) -> p h t", h=2))
                            if RW_CUT == 22:
                                return
                            if st < 5:
                                evac(nMk.v(), pC[:, 256:512].re("p (h t) -> p h t", h=2))
                            if RW_CUT == 9:
                                return
                            pD = nextps()
                            for hh in range(2):
                                kb.mm(pD[:, hh * 128:(hh + 1) * 128], nMkT[:, hh, :], Pm[:, hh, :])
                            kb.tt(Pm.v(), Pm.v(), pD[:, 0:256].re("p (h t) -> p h t", h=2), ALU.add)
                            Mk, MkT = nMk.v(), nMkT.v()
                        if RW_CUT == 5:
                            return
                        pX = nextps()
                        for hh in range(2):
                            hs_ = slice(hh * 64, (hh + 1) * 64)
                            kb.mm(pX[:, hs_], KKt[:, cs], SP[:, hh, :], start=True, stop=False)
                            kb.mm(pX[:, hs_], AM[:, hh, 1, :], VT[:, c, hs_], start=False, stop=True)
                        kb.ts(Xn.v(), pX[:, 0:128], -1.0, None, op0=ALU.mult)
                        pU = nextps()
                        for hh in range(2):
                            hs_ = slice(hh * 64, (hh + 1) * 64)
                            kb.mm(pU[:, hs_], Pm[:, hh, :], Xn[:, hs_])
                        evac(Ut.v(), pU[:, 0:128])
                        pY = nextps()
                        for hh in range(2):
                            bs = slice(hh * 64, hh * 64 + 64); hs_ = slice(hh * 64, (hh + 1) * 64)
                            kb.mm(pY[bs, 0:128], SP[:, hh, :], Rt[:, cs], start=True, stop=False)
                            kb.mm(pY[bs, 0:128], Ut[:, hs_], AM[:, hh, 2, :], start=False, stop=False)
                            kb.mm(pY[bs, 0:128], VT[:, c, hs_], AM[:, hh, 3, :], start=False, stop=True)
                        if d == 0:
                            evac(YACC[:, cs], pY[:, 0:128])
                        else:
                            kb.tt(YACC[:, cs], YACC[:, cs], pY[:, 0:128], ALU.add)
                        if RW_CUT == 6:
                            return
                        pS = nextps()
                        for hh in range(2):
                            bs = slice(hh * 64, hh * 64 + 64); hs_ = slice(hh * 64, (hh + 1) * 64)
                            kb.mm(pS[bs, 0:64], BKt[:, hs_], Ut[:, hs_], start=True, stop=False)
                            kb.mm(pS[bs, 0:64], BKt[:, 128 + hh * 64:128 + (hh + 1) * 64], VT[:, c, hs_], start=False, stop=True)
                        kb.stt(S.v(), S.v(), ETOT[:, c:c + 1], pS[:, 0:64], ALU.mult, ALU.add)
                        for hh in range(2):
                            kb.ts(SP[:, hh, :], S.v(), blk[:, hh * 64:hh * 64 + 1], None, op0=ALU.mult)
                for (t0, tw) in GROUPS:
                    g_ = slice(t0, t0 + tw)
                    ps = nextps()[:, 0:tw]
                    kb.mm(ps, blk, YACC[:, g_])
                    kb.stt(TMP[:, g_], ps, -1.0 / 64, YACC[:, g_], ALU.mult, ALU.add)
                kb.act(TMP2.v(), TMP.v(), AF.Square)
                for (t0, tw) in GROUPS:
                    g_ = slice(t0, t0 + tw)
                    ps = nextps()[:, 0:tw]
                    kb.mm(ps, blk, TMP2[:, g_])
                    kb.ts(LWt[:, g_], ps, 1.0 / 64, 64e-5, op0=ALU.mult, op1=ALU.add)
                kb.act(LWt.v(), LWt.v(), AF.Sqrt); kb.recip(LWt.v(), LWt.v())
                kb.tt(TMP.v(), TMP.v(), LWt.v(), ALU.mult)
                kb.ts(TMP.v(), TMP.v(), col("lnw"), col("lnb"), op0=ALU.mult, op1=ALU.add)
                for (t0, tw) in GROUPS:
                    g_ = slice(t0, t0 + tw)
                    ps = nextps()[:, 0:tw]
                    kb.mm(ps, blk, BON[:, g_])
                    kb.tt(TMP2[:, g_], ps, Vv[:, g_], ALU.mult)
                kb.tt(TMP.v(), TMP.v(), TMP2.v(), ALU.add)
                kb.dma(TMP2.v(), GG[pp * 128:(pp + 1) * 128, :])
                kb.tt(TMP.v(), TMP.v(), TMP2.v(), ALU.mult)
                kb.dma(YA[pp * 128:(pp + 1) * 128, :], TMP.v())

        PHASES["rwkv"] = phase_rwkv

        def bcmid(v2, n):
            t_ = v2.ap.shape[1]
            return V(v2.buf, v2.ap.rearrange("p (o t) -> p o t", o=1).to_broadcast([v2.ap.shape[0], n, t_]))

        def cview(off):
            return CST[:, off:off + 128]

        DIRC = {0: (CS_UI, CS_LS), 1: (CS_LI, CS_US)}

        def conv_silu(x, acc, wname, bname, ti, scale=None):
            def wc(k):
                o = CP_OFF["%s%d" % (wname, k)] + ti
                return CP[:, o:o + 1]
            bo = CP_OFF[bname] + ti
            kb.ts(acc.v(), x.v(), wc(2), CP[:, bo:bo + 1], op0=ALU.mult, op1=ALU.add)
            for k in (0, 1, 3, 4):
                off = k - 2
                for (a, e_) in SEGS:
                    lo = max(a, a - off); hi = min(e_, e_ - off)
                    kb.stt(acc[:, lo:hi], x[:, lo + off:hi + off], wc(k), acc[:, lo:hi], ALU.mult, ALU.add)
            kb.act(acc.v(), acc.v(), AF.Silu)
            if scale is not None:
                kb.ts(acc.v(), acc.v(), scale, None, op0=ALU.mult)

        def phase_mlstm(b, l):
            kb.release(0)
            HACC = kb.A([128, 18, 512])
            m0 = kb.aoff
            xin = kb.A([128, T])
            QM = [[kb.A([128, T]) for _ in range(2)] for _ in range(2)]
            KT = [kb.A([128, T]) for _ in range(2)]
            V1 = kb.A([128, 18, 4 * 129])
            GI = kb.A([128, 18, 8]); GF = kb.A([128, 18, 8]); GR = kb.A([128, 18, 16]); GB = kb.A([128, 16])
            CnS = [kb.A([128, 129]) for _ in range(4)]
            rhsF = kb.A([128, 4, 128]); Dm = kb.A([128, 4, 128]); Sc = kb.A([128, 4, 128]); TOTt = kb.A([128, 4, 129])
            KW = kb.A([128, 4, 128]); KTOK = kb.A([128, 256]); EX = kb.A([128, 12]); T12 = kb.A([128, 12])
            dn = kb.A([128, 4]); Hc = kb.A([128, 4, 128])
            acc = kb.A([128, T])
            for ti in range(4):
                kb.dma(xin.v(), UF[C_MLQK + ti * 128:C_MLQK + (ti + 1) * 128, :])
                if ti < 2:
                    conv_silu(xin, acc, "mlcw", "mlcb", ti)
                    for hh in range(2):
                        kb.ts(QM[ti][hh].v(), acc.v(), blk[:, hh * 64:hh * 64 + 1], None, op0=ALU.mult)
                else:
                    conv_silu(xin, KT[ti - 2], "mlcw", "mlcb", ti, scale=0.125)
            V14 = V1.v().re("p c (h x) -> p c h x", h=4)
            kb.memset(V1.v(), 1.0)
            for c in range(18):
                kb.dma(V14[:, c, :, 0:128], VML[c * 128:(c + 1) * 128, :].re("p (h v) -> p h v", h=4), q=("sp" if c % 2 else "pool"))
            kb.dma(GR.v(), SMALL[:, 0:16].re("(c p) g -> p c g", p=128), allow_slow_non_contiguous=True)
            kb.dma(GB.v(), V(rowp, rowp.t[l, RP_MLGB:RP_MLGB + 16].partition_broadcast(128)))
            kb.tt(GR.v(), GR.v(), bcmid(GB.v(), 18), ALU.add)
            GR4 = GR.v().re("p c (d g h) -> p c d g h", d=2, g=2)
            for d in range(2):
                kb.copy(GI[:, :, d * 4:(d + 1) * 4], GR4[:, :, d, 0, :])
                kb.act(GF[:, :, d * 4:(d + 1) * 4], GR4[:, :, d, 1, :], AF.Sigmoid)
            kb.act(GF.v(), GF.v(), AF.Ln)
            for d in range(2):
                tri, smat = DIRC[d]
                for h in range(4):
                    kb.memset(CnS[h].v(), 0.0)
                for c in CHUNK_ORDER[d]:
                    cs = slice(c * 128, (c + 1) * 128)
                    gi = GI[:, c, d * 4:(d + 1) * 4]; gf = GF[:, c, d * 4:(d + 1) * 4]
                    kb.tt(rhsF.v(), bcmid(cview(tri), 4), bc3(gf, 128), ALU.mult)
                    pSeg = nextps()
                    kb.mm(pSeg[:, 0:512], cview(smat), rhsF.v().re("p h t -> p (h t)"))
                    pB2 = nextps()
                    kb.mm(pB2[:, 0:4], cview(tri), gf)
                    kb.mm(pB2[:, 4:8], cview(smat), gf)
                    kb.mm(pB2[:, 8:12], ones, gf)
                    kb.copy(T12.v(), pB2[:, 0:12])
                    kb.tt(T12[:, 4:8], T12[:, 4:8], gi, ALU.add)
                    kb.act(EX.v(), T12.v(), AF.Exp)
                    kb.tt(Dm.v(), pSeg[:, 0:512].re("p (h t) -> p h t", h=4), bc3(gi, 128), ALU.add)
                    kb.act(Dm.v(), Dm.v(), AF.Exp)
                    kb.tt(Dm.v(), Dm.v(), bcmid(cview(tri), 4), ALU.mult)
                    pQK = nextps()
                    for h in range(4):
                        kb.mm(pQK[:, h * 128:(h + 1) * 128], KT[h // 2][:, cs], QM[h // 2][h % 2][:, cs])
                    kb.tt(Sc.v(), pQK[:, 0:512].re("p (h t) -> p h t", h=4), Dm.v(), ALU.mult)
                    pI = nextps(); pE = nextps()
                    for h in range(4):
                        o = (h // 2) * 512 + (h % 2) * 129
                        kb.mm(pI[:, o:o + 129], Sc[:, h, :], V14[:, c, h, :])
                        kb.mm(pE[:, o:o + 129], QM[h // 2][h % 2][:, cs], CnS[h].v())
                    for h in range(4):
                        o = (h // 2) * 512 + (h % 2) * 129
                        kb.ts(TOTt[:, h, :], pE[:, o:o + 129], EX[:, h:h + 1], None, op0=ALU.mult)
                    for half in range(2):
                        kb.tt(TOTt[:, 2 * half:2 * half + 2, :], TOTt[:, 2 * half:2 * half + 2, :],
                              pI[:, half * 512:half * 512 + 258].re("p (h x) -> p h x", h=2), ALU.add)
                    kb.act(dn.v(), TOTt[:, :, 128], AF.Abs)
                    kb.ts(dn.v(), dn.v(), 1.0, None, op0=ALU.max)
                    kb.recip(dn.v(), dn.v())
                    hv = HACC[:, c, :].re("p (h v) -> p h v", h=4)
                    if d == 0:
                        kb.tt(hv, TOTt[:, :, 0:128], bc3(dn.v(), 128), ALU.mult)
                    else:
                        kb.tt(Hc.v(), TOTt[:, :, 0:128], bc3(dn.v(), 128), ALU.mult)
                        kb.tt(hv, hv, Hc.v(), ALU.add)
                    pT = nextps()
                    kb.tr(pT[:, 0:128], KT[0][:, cs], ident)
                    kb.tr(pT[:, 128:256], KT[1][:, cs], ident)
                    evac(KTOK.v(), pT[:, 0:256])
                    pSt = nextps()
                    for h in range(4):
                        o = (h // 2) * 512 + (h % 2) * 129
                        kb.ts(KW[:, h, :], KTOK[:, (h // 2) * 128:(h // 2 + 1) * 128], EX[:, 4 + h:5 + h], None, op0=ALU.mult)
                        kb.mm(pSt[:, o:o + 129], KW[:, h, :], V14[:, c, h, :])
                    for h in range(4):
                        o = (h // 2) * 512 + (h % 2) * 129
                        kb.stt(CnS[h].v(), CnS[h].v(), EX[:, 8 + h:9 + h], pSt[:, o:o + 129], ALU.mult, ALU.add)
            kb.release(m0)
            YR = [kb.A([128, T]) for _ in range(4)]; SO = [kb.A([128, T]) for _ in range(4)]
            sq = kb.A([128, 4, 128]); ssq = kb.A([128, 4]); hn = kb.A([128, 4, 128])
            for h in range(4):
                kb.dma(SO[h].v(), UF[C_MLO + h * 128:C_MLO + (h + 1) * 128, :], q=("sp" if h % 2 else "pool"))
            for c in range(18):
                cs = slice(c * 128, (c + 1) * 128)
                hv = HACC[:, c, :].re("p (h v) -> p h v", h=4)
                kb.act(sq.v(), hv, AF.Square)
                kb.reduce(ssq.v(), sq.v(), ALU.add)
                kb.ts(ssq.v(), ssq.v(), 1.0 / 128, EPS, op0=ALU.mult, op1=ALU.add)
                kb.act(ssq.v(), ssq.v(), AF.Sqrt); kb.recip(ssq.v(), ssq.v())
                kb.tt(hn.v(), hv, bc3(ssq.v(), 128), ALU.mult)
                pT = nextps()
                for h in range(4):
                    kb.tr(pT[:, h * 128:(h + 1) * 128], hn[:, h, :], ident)
                for h in range(4):
                    o = CP_OFF["mlng"] + h
                    kb.stt(YR[h][:, cs], pT[:, h * 128:(h + 1) * 128], CP[:, o:o + 1], SO[h][:, cs], ALU.mult, ALU.mult)
            for h in range(4):
                kb.dma(YB[h * 128:(h + 1) * 128, :], YR[h].v())

        PHASES["mlstm"] = phase_mlstm

        XST = scratch("XST", [T, 1024]); YS0 = scratch("YS0", [T, 1024])

        def phase_ssd(b, l):
            kb.release(0)
            BT = [kb.A([128, T]) for _ in range(4)]; CT = [kb.A([128, T]) for _ in range(4)]
            m0 = kb.aoff
            xin = kb.A([128, T]); acc = kb.A([128, T]); XO = [kb.A([128, 4, 128]) for _ in range(2)]
            for ti in range(16):
                kb.dma(xin.v(), UF[C_XBC + ti * 128:C_XBC + (ti + 1) * 128, :], q=("sp" if ti % 2 else "pool"))
                if ti < 8:
                    conv_silu(xin, acc, "mbcw", "mbcb", ti)
                    for c0 in range(0, 18, 4):
                        n = min(4, 18 - c0)
                        pT = nextps()
                        for j in range(n):
                            kb.tr(pT[:, j * 128:(j + 1) * 128], acc[:, (c0 + j) * 128:(c0 + j + 1) * 128], ident)
                        xo = XO[(c0 // 4) % 2]
                        evac(xo[:, 0:n, :], pT[:, 0:n * 128].re("p (c f) -> p c f", c=n))
                        kb.dma(XST[c0 * 128:(c0 + n) * 128, ti * 128:(ti + 1) * 128].re("(c p) f -> p c f", p=128), xo[:, 0:n, :])
                elif ti < 12:
                    conv_silu(xin, BT[ti - 8], "mbcw", "mbcb", ti)
                else:
                    conv_silu(xin, CT[ti - 12], "mbcw", "mbcb", ti)
            kb.release(m0)
            DTt = kb.A([128, 18, 32]); DA = kb.A([128, 18, 32]); RB_ = kb.A([128, 32]); AN = kb.A([128, 32])
            DROW = kb.A([128, 1024])
            rhsF = kb.A([128, 16, 128]); Dm = kb.A([128, 16, 128]); Mt = kb.A([128, 16, 128]); CBs = kb.A([128, 4, 128])
            XC = kb.A([128, 16, 64]); XD = kb.A([128, 16, 64]); XW = kb.A([128, 16, 64]); Yc = kb.A([128, 16, 64])
            Y0 = kb.A([128, 16, 64]); HST = kb.A([128, 16, 64]); BK = kb.A([128, 4, 128]); tmp = kb.A([128, 1024])
            ZC = kb.A([128, 1024]); YO = kb.A([128, 8, 128]); EX = kb.A([128, 48]); ssq = kb.A([128, 4])
            kb.dma(DTt.v(), SMALL[:, 16:48].re("(c p) g -> p c g", p=128), allow_slow_non_contiguous=True)
            kb.dma(RB_.v(), V(rowp, rowp.t[l, RP_DTB:RP_DTB + 32].partition_broadcast(128)))
            kb.dma(AN.v(), V(rowp, rowp.t[l, RP_ALOG:RP_ALOG + 32].partition_broadcast(128)))
            kb.dma(DROW.v(), V(rowp, rowp.t[l, RP_MBD:RP_MBD + 1024].partition_broadcast(128)))
            kb.tt(DTt.v(), DTt.v(), bcmid(RB_.v(), 18), ALU.add)
            kb.act(DTt.v(), DTt.v(), AF.Exp)
            kb.act(DTt.v(), DTt.v(), AF.Ln, bias=1.0)
            kb.act(AN.v(), AN.v(), AF.Exp)
            kb.ts(AN.v(), AN.v(), -1.0, None, op0=ALU.mult)
            kb.tt(DA.v(), DTt.v(), bcmid(AN.v(), 18), ALU.mult)
            for d in range(2):
                tri, smat = DIRC[d]
                kb.memset(HST.v(), 0.0)
                for c in CHUNK_ORDER[d]:
                    cs = slice(c * 128, (c + 1) * 128)
                    da = DA[:, c, d * 16:(d + 1) * 16]; dt = DTt[:, c, d * 16:(d + 1) * 16]
                    kb.dma(XC.v().re("p h x -> p (h x)"), XST[c * 128:(c + 1) * 128, :])
                    kb.tt(rhsF.v(), bcmid(cview(tri), 16), bc3(da, 128), ALU.mult)
                    pS = [nextps(), nextps()]
                    for q in range(4):
                        kb.mm(pS[q // 2][:, (q % 2) * 512:(q % 2 + 1) * 512], cview(smat),
                              rhsF[:, q * 4:(q + 1) * 4, :].re("p h t -> p (h t)"))
                    pB2 = nextps()
                    kb.mm(pB2[:, 0:16], cview(tri), da)
                    kb.mm(pB2[:, 16:32], cview(smat), da)
                    kb.mm(pB2[:, 32:48], ones, da)
                    kb.act(EX.v(), pB2[:, 0:48], AF.Exp)
                    for half in range(2):
                        kb.act(Dm[:, half * 8:(half + 1) * 8, :], pS[half].v().re("p (h t) -> p h t", h=8), AF.Exp)
                    kb.tt(Dm.v(), Dm.v(), bcmid(cview(tri), 16), ALU.mult)
                    pCB = nextps()
                    for g in range(4):
                        kb.mm(pCB[:, g * 128:(g + 1) * 128], BT[g][:, cs], CT[g][:, cs])
                    evac(CBs.v(), pCB[:, 0:512].re("p (g t) -> p g t", g=4))
                    cb4 = V(CBs, CBs.t.rearrange("p g (o t) -> p g o t", o=1).to_broadcast([128, 4, 4, 128]))
                    kb.tt(Mt.v().re("p (g j) t -> p g j t", g=4), Dm.v().re("p (g j) t -> p g j t", g=4), cb4, ALU.mult)
                    kb.tt(XD.v(), XC.v(), bc3(dt, 64), ALU.mult)
                    pY = nextps(); pE = nextps()
                    for hd in range(16):
                        kb.mm(pY[:, hd * 64:(hd + 1) * 64], Mt[:, hd, :], XD[:, hd, :])
                    for hd in range(16):
                        kb.mm(pE[:, hd * 64:(hd + 1) * 64], CT[hd // 4][:, cs], HST[:, hd, :])
                    kb.tt(Yc.v(), pE.v().re("p (h x) -> p h x", h=16), bc3(EX[:, 0:16], 64), ALU.mult)
                    kb.tt(Yc.v(), Yc.v(), pY.v().re("p (h x) -> p h x", h=16), ALU.add)
                    pT = nextps()
                    for g in range(4):
                        kb.tr(pT[:, g * 128:(g + 1) * 128], BT[g][:, cs], ident)
                    evac(BK.v(), pT[:, 0:512].re("p (g t) -> p g t", g=4))
                    kb.tt(XW.v(), XD.v(), bc3(EX[:, 16:32], 64), ALU.mult)
                    pH = nextps()
                    for hd in range(16):
                        kb.mm(pH[:, hd * 64:(hd + 1) * 64], BK[:, hd // 4, :], XW[:, hd, :])
                    kb.tt(HST.v(), HST.v(), bc3(EX[:, 32:48], 64), ALU.mult)
                    kb.tt(HST.v(), HST.v(), pH.v().re("p (h x) -> p h x", h=16), ALU.add)
                    Ycf = Yc.v().re("p h x -> p (h x)")
                    if d == 0:
                        kb.dma(YS0[c * 128:(c + 1) * 128, :], Ycf, q="pool")
                        continue
                    kb.dma(Y0.v().re("p h x -> p (h x)"), YS0[c * 128:(c + 1) * 128, :], q="pool")
                    kb.dma(ZC.v(), ZT[c * 128:(c + 1) * 128, :], q="pool")
                    kb.tt(Yc.v(), Yc.v(), Y0.v(), ALU.add)
                    kb.tt(tmp.v(), XC.v().re("p h x -> p (h x)"), DROW.v(), ALU.mult)
                    kb.tt(Ycf, Ycf, tmp.v(), ALU.add)
                    kb.tt(Ycf, Ycf, ZC.v(), ALU.mult)
                    kb.act(tmp.v(), Ycf, AF.Square)
                    kb.reduce(ssq.v(), tmp.v().re("p (g x) -> p g x", g=4), ALU.add)
                    kb.ts(ssq.v(), ssq.v(), 1.0 / 256, EPS, op0=ALU.mult, op1=ALU.add)
                    kb.act(ssq.v(), ssq.v(), AF.Sqrt); kb.recip(ssq.v(), ssq.v())
                    y4 = Yc.v().re("p (g j) x -> p g (j x)", g=4)
                    kb.tt(y4, y4, bc3(ssq.v(), 256), ALU.mult)
                    pTs = [nextps(), nextps()]
                    for kt in range(8):
                        kb.tr(pTs[kt // 4][:, (kt % 4) * 128:(kt % 4 + 1) * 128], Ycf[:, kt * 128:(kt + 1) * 128], ident)
                    for kt in range(8):
                        o = CP_OFF["mbng"] + kt
                        kb.ts(YO[:, kt, :], pTs[kt // 4][:, (kt % 4) * 128:(kt % 4 + 1) * 128], CP[:, o:o + 1], None, op0=ALU.mult)
                    kb.dma(YC.v().re("(k p) t -> p k t", p=128)[:, :, cs], YO.v())

        PHASES["ssd"] = phase_ssd

        def phase_merge(b, l):
            kb.release(0)
            PA = kb.Ab([128, 4, 1024]); PB = kb.Ab([128, 4, 1024]); PC = kb.Ab([128, 8, 1024]); WO = kb.Ab([128, 8, 1024])
            ST = [kb.A([128, 4, 1024])] * 2
            for j, (dst, src) in enumerate([(PA[:, 0:4, :], p_a[l]), (PB[:, 0:4, :], p_b[l]), (PC[:, 0:4, :], p_c[l, 0:512, :]),
                                            (PC[:, 4:8, :], p_c[l, 512:1024, :]), (WO[:, 0:4, :], w_out[l, 0:512, :]), (WO[:, 4:8, :], w_out[l, 512:1024, :])]):
                st = ST[j % 2]
                kb.dma(st.v(), src.re("(k p) n -> p k n", p=128), q=("sp" if j % 2 else "pool"))
                castb(dst, st.v())
            G1 = [kb.A([128, 1024]) for _ in range(2)]
            for which in range(2):
                kb.dma(G1[which].v(), V(MOD, MOD.t[which, 2048:3072].partition_broadcast(128)))
            YAg = kb.A([128, 4, 512]); YBg = kb.A([128, 4, 512]); YCg = kb.A([128, 8, 512])
            YAb = kb.Ab([128, 4, 512]); YBb = kb.Ab([128, 4, 512]); YCb = kb.Ab([128, 8, 512]); mTb = kb.Ab([128, 8, 512])
            GT = kb.A([128, 3, 512]); mT = kb.A([128, 8, 512]); tmp = kb.A([128, 512])
            xt = kb.A([128, 1024]); ol = kb.A([128, 1024])
            for (t0, tw) in GROUPS:
                kb.dma(YAg[:, :, 0:tw], YA.v().re("(k p) t -> p k t", p=128)[:, :, t0:t0 + tw])
                kb.dma(YBg[:, :, 0:tw], YB.v().re("(k p) t -> p k t", p=128)[:, :, t0:t0 + tw], q="pool")
                kb.dma(YCg[:, :, 0:tw], YC.v().re("(k p) t -> p k t", p=128)[:, :, t0:t0 + tw])
                for Yg_, Yb_ in ((YAg, YAb), (YBg, YBb), (YCg, YCb)):
                    castb(Yb_[:, :, 0:tw], Yg_[:, :, 0:tw])
                for dt_ in range(8):
                    ds_ = slice(dt_ * 128, (dt_ + 1) * 128)
                    for br in range(3):
                        r0 = C_G + br * 1024 + dt_ * 128
                        kb.dma(GT[:, br, 0:tw], UF[r0:r0 + 128, t0:t0 + tw], q=("pool" if br % 2 else "sp"))
                    for br, (Wt, Yg, nk) in enumerate([(PA, YAb, 4), (PB, YBb, 4), (PC, YCb, 8)]):
                        ps = nextps()[:, 0:tw]
                        for k in range(nk):
                            kb.mm(ps, Wt[:, k, ds_], Yg[:, k, 0:tw], start=(k == 0), stop=(k == nk - 1))
                        if br == 0:
                            kb.tt(mT[:, dt_, 0:tw], ps, GT[:, br, 0:tw], ALU.mult)
                        else:
                            kb.tt(tmp[:, 0:tw], ps, GT[:, br, 0:tw], ALU.mult)
                            kb.tt((mTb if br == 2 else mT)[:, dt_, 0:tw], mT[:, dt_, 0:tw], tmp[:, 0:tw], ALU.add)
                for tile in range(tw // 128):
                    i = t0 // 128 + tile
                    load_x(b, l, i, xt)
                    for half in range(2):
                        ps = nextps()[:, 0:512]
                        for k in range(8):
                            kb.mm(ps, mTb[:, k, tile * 128:(tile + 1) * 128], WO[:, k, half * 512:(half + 1) * 512],
                                  start=(k == 0), stop=(k == 7))
                        kb.tt(ol[:, half * 512:(half + 1) * 512], ps, G1[1 if i < 2 else 0][:, half * 512:(half + 1) * 512], ALU.mult)
                    kb.tt(xt.v(), xt.v(), ol.v(), ALU.add)
                    store_x(b, l, i, xt)

        PHASES["merge"] = phase_merge

        def phase_moe(b, l):
            kb.release(0)
            G2 = [kb.A([128, 1024]) for _ in range(2)]
            for which in range(2):
                kb.dma(G2[which].v(), V(MOD, MOD.t[which, 5120:6144].partition_broadcast(128)))
            mA = kb.aoff
            gs = {}
            for which in (0, 1):
                gs[which] = mod_tiles(l, which, 4096, 3072, RP_N2)
            xts = [kb.A([128, D]) for _ in range(2)]; hs = [kb.A([128, D]) for _ in range(2)]
            junk = kb.A([128, D]); ssq = kb.A([128, 1]); rstd = kb.A([128, 1])
            hTt = kb.A([128, 8, 128]); RW = kb.A([128, 8, 16]); AFFT = kb.A([16, T])
            lg = kb.A([128, 16]); mx = kb.A([128, 1]); sm = kb.A([128, 1])
            kb.dma(RW.v(), router_w[l].re("(k p) e -> p k e", p=128), allow_slow_non_contiguous=True)
            for i in range(NT):
                xt = xts[i % 2]; h = hs[i % 2]
                kb.dma(xt.v(), XS[b, i * 128:(i + 1) * 128, :])
                gsc, sh = gs[1 if i < 2 else 0]
                rmsnorm(xt.v(), h.v(), gsc.v(), sh.v(), junk.v(), ssq.v(), rstd.v())
                kb.dma(H2[i * 128:(i + 1) * 128, :], h.v(), q="pool")
                transpose_tile(h, lambda k0: hTt[:, k0:k0 + 4, :])
                ps = nextps()
                for k in range(8):
                    kb.mm(ps[:, 0:16], hTt[:, k, :], RW[:, k, :], start=(k == 0), stop=(k == 7))
                kb.reduce(mx.v(), ps[:, 0:16], ALU.max)
                kb.ts(mx.v(), mx.v(), -1.0, None, op0=ALU.mult)
                kb.act(lg.v(), ps[:, 0:16], AF.Exp, bias=mx.v(), accum=sm.v())
                kb.recip(sm.v(), sm.v())
                kb.ts(lg.v(), lg.v(), sm.v(), None, op0=ALU.mult)
                ps2 = nextps()
                kb.tr(ps2[0:16, 0:128], lg.v(), ident)
                evac(AFFT[:, i * 128:(i + 1) * 128], ps2[0:16, 0:128])
            kb.dma(AFS.v(), AFFT.v())
            for (tile0, ntile, cap) in [(0, 2, 32), (2, 16, 256)]:
                kb.release(mA)
                n = ntile * 128
                cw = min(cap, 128); nct = cap // cw
                SELG_T = kb.A([128, ntile, 16]); RANK_T = kb.A([128, ntile, 16]); MASK_T = kb.A([128, ntile, 16])
                mB = kb.aoff
                work = kb.A([16, n]); selg = kb.A([16, n]); mask = kb.A([16, n]); rank = kb.A([16, n]); zer = kb.A([16, n])
                mx8 = kb.A([16, 8])
                kb.dma(work.v(), AFS[0:16, tile0 * 128:tile0 * 128 + n])
                kb.dma(selg.v(), AFS[0:16, tile0 * 128:tile0 * 128 + n], q="pool")
                for it in range(cap // 8):
                    kb.op("dve", lambda: nc.vector.max(out=mx8.t[:], in_=work.t[:]), [work.v()], [mx8.v()])
                    kb.op("dve", lambda: nc.vector.match_replace(out=work.t[:], in_to_replace=mx8.t[:], in_values=work.t[:], imm_value=0.0),
                          [work.v(), mx8.v()], [work.v()])
                kb.tt(selg.v(), selg.v(), work.v(), ALU.subtract)
                kb.ts(mask.v(), selg.v(), 0.0, None, op0=ALU.is_gt)
                kb.memset(zer.v(), 0.0)
                kb.scan(rank.v(), mask.v(), zer.v(), 0.0, ALU.add, ALU.add)
                for i in range(ntile):
                    ps = nextps()
                    kb.tr(ps[:, 0:16], selg[:, i * 128:(i + 1) * 128], ident[0:16, 0:16])
                    kb.tr(ps[:, 16:32], rank[:, i * 128:(i + 1) * 128], ident[0:16, 0:16])
                    kb.copy(SELG_T[:, i, :], ps[:, 0:16])
                    kb.copy(RANK_T[:, i, :], ps[:, 16:32])
                kb.ts(MASK_T.v(), SELG_T.v(), 0.0, None, op0=ALU.is_gt)
                kb.release(mB)
                OUT = kb.A([128, ntile, 1024]); SelE = kb.Ab([128, ntile, cap]); SelGT = kb.Ab([128, nct, n])
                H2T = [kb.A([128, 1024]) for _ in range(2)]; W = [kb.A([128, 8, 512]) for _ in range(2)]
                H2B = [kb.Ab([128, 1024]) for _ in range(2)]; WBb = [kb.Ab([128, 8, 512]) for _ in range(2)]
                xeT = kb.Ab([128, 8, cap]); actT = kb.Ab([128, 8, cap]); sa = kb.A([128, cap]); ye = kb.Ab([128, nct, 1024])
                SG_ = [kb.A([128, cap]) for _ in range(2)]; xt = H2T[0]
                iota = CST[:, CS_IOTA:CS_IOTA + cap]

                wcnt = {"w": 0}

                def load_w(src):
                    wb = W[wcnt["w"] % 2]; wbb = WBb[wcnt["w"] % 2]
                    q = "sp" if wcnt["w"] % 2 else "pool"
                    wcnt["w"] += 1
                    kb.dma(wb.v(), src.re("(k p) n -> p k n", p=128), q=q)
                    castb(wbb.v(), wb.v())
                    return wbb

                for e in range(16):
                    for i in range(ntile):
                        kb.ts(SelE[:, i, :], iota, RANK_T[:, i, e:e + 1], MASK_T[:, i, e:e + 1], op0=ALU.is_equal, op1=ALU.mult)
                        sg = SG_[i % 2]
                        kb.ts(sg.v(), iota, RANK_T[:, i, e:e + 1], SELG_T[:, i, e:e + 1], op0=ALU.is_equal, op1=ALU.mult)
                        ps = nextps()
                        for ct in range(nct):
                            kb.tr(ps[0:cw, ct * 128:(ct + 1) * 128], sg[:, ct * cw:(ct + 1) * cw], ident)
                        evac(SelGT[0:cw, :, i * 128:(i + 1) * 128], ps[0:cw, 0:nct * 128].re("p (c t) -> p c t", c=nct))
                    for pas in range(2):
                        for i in range(ntile):
                            h2t = H2T[i % 2]
                            kb.dma(h2t.v(), H2[(tile0 + i) * 128:(tile0 + i + 1) * 128, :], q=("sp" if i % 2 else "pool"))
                            h2b = H2B[i % 2]
                            castb(h2b.v(), h2t.v())
                            for kk in range(4):
                                k = pas * 4 + kk
                                pg = PS[kk // 2][:, (kk % 2) * 512:(kk % 2) * 512 + cap]
                                kb.mm(pg, h2b[:, k * 128:(k + 1) * 128], SelE[:, i, :], start=(i == 0), stop=(i == ntile - 1))
                        for kk in range(4):
                            k = pas * 4 + kk
                            evac(xeT[:, k, :], PS[kk // 2][:, (kk % 2) * 512:(kk % 2) * 512 + cap])
                    for half in range(2):
                        wg = load_w(e_wg[l, e, :, half * 512:(half + 1) * 512])
                        wu = load_w(e_wu[l, e, :, half * 512:(half + 1) * 512])
                        for ft in range(4):
                            p1 = nextps()[:, 0:cap]; p2 = nextps()[:, 0:cap]
                            for k in range(8):
                                kb.mm(p1, wg[:, k, ft * 128:(ft + 1) * 128], xeT[:, k, :], start=(k == 0), stop=(k == 7))
                            for k in range(8):
                                kb.mm(p2, wu[:, k, ft * 128:(ft + 1) * 128], xeT[:, k, :], start=(k == 0), stop=(k == 7))
                            kb.act(sa.v(), p1, AF.Silu)
                            kb.tt(actT[:, half * 4 + ft, :], sa.v(), p2, ALU.mult)
                    for ch in range(2):
                        wd = load_w(e_wd[l, e, :, ch * 512:(ch + 1) * 512])
                        for ct in range(nct):
                            ps = nextps()
                            for k in range(8):
                                kb.mm(ps[0:cw, 0:512], actT[:, k, ct * cw:(ct + 1) * cw], wd[:, k, :], start=(k == 0), stop=(k == 7))
                            evac(ye[0:cw, ct, ch * 512:(ch + 1) * 512], ps[0:cw, 0:512])
                    for i in range(ntile):
                        for ch in range(2):
                            ps = nextps()
                            for ct in range(nct):
                                kb.mm(ps[:, 0:512], SelGT[0:cw, ct, i * 128:(i + 1) * 128], ye[0:cw, ct, ch * 512:(ch + 1) * 512],
                                      start=(ct == 0), stop=(ct == nct - 1))
                            o_ = OUT[:, i, ch * 512:(ch + 1) * 512]
                            if e == 0:
                                evac(o_, ps[:, 0:512])
                            else:
                                kb.tt(o_, o_, ps[:, 0:512], ALU.add)
                which = 1 if tile0 == 0 else 0
                for i in range(ntile):
                    rows = XS[b, (tile0 + i) * 128:(tile0 + i + 1) * 128, :]
                    kb.dma(xt.v(), rows)
                    kb.tt(OUT[:, i, :], OUT[:, i, :], G2[which].v(), ALU.mult)
                    kb.tt(xt.v(), xt.v(), OUT[:, i, :], ALU.add)
                    kb.dma(rows, xt.v(), q="pool")

        PHASES["moe"] = phase_moe

        AFS = scratch("AFS", [16, T])

        def phase_final():
            kb.release(0)
            gsc = kb.A([128, D]); xts = [kb.A([128, D]) for _ in range(2)]; hs = [kb.A([128, D]) for _ in range(2)]
            junk = kb.A([128, D]); ssq = kb.A([128, 1]); rstd = kb.A([128, 1])
            kb.dma(gsc.v(), V(final_g, final_g.t[0, :].partition_broadcast(128)))
            for b in range(NB):
                for j in range(16):
                    xt = xts[j % 2]; h = hs[j % 2]
                    kb.dma(xt.v(), XS[b, 256 + j * 128:256 + (j + 1) * 128, :])
                    rmsnorm(xt.v(), h.v(), gsc.v(), None, junk.v(), ssq.v(), rstd.v())
                    kb.dma(out[b, j * 128:(j + 1) * 128, :], h.v(), q="pool")

        for b in range(NB):
            kb.dma(XS[b, 0:256, :], ctx_in[b], q="sp")
            kb.dma(XS[b, 256:T, :], x_in[b], q="pool")
        seq = stop if stop is not None else ["mod", "in", "rwkv", "mlstm", "ssd", "merge", "moe"]
        for b in range(NB):
            for l in range(NL):
                for ph in seq:
                    PHASES[ph](b, l)
        if stop is None:
            phase_final()
        kb.release(0)
        kb.finish([out])
        print("instructions:", kb.nins, flush=True)
    return nc


WNAMES = ["ada_w", "ada_b", "w_in", "rw_w2", "rw_a2", "rw_g2", "p_a", "p_b", "p_c", "w_out", "router_w", "e_wg", "e_wu", "e_wd"]


def make_in_maps(inputs, ncores=8, nb=2, LW=4, EW=16):
    P = {k: np.asarray(v) for k, v in inputs.items()}
    shared = {k: np.ascontiguousarray(P[k][:LW, :EW] if k.startswith("e_w") else P[k][:LW], dtype=np.float32) for k in WNAMES}
    shared["final_g"] = np.ascontiguousarray(P["final_g"].reshape(1, D), dtype=np.float32)
    shared["colp"] = np.stack([host_colpack(P, l) for l in range(LW)])
    shared["rowp"] = np.stack([host_rowpack(P, l) for l in range(LW)])
    shared["cst"] = host_consts()
    shared["cctx"] = np.ascontiguousarray(P["c_ctx"].reshape(8, 128).T, dtype=np.float32)
    maps = []
    for c in range(ncores):
        m = dict(shared)
        sl = slice(c * nb, (c + 1) * nb)
        m["x"] = np.ascontiguousarray(P["x"][sl], dtype=np.float32)
        m["ctx"] = np.ascontiguousarray(P["ctx"][sl], dtype=np.float32)
        m["ccol"] = np.ascontiguousarray(P["c"][sl].reshape(nb, 8, 128).transpose(0, 2, 1), dtype=np.float32)
        maps.append(m)
    return maps


def kernel(**inputs):
    nc = build(NB=2, NL=4)
    maps = make_in_maps(inputs, 8, 2)
    res = run_bass_kernel_spmd(nc, maps, core_ids=list(range(8)))
    return np.concatenate([r["out"] for r in res.results], axis=0).astype(np.float32)
```

```python
import numpy as np
from contextlib import ExitStack
import concourse.bass as bass
import concourse.mybir as mybir
from concourse.bass_utils import run_bass_kernel_spmd

F32 = mybir.dt.float32
F32R = mybir.dt.float32r
BF16 = mybir.dt.bfloat16
AF = mybir.ActivationFunctionType
ALU = mybir.AluOpType
AX = mybir.AxisListType
NDS = 8


class V:
    __slots__ = ("buf", "ap")

    def __init__(s, buf, ap):
        s.buf = buf
        s.ap = ap

    def __getitem__(s, idx):
        return V(s.buf, s.ap[idx])

    def re(s, pat, **kw):
        return V(s.buf, s.ap.rearrange(pat, **kw))

    def bc(s, shape):
        return V(s.buf, s.ap.to_broadcast(list(shape)))

    def pbc(s, n):
        return V(s.buf, s.ap.partition_broadcast(n))

    def r(s):
        return V(s.buf, s.ap.bitcast(F32R))


class Buf:
    __slots__ = ("t", "w", "r", "psum")

    def __init__(s, t, psum=False):
        s.t = t
        s.w = None
        s.r = {}
        s.psum = psum

    def __getitem__(s, idx):
        return V(s, s.t[idx])

    def v(s):
        return V(s, s.t[:])


class KB:
    def __init__(s, nc, es):
        s.nc = nc
        s.es = es
        s.eng = {"pe": nc.tensor, "dve": nc.vector, "act": nc.scalar, "pool": nc.gpsimd, "sp": nc.sync}
        s.semobj = {}
        s.cnt = {}
        for e in ("pe", "dve", "act", "pool"):
            s.semobj[e] = es.enter_context(nc.semaphore("s_" + e))
            s.cnt[e] = 0
        s.dq = ("sp", "pool", "act")
        s.dma_rr = {q: 0 for q in s.dq}
        s.dma_val = {}
        for q in s.dq:
            for i in range(NDS):
                s.semobj[(q, i)] = es.enter_context(nc.semaphore("d_%s%d" % (q, i)))
                s.dma_val[(q, i)] = 0
        s.waited = {e: {} for e in s.eng}
        s.nins = 0

    def sb(s, name, shape, dt=F32):
        return Buf(s.es.enter_context(s.nc.sbuf_tensor(name, list(shape), dt)))

    def ps(s, name, shape, dt=F32):
        return Buf(s.es.enter_context(s.nc.psum_tensor(name, list(shape), dt)), psum=True)

    def dram(s, name, shape, dt=F32, kind="Internal"):
        return Buf(s.nc.dram_tensor(name, list(shape), dt, kind=kind).ap())

    def _waits(s, e, reads, writes, is_dma):
        deps = {}

        def need(ev):
            if ev is None:
                return
            k, val, src = ev
            if src == "pe" and e == "pe" and not is_dma:
                return
            if deps.get(k, 0) < val:
                deps[k] = val

        for x in reads:
            need(x.buf.w)
            if x.buf.psum:
                for k, (val, src) in x.buf.r.items():
                    if src != e:
                        need((k, val, src))
        for x in writes:
            need(x.buf.w)
            for k, (val, src) in x.buf.r.items():
                need((k, val, src))
        w = s.waited[e]
        for k, val in deps.items():
            if w.get(k, 0) < val:
                s.eng[e].wait_ge(s.semobj[k], val)
                w[k] = val
                s.nins += 1

    def _post(s, ev, reads, writes):
        k, val, src = ev
        for x in reads:
            x.buf.r[k] = (val, src)
        for x in writes:
            x.buf.w = ev
            x.buf.r = {}

    def op(s, e, fn, reads, writes):
        s._waits(e, reads, writes, False)
        ins = fn()
        s.cnt[e] += 1
        ins.then_inc(s.semobj[e], 1)
        s.nins += 1
        s._post((e, s.cnt[e], e), reads, writes)

    def dma(s, out, in_, q="sp", **kw):
        s._waits(q, [in_], [out], True)
        slot = s.dma_rr[q]
        s.dma_rr[q] = (slot + 1) % NDS
        k = (q, slot)
        prev = s.dma_val[k]
        if s.waited[q].get(k, 0) < prev:
            s.eng[q].wait_ge(s.semobj[k], prev)
            s.waited[q][k] = prev
        ins = s.eng[q].dma_start(out=out.ap, in_=in_.ap, **kw)
        ins.then_inc(s.semobj[k], 16)
        s.nins += 1
        s.dma_val[k] = prev + 16
        s._post((k, prev + 16, "dma"), [in_], [out])

    def finish(s, bufs):
        for b in bufs:
            ev = b.w
            if ev is not None:
                k, val, _ = ev
                s.eng["sp"].wait_ge(s.semobj[k], val)
        for k, val in s.dma_val.items():
            if val:
                s.eng["sp"].wait_ge(s.semobj[k], val)

    def mm(s, out, lhsT, rhs, start=True, stop=True):
        s.op("pe", lambda: s.nc.tensor.matmul(out.ap, lhsT.ap, rhs.ap, start=start, stop=stop),
             [lhsT, rhs], [out])

    def tr(s, out, in_, ident):
        s.op("pe", lambda: s.nc.tensor.transpose(out.ap, in_.ap, ident.ap), [in_, ident], [out])

    def act(s, out, in_, func, bias=None, scale=1.0, accum=None):
        rd = [in_]
        kw = {}
        if isinstance(bias, V):
            rd.append(bias)
            kw["bias"] = bias.ap
        elif bias is not None:
            kw["bias"] = bias
        if isinstance(scale, V):
            rd.append(scale)
            kw["scale"] = scale.ap
        else:
            kw["scale"] = scale
        wr = [out]
        if accum is not None:
            wr.append(accum)
            kw["accum_out"] = accum.ap
        s.op("act", lambda: s.nc.scalar.activation(out=out.ap, in_=in_.ap, func=func, **kw), rd, wr)

    def tt(s, out, a, b, op, e="dve"):
        s.op(e, lambda: s.eng[e].tensor_tensor(out.ap, a.ap, b.ap, op), [a, b], [out])

    def ts(s, out, a, s1, s2=None, op0=ALU.mult, op1=None, accum=None, e="dve"):
        rd = [a]
        a1 = s1.ap if isinstance(s1, V) else s1
        a2 = s2.ap if isinstance(s2, V) else s2
        if isinstance(s1, V):
            rd.append(s1)
        if isinstance(s2, V):
            rd.append(s2)
        kw = {}
        if op1 is not None:
            kw["op1"] = op1
        wr = [out]
        if accum is not None:
            kw["accum_out"] = accum.ap
            wr.append(accum)
        s.op(e, lambda: s.eng[e].tensor_scalar(out.ap, a.ap, a1, a2, op0, **kw), rd, wr)

    def stt(s, out, a, sc, b, op0, op1, e="dve"):
        rd = [a, b]
        a1 = sc.ap if isinstance(sc, V) else sc
        if isinstance(sc, V):
            rd.append(sc)
        s.op(e, lambda: s.eng[e].scalar_tensor_tensor(out.ap, a.ap, a1, b.ap, op0, op1), rd, [out])

    def copy(s, out, in_, e="dve"):
        if e == "act":
            s.act(out, in_, AF.Copy)
        else:
            s.op(e, lambda: s.eng[e].tensor_copy(out.ap, in_.ap), [in_], [out])

    def memset(s, out, val, e="dve"):
        s.op(e, lambda: s.eng[e].memset(out.ap, val), [], [out])

    def recip(s, out, in_):
        s.op("dve", lambda: s.nc.vector.reciprocal(out.ap, in_.ap), [in_], [out])

    def reduce(s, out, in_, op, axis=AX.X):
        s.op("dve", lambda: s.nc.vector.tensor_reduce(out.ap, in_.ap, axis, op), [in_], [out])


def _kb_barrier(s):
    for e in s.eng:
        w = s.waited[e]
        for k in ("pe", "dve", "act", "pool"):
            if w.get(k, 0) < s.cnt[k]:
                s.eng[e].wait_ge(s.semobj[k], s.cnt[k])
                w[k] = s.cnt[k]
                s.nins += 1
        for k, val in s.dma_val.items():
            if val and w.get(k, 0) < val:
                s.eng[e].wait_ge(s.semobj[k], val)
                w[k] = val
                s.nins += 1


def _kb_init_arena(s, n):
    s.arena = s.es.enter_context(s.nc.sbuf_tensor("arena", [128, n], F32))
    s.aoff = 0
    s.an = n


def _kb_A(s, shape):
    n = int(np.prod(shape[1:]))
    assert s.aoff + n <= s.an, ("arena overflow", s.aoff, n, s.an)
    ap = s.arena[0:shape[0], s.aoff:s.aoff + n]
    s.aoff += n
    if len(shape) == 3:
        ap = ap.rearrange("p (a b) -> p a b", a=shape[1])
    elif len(shape) == 4:
        ap = ap.rearrange("p (a b c) -> p a b c", a=shape[1], b=shape[2])
    return Buf(ap)


def _kb_release(s, mark=0):
    s.barrier()
    s.aoff = mark


def _kb_scan(s, out, d0, d1, init, op0, op1):
    s.op("dve", lambda: s.nc.vector.tensor_tensor_scan(out.ap, d0.ap, d1.ap, init, op0, op1), [d0, d1], [out])


def _kb_Ab(s, shape):
    n = int(np.prod(shape[1:]))
    nf = (n + 1) // 2
    assert s.aoff + nf <= s.an, ("arena overflow", s.aoff, nf, s.an)
    ap = s.arena[0:shape[0], s.aoff:s.aoff + nf].bitcast(BF16)[:, 0:n]
    s.aoff += nf
    if len(shape) == 3:
        ap = ap.rearrange("p (a b) -> p a b", a=shape[1])
    return Buf(ap)


KB.Ab = _kb_Ab
KB.barrier = _kb_barrier
KB.init_arena = _kb_init_arena
KB.A = _kb_A
KB.release = _kb_release
KB.scan = _kb_scan

D = 1024
T = 2304
NT = 18
EPS = 1e-6
GROUPS = [(0, 512), (512, 512), (1024, 512), (1536, 512), (2048, 256)]
SEGS = [(0, 256), (256, 2304)]
C_MLQK, C_MLV, C_MLO, C_MLG, C_Z, C_XBC, C_DT, C_G = 1792, 2304, 2816, 3328, 3344, 4368, 6416, 6448
CHUNK_ORDER = {0: list(range(18)), 1: [1, 0] + list(range(17, 1, -1))}

CP_OFF = {}
_c = 0
for _n, _w in [("mu", 14), ("w0_0", 4), ("w0_1", 4), ("a0_0", 4), ("a0_1", 4), ("kk", 4), ("ka", 4), ("rk", 4),
               ("lnw", 4), ("lnb", 4)] + [("mlcw%d" % k, 4) for k in range(5)] + [("mlcb", 4), ("mlng", 4)] + \
              [("mbcw%d" % k, 16) for k in range(5)] + [("mbcb", 16), ("mbng", 8), ("om", 14), ("hm", 14)]:
    CP_OFF[_n] = _c
    _c += _w
NCP = _c
RP_N1, RP_N2, RP_MLGB, RP_DTB, RP_ALOG, RP_MBD, NRP = 0, 1024, 2048, 2064, 2096, 2128, 3152
CS_ID, CS_UI, CS_US, CS_LI, CS_LS, CS_ONE, CS_BLK, CS_IOTA, CS_RST, NCS = 0, 128, 256, 384, 512, 640, 768, 896, 1152, 1152 + 2304


def host_consts():
    p = np.arange(128)[:, None]
    f = np.arange(128)[None, :]
    c = np.zeros((128, NCS), np.float32)
    c[:, CS_ID:CS_ID + 128] = (p == f)
    c[:, CS_UI:CS_UI + 128] = (p <= f)
    c[:, CS_US:CS_US + 128] = (p < f)
    c[:, CS_LI:CS_LI + 128] = (p >= f)
    c[:, CS_LS:CS_LS + 128] = (p > f)
    c[:, CS_ONE:CS_ONE + 128] = 1.0
    c[:, CS_BLK:CS_BLK + 128] = (p // 64 == f // 64)
    c[:, CS_IOTA:CS_IOTA + 256] = np.arange(1, 257)[None, :]
    rst = np.ones(2304, np.float32)
    rst[::128] = 0.0
    c[:, CS_RST:CS_RST + 2304] = rst[None, :]
    return c


def host_colpack(P, l):
    cp = np.zeros((128, NCP), np.float32)

    def put(name, vec):
        v = np.asarray(vec, np.float32).reshape(-1, 128).T
        cp[:, CP_OFF[name]:CP_OFF[name] + v.shape[1]] = v

    put("mu", P["rw_mu"][l])
    for d in range(2):
        put("w0_%d" % d, P["rw_w0"][l, d])
        put("a0_%d" % d, P["rw_a0"][l, d])
    put("kk", P["rw_kk"][l]); put("ka", P["rw_ka"][l]); put("rk", P["rw_rk"][l])
    put("lnw", P["rw_ln_w"][l]); put("lnb", P["rw_ln_b"][l])
    for k in range(5):
        put("mlcw%d" % k, P["ml_conv_w"][l, k])
        put("mbcw%d" % k, P["mb_conv_w"][l, k])
    put("mlcb", P["ml_conv_b"][l]); put("mlng", P["ml_norm_g"][l])
    put("mbcb", P["mb_conv_b"][l]); put("mbng", P["mb_norm_g"][l])
    return cp


def host_rowpack(P, l):
    r = np.zeros((NRP,), np.float32)
    r[RP_N1:RP_N1 + 1024] = P["norm1_g"][l]
    r[RP_N2:RP_N2 + 1024] = P["norm2_g"][l]
    r[RP_MLGB:RP_MLGB + 16] = P["ml_gate_b"][l]
    r[RP_DTB:RP_DTB + 32] = P["mb_dt_bias"][l].reshape(-1)
    r[RP_ALOG:RP_ALOG + 32] = P["mb_a_log"][l].reshape(-1)
    r[RP_MBD:RP_MBD + 1024] = np.repeat(P["mb_d"][l], 64)
    return r

RW_CUT = 0


def build(NB=2, NL=4, dbg=(), stop=None, LW=4, EW=16):
    nc = bass.Bass("TRN2", target_bir_lowering=False)
    es = ExitStack()
    with es:
        kb = KB(nc, es)

        def din(name, shape):
            return kb.dram(name, shape, kind="ExternalInput")

        def scratch(name, shape):
            return kb.dram(name, shape, kind=("ExternalOutput" if name in dbg else "Internal"))

        x_in = din("x", [NB, 2048, D]); ctx_in = din("ctx", [NB, 256, D])
        ccol = din("ccol", [NB, 128, 8]); cctx = din("cctx", [128, 8])
        ada_w = din("ada_w", [LW, D, 6 * D]); ada_b = din("ada_b", [LW, 6 * D]); w_in = din("w_in", [LW, D, 9520])
        rw_w2 = din("rw_w2", [LW, 2, 64, 512]); rw_a2 = din("rw_a2", [LW, 2, 64, 512]); rw_g2 = din("rw_g2", [LW, 128, 512])
        p_a = din("p_a", [LW, 512, D]); p_b = din("p_b", [LW, 512, D]); p_c = din("p_c", [LW, D, D]); w_out = din("w_out", [LW, D, D])
        router_w = din("router_w", [LW, D, 16])
        e_wg = din("e_wg", [LW, EW, D, D]); e_wu = din("e_wu", [LW, EW, D, D]); e_wd = din("e_wd", [LW, EW, D, D])
        final_g = din("final_g", [1, D]); colp = din("colp", [LW, 128, NCP]); rowp = din("rowp", [LW, NRP])
        cst = din("cst", [128, NCS])
        out = kb.dram("out", [NB, 2048, D], kind="ExternalOutput")
        XS = scratch("XS", [NB, T, D]); MOD = scratch("MOD", [2, 6 * D]); UF = scratch("UF", [9520, T])
        VML = scratch("VML", [T, 512]); ZT = scratch("ZT", [T, 1024]); SMALL = scratch("SMALL", [T, 48])
        GG = scratch("GG", [512, T]); YA = scratch("YA", [512, T]); YB = scratch("YB", [512, T]); YC = scratch("YC", [1024, T])
        H2 = scratch("H2", [T, D])

        PS = [kb.ps("ps%d" % i, [128, 1024]) for i in range(4)]
        CST = kb.sb("CST", [128, NCS]); CP = kb.sb("CP", [128, NCP])
        kb.init_arena(45000)
        kb.dma(CST.v(), cst.v())
        ident = CST[:, CS_ID:CS_ID + 128]
        ones = CST[:, CS_ONE:CS_ONE + 128]
        blk = CST[:, CS_BLK:CS_BLK + 128]
        state = {"ev": 0}

        def castb(dst, src):
            state["ev"] += 1
            if state["ev"] % 2:
                kb.act(dst, src, AF.Copy)
            else:
                kb.copy(dst, src)

        def evac(o, i, func=None):
            if func is not None:
                kb.act(o, i, func)
                return
            state["ev"] += 1
            if state["ev"] % 2:
                kb.act(o, i, AF.Copy)
            else:
                kb.copy(o, i)

        def xrows(b, l, i):
            if i < 2:
                return [(slice(0, 128), XS[b, i * 128:(i + 1) * 128, :])]
            j = i - 2
            if l % 2 == 0:
                return [(slice(0, 128), XS[b, 256 + j * 128:256 + (j + 1) * 128, :])]
            lat = XS[b, 256:T, :].re("(r c) d -> c r d", c=64)
            return [(slice(cc * 32, (cc + 1) * 32), lat[4 * j + cc]) for cc in range(4)]

        def load_x(b, l, i, xt, q="sp"):
            for ps_, src in xrows(b, l, i):
                kb.dma(xt[ps_, :], src, q=q)

        def store_x(b, l, i, xt, q="pool"):
            for ps_, dst in xrows(b, l, i):
                kb.dma(dst, xt[ps_, :], q=q)

        def rmsnorm(xt, h, gsc, sh, junk, ssq, rstd):
            kb.act(junk, xt, AF.Square, accum=ssq)
            kb.ts(rstd, ssq, 1.0 / D, EPS, op0=ALU.mult, op1=ALU.add)
            kb.act(rstd, rstd, AF.Sqrt)
            kb.recip(rstd, rstd)
            kb.stt(h, xt, rstd, gsc, ALU.mult, ALU.mult)
            if sh is not None:
                kb.tt(h, h, sh, ALU.add)

        def transpose_tile(h, dst_fn):
            for half in range(2):
                p = PS[half]
                for kk in range(4):
                    k = half * 4 + kk
                    kb.tr(p[:, kk * 128:(kk + 1) * 128], h[:, k * 128:(k + 1) * 128], ident)
                evac(dst_fn(half * 4), p[:, 0:512].re("p (k n) -> p k n", k=4))

        def phase_mod(b, l):
            kb.release(0)
            cc = kb.A([128, 8, 2]); modsb = kb.A([2, 6 * D]); bias = kb.A([2, 6 * D])
            W = [kb.A([128, 8, 512]) for _ in range(2)]
            kb.dma(cc[:, :, 0], ccol[b], allow_slow_non_contiguous=True)
            kb.dma(cc[:, :, 1], cctx.v(), allow_slow_non_contiguous=True)
            kb.act(cc.v(), cc.v(), AF.Silu)
            kb.dma(bias.v(), V(ada_b, ada_b.t[l, :].partition_broadcast(2)))
            for n in range(12):
                wb = W[n % 2]
                kb.dma(wb.v(), ada_w[l, :, n * 512:(n + 1) * 512].re("(k p) n -> p k n", p=128), q=("sp" if n % 2 else "pool"))
                ps = PS[n % 2][0:2, 0:512]
                for k in range(8):
                    kb.mm(ps, cc[:, k, :], wb[:, k, :], start=(k == 0), stop=(k == 7))
                kb.tt(modsb[:, n * 512:(n + 1) * 512], ps, bias[:, n * 512:(n + 1) * 512], ALU.add)
            kb.dma(MOD.v(), modsb.v())
            kb.dma(CP.v(), colp[l])
            om = CP_OFF["om"]; hm = CP_OFF["hm"]; mu = CP_OFF["mu"]
            kb.ts(CP[:, om:om + 14], CP[:, mu:mu + 14], -1.0, 1.0, op0=ALU.mult, op1=ALU.add)
            kb.ts(CP[:, hm:hm + 14], CP[:, mu:mu + 14], 0.5, None, op0=ALU.mult)

        def mod_tiles(l, which, sc_off, sh_off, g_off):
            gsc = kb.A([128, D]); sh = kb.A([128, D]); grow = kb.A([128, D])
            kb.dma(grow.v(), V(rowp, rowp.t[l, g_off:g_off + D].partition_broadcast(128)))
            kb.dma(gsc.v(), V(MOD, MOD.t[which, sc_off:sc_off + D].partition_broadcast(128)))
            kb.dma(sh.v(), V(MOD, MOD.t[which, sh_off:sh_off + D].partition_broadcast(128)))
            kb.stt(gsc.v(), gsc.v(), 1.0, grow.v(), ALU.add, ALU.mult)
            return gsc, sh

        def phase_in(b, l):
            kb.release(0)
            hT = kb.Ab([128, 8, T])
            m0 = kb.aoff
            gs = {}
            for which in (0, 1):
                gs[which] = mod_tiles(l, which, 1024, 0, RP_N1)
            xts = [kb.A([128, D]) for _ in range(2)]; hs = [kb.A([128, D]) for _ in range(2)]
            junk = kb.A([128, D]); ssq = kb.A([128, 1]); rstd = kb.A([128, 1])
            for i in range(NT):
                xt = xts[i % 2]; h = hs[i % 2]
                load_x(b, l, i, xt)
                gsc, sh = gs[1 if i < 2 else 0]
                rmsnorm(xt.v(), h.v(), gsc.v(), sh.v(), junk.v(), ssq.v(), rstd.v())
                transpose_tile(h, lambda k0: hT[:, k0:k0 + 4, i * 128:(i + 1) * 128])
            kb.release(m0)
            W = [kb.A([128, 8, 512]) for _ in range(2)]; WB = [kb.Ab([128, 8, 512]) for _ in range(2)]
            RB = [kb.A([128, T]) for _ in range(2)]; RS = kb.A([128, T]); RO = [kb.A([128, T]) for _ in range(2)]
            TO = [kb.A([128, 1024]) for _ in range(2)]
            cnt = {"w": 0, "m": 0, "p": 0}

            def load_w(c0, ncol):
                wb = W[cnt["w"] % 2]; wbb = WB[cnt["w"] % 2]
                q = "sp" if cnt["w"] % 2 else "pool"
                cnt["w"] += 1
                kb.dma(wb[:, :, 0:ncol], w_in[l, :, c0:c0 + ncol].re("(k p) n -> p k n", p=128), q=q)
                castb(wbb[:, :, 0:ncol], wb[:, :, 0:ncol])
                return wbb

            def nextps():
                cnt["p"] += 1
                return PS[cnt["p"] % 4]

            for (g0, gn, kind) in [(0, 1792, "rw"), (C_MLQK, 512, "copy"), (C_MLO, 512, "sig"), (C_XBC, 2048, "copy"), (C_G, 3072, "sig")]:
                for c0 in range(g0, g0 + gn, 512):
                    ncol = min(512, g0 + gn - c0)
                    wb = load_w(c0, ncol)
                    for m in range(ncol // 128):
                        row0 = c0 + m * 128
                        rb = RB[cnt["m"] % 2]; ro = RO[cnt["m"] % 2]
                        cnt["m"] += 1
                        dst = rb if kind == "rw" else ro
                        for (t0, tw) in GROUPS:
                            ps = nextps()[:, 0:tw]
                            for k in range(8):
                                kb.mm(ps, wb[:, k, m * 128:(m + 1) * 128], hT[:, k, t0:t0 + tw], start=(k == 0), stop=(k == 7))
                            evac(dst[:, t0:t0 + tw], ps, AF.Sigmoid if kind == "sig" else None)
                        if kind == "rw":
                            ti = row0 // 128
                            for (a, e_) in SEGS:
                                kb.tt(RS[:, a + 1:e_ - 1], rb[:, a:e_ - 2], rb[:, a + 2:e_], ALU.add)
                                kb.copy(RS[:, a:a + 1], rb[:, a + 1:a + 2])
                                kb.copy(RS[:, e_ - 1:e_], rb[:, e_ - 2:e_ - 1])
                            kb.ts(ro.v(), rb.v(), CP[:, CP_OFF["om"] + ti:CP_OFF["om"] + ti + 1], None, op0=ALU.mult)
                            kb.stt(ro.v(), RS.v(), CP[:, CP_OFF["hm"] + ti:CP_OFF["hm"] + ti + 1], ro.v(), ALU.mult, ALU.add)
                            if ti == 12:
                                kb.act(ro[0:64, :], ro[0:64, :], AF.Tanh)
                            if ti == 13:
                                kb.act(ro.v(), ro.v(), AF.Sigmoid)
                        kb.dma(UF[row0:row0 + 128, :], ro.v(), q="sp")
            for (c0, ncol, dstD, func) in [(C_MLV, 512, VML, None), (C_Z, 512, ZT, AF.Silu), (C_Z + 512, 512, ZT, AF.Silu)]:
                wb = load_w(c0, ncol)
                dc0 = (c0 - C_Z) if dstD is ZT else 0
                for i in range(NT):
                    ps = nextps()[:, 0:512]
                    for k in range(8):
                        kb.mm(ps, hT[:, k, i * 128:(i + 1) * 128], wb[:, k, :], start=(k == 0), stop=(k == 7))
                    to = TO[i % 2]
                    evac(to[:, 0:512], ps, func)
                    kb.dma(dstD[i * 128:(i + 1) * 128, dc0:dc0 + 512], to[:, 0:512], q="sp")
            wst = W[cnt["w"] % 2]; wb = WB[cnt["w"] % 2]; cnt["w"] += 1
            kb.dma(wst[:, :, 0:16], w_in[l, :, C_MLG:C_MLG + 16].re("(k p) n -> p k n", p=128))
            kb.dma(wst[:, :, 16:48], w_in[l, :, C_DT:C_DT + 32].re("(k p) n -> p k n", p=128))
            castb(wb[:, :, 0:48], wst[:, :, 0:48])
            for i in range(NT):
                ps = nextps()[:, 0:48]
                for k in range(8):
                    kb.mm(ps, hT[:, k, i * 128:(i + 1) * 128], wb[:, k, 0:48], start=(k == 0), stop=(k == 7))
                to = TO[i % 2]
                evac(to[:, 0:48], ps)
                kb.dma(SMALL[i * 128:(i + 1) * 128, :], to[:, 0:48], q="sp")

        PHASES = {"mod": phase_mod, "in": phase_in}

        MASK4 = [kb.sb("mask4_%d" % d, [128, 2, 4, 128]) for d in range(2)]
        MST2 = [kb.sb("mst2_%d" % d, [128, 2, 128]) for d in range(2)]
        ID2 = kb.sb("id2", [128, 2, 128])
        TRI = {0: (CS_UI, CS_US, CS_LS), 1: (CS_LI, CS_LS, CS_US)}
        for d in range(2):
            inc, stc, stT = TRI[d]
            for h in range(2):
                for q in range(4):
                    src = stc if q < 2 else inc
                    kb.copy(MASK4[d][:, h, q, :], CST[:, src:src + 128])
                kb.copy(MST2[d][:, h, :], CST[:, stT:stT + 128])
        for h in range(2):
            kb.copy(ID2[:, h, :], ident)
        pcnt = {"p": 0}

        def nextps():
            pcnt["p"] += 1
            return PS[pcnt["p"] % 4]

        def bc3(v2, n):
            c = v2.ap.shape[1]
            return V(v2.buf, v2.ap.rearrange("p (c o) -> p c o", o=1).to_broadcast([v2.ap.shape[0], c, n]))

        def phase_rwkv(b, l):
            kb.release(0)
            RST = CST[:, CS_RST:CS_RST + T]
            TWXA = kb.A([128, T]); kb.dma(TWXA.v(), UF[1536:1664, :])
            W2A2 = kb.A([128, 2, 512])
            for d in range(2):
                kb.dma(W2A2[0:64, d, :], rw_w2[l, d]); kb.dma(W2A2[64:128, d, :], rw_a2[l, d])
            m1 = kb.aoff
            SG = kb.A([128, T]); kb.dma(SG.v(), UF[1664:1792, :])
            G2 = kb.A([128, 512]); kb.dma(G2.v(), rw_g2[l])
            ro = kb.A([128, T])
            for m in range(4):
                for (t0, tw) in GROUPS:
                    ps = nextps()[:, 0:tw]
                    kb.mm(ps, G2[:, m * 128:(m + 1) * 128], SG[:, t0:t0 + tw])
                    evac(ro[:, t0:t0 + tw], ps)
                kb.dma(GG[m * 128:(m + 1) * 128, :], ro.v())
            if RW_CUT == 1:
                return
            for pp in range(4 if RW_CUT == 0 else 1):
                kb.release(m1)
                Rr = kb.A([128, T]); Kk = kb.A([128, T]); Vv = kb.A([128, T]); KK = kb.A([128, T])
                TMP = kb.A([128, T]); TMP2 = kb.A([128, T]); VT = kb.A([128, 18, 128]); YACC = kb.A([128, T]); BON = kb.A([128, T])
                LWt = kb.A([128, T]); BB = kb.A([128, T]); KD = kb.A([128, T]); Rt = kb.A([128, T]); KKt = kb.A([128, T])
                BtW = kb.A([128, T]); KDtW = kb.A([128, T])
                CW = TMP; E = TMP2
                TOT = kb.A([128, 18]); ETOT = kb.A([128, 18]); S = kb.A([128, 64]); SP = kb.A([128, 2, 64])
                AM = kb.A([128, 2, 4, 128]); MTa = kb.A([128, 2, 128]); Pm = kb.A([128, 2, 128])
                MB = [(kb.A([128, 2, 128]), kb.A([128, 2, 128])) for _ in range(2)]
                Xn = kb.A([128, 128]); Ut = kb.A([128, 128]); BKt = kb.A([128, 256])
                kb.dma(Rr.v(), UF[pp * 128:(pp + 1) * 128, :])
                kb.dma(Kk.v(), UF[512 + pp * 128:512 + (pp + 1) * 128, :], q="pool")
                kb.dma(Vv.v(), UF[1024 + pp * 128:1024 + (pp + 1) * 128, :])

                def col(name, i=pp):
                    return CP[:, CP_OFF[name] + i:CP_OFF[name] + i + 1]

                kb.ts(KK.v(), Kk.v(), col("kk"), None, op0=ALU.mult)
                kb.act(TMP.v(), KK.v(), AF.Square)
                for (t0, tw) in GROUPS:
                    ps = nextps()[:, 0:tw]
                    kb.mm(ps, blk, TMP[:, t0:t0 + tw])
                    kb.ts(TMP2[:, t0:t0 + tw], ps, 1e-12, None, op0=ALU.add)
                kb.act(TMP2.v(), TMP2.v(), AF.Sqrt); kb.recip(TMP2.v(), TMP2.v())
                kb.tt(KK.v(), KK.v(), TMP2.v(), ALU.mult)
                for c in range(18):
                    ps = nextps()[:, 0:128]
                    kb.tr(ps, Vv[:, c * 128:(c + 1) * 128], ident)
                    evac(VT[:, c, :], ps)
                if RW_CUT == 2:
                    return
                for d in range(2):
                    for (t0, tw) in GROUPS:
                        ps = nextps()[:, 0:tw]
                        kb.mm(ps, W2A2[0:64, d, pp * 128:(pp + 1) * 128], TWXA[0:64, t0:t0 + tw])
                        kb.act(LWt[:, t0:t0 + tw], ps, AF.Sigmoid, bias=col("w0_%d" % d))
                        ps = nextps()[:, 0:tw]
                        kb.mm(ps, W2A2[64:128, d, pp * 128:(pp + 1) * 128], TWXA[64:128, t0:t0 + tw])
                        kb.act(BB[:, t0:t0 + tw], ps, AF.Sigmoid, bias=col("a0_%d" % d))
                    kb.ts(LWt.v(), LWt.v(), -float(np.exp(-0.5)), None, op0=ALU.mult)
                    kb.ts(KD.v(), BB.v(), -1.0, col("ka"), op0=ALU.add, op1=ALU.mult)
                    kb.stt(KD.v(), KD.v(), 1.0, Kk.v(), ALU.add, ALU.mult)
                    if d == 0:
                        kb.stt(BON.v(), KD.v(), col("rk"), Rr.v(), ALU.mult, ALU.mult)
                    else:
                        kb.stt(E.v(), KD.v(), col("rk"), Rr.v(), ALU.mult, ALU.mult)
                        kb.tt(BON.v(), BON.v(), E.v(), ALU.add)
                    kb.tt(BB.v(), BB.v(), KK.v(), ALU.mult)
                    kb.scan(CW.v(), RST, LWt.v(), 0.0, ALU.mult, ALU.add)
                    CW3 = CW.v().re("p (c t) -> p c t", t=128)
                    kb.copy(TOT.v(), CW3[:, :, 127])
                    if d == 1:
                        kb.tt(CW3, bc3(TOT.v(), 128), CW3, ALU.subtract)
                        kb.tt(CW.v(), CW.v(), LWt.v(), ALU.add)
                    kb.act(ETOT.v(), TOT.v(), AF.Exp)
                    kb.act(E.v(), CW.v(), AF.Exp)
                    kb.tt(Rt.v(), Rr.v(), E.v(), ALU.mult)
                    kb.tt(E.v(), CW.v(), LWt.v(), ALU.subtract)
                    kb.act(E.v(), E.v(), AF.Exp)
                    kb.tt(KKt.v(), KK.v(), E.v(), ALU.mult)
                    kb.act(E.v(), CW.v(), AF.Exp, scale=-1.0)
                    kb.tt(BB.v(), BB.v(), E.v(), ALU.mult)
                    kb.tt(KD.v(), KD.v(), E.v(), ALU.mult)
                    r3 = "p (c t) -> p c t"
                    kb.tt(BtW.v().re(r3, t=128), BB.v().re(r3, t=128), bc3(ETOT.v(), 128), ALU.mult)
                    kb.tt(KDtW.v().re(r3, t=128), KD.v().re(r3, t=128), bc3(ETOT.v(), 128), ALU.mult)
                    if RW_CUT == 3:
                        return
                    kb.memset(S.v(), 0.0)
                    kb.memset(SP.v(), 0.0)
                    for c in CHUNK_ORDER[d]:
                        cs = slice(c * 128, (c + 1) * 128)
                        pT = nextps()
                        kb.tr(pT[:, 0:128], BtW[:, cs], ident)
                        kb.tr(pT[:, 128:256], KDtW[:, cs], ident)
                        evac(BKt.v(), pT[:, 0:256])
                        pA = nextps(); pB = nextps()
                        for hh in range(2):
                            bs = slice(hh * 64, hh * 64 + 64)
                            o = hh * 512
                            kb.mm(pA[:, o:o + 128], BB[bs, cs], KKt[bs, cs])
                            kb.mm(pA[:, o + 128:o + 256], KD[bs, cs], KKt[bs, cs])
                            kb.mm(pA[:, o + 256:o + 384], BB[bs, cs], Rt[bs, cs])
                            kb.mm(pA[:, o + 384:o + 512], KD[bs, cs], Rt[bs, cs])
                            kb.mm(pB[:, o:o + 128], KKt[bs, cs], BB[bs, cs])
                        kb.tt(AM.v(), pA.v().re("p (h q t) -> p h q t", h=2, q=4), MASK4[d].v(), ALU.mult)
                        kb.tt(MTa.v(), pB.v().re("p (h x) -> p h x", h=2)[:, :, 0:128], MST2[d].v(), ALU.mult)
                        kb.tt(Pm.v(), ID2.v(), AM[:, :, 0, :], ALU.subtract)
                        if RW_CUT == 4:
                            return
                        Mk = AM[:, :, 0, :]; MkT = MTa.v()
                        for st in range(6):
                            if RW_CUT >= 10 and st >= RW_CUT - 10:
                                return
                            pC = nextps(); pC2 = nextps()
                            for hh in range(2):
                                kb.mm(pC[:, hh * 128:(hh + 1) * 128], Mk[:, hh, :], MkT[:, hh, :])
                            if st < 5:
                                for hh in range(2):
                                    kb.mm(pC2[:, hh * 128:(hh + 1) * 128], MkT[:, hh, :], Mk[:, hh, :])
                            nMkT, nMk = MB[st % 2]
                            kb.act(nMkT.v(), pC[:, 0:256].re("p (h t) -> p h t", h=2), AF.Copy)
                            if st < 5:
                                kb.copy(nMk.v(), pC2[:, 0:256].re("p (h t) -> p h t", h=2))
                            if RW_CUT == 9:
                                return
                            pD = nextps()
                            for hh in range(2):
                                kb.mm(pD[:, hh * 128:(hh + 1) * 128], nMkT[:, hh, :], Pm[:, hh, :])
                            kb.tt(Pm.v(), Pm.v(), pD[:, 0:256].re("p (h t) -> p h t", h=2), ALU.add)
                            Mk, MkT = nMk.v(), nMkT.v()
                        if RW_CUT == 5:
                            return
                        pX = nextps()
                        for hh in range(2):
                            hs_ = slice(hh * 64, (hh + 1) * 64)
                            kb.mm(pX[:, hs_], KKt[:, cs], SP[:, hh, :], start=True, stop=False)
                            kb.mm(pX[:, hs_], AM[:, hh, 1, :], VT[:, c, hs_], start=False, stop=True)
                        kb.ts(Xn.v(), pX[:, 0:128], -1.0, None, op0=ALU.mult)
                        pU = nextps()
                        for hh in range(2):
                            hs_ = slice(hh * 64, (hh + 1) * 64)
                            kb.mm(pU[:, hs_], Pm[:, hh, :], Xn[:, hs_])
                        evac(Ut.v(), pU[:, 0:128])
                        pY = nextps()
                        for hh in range(2):
                            bs = slice(hh * 64, hh * 64 + 64); hs_ = slice(hh * 64, (hh + 1) * 64)
                            kb.mm(pY[bs, 0:128], SP[:, hh, :], Rt[:, cs], start=True, stop=False)
                            kb.mm(pY[bs, 0:128], Ut[:, hs_], AM[:, hh, 2, :], start=False, stop=False)
                            kb.mm(pY[bs, 0:128], VT[:, c, hs_], AM[:, hh, 3, :], start=False, stop=True)
                        if d == 0:
                            evac(YACC[:, cs], pY[:, 0:128])
                        else:
                            kb.tt(YACC[:, cs], YACC[:, cs], pY[:, 0:128], ALU.add)
                        if RW_CUT == 6:
                            return
                        pS = nextps()
                        for hh in range(2):
                            bs = slice(hh * 64, hh * 64 + 64); hs_ = slice(hh * 64, (hh + 1) * 64)
                            kb.mm(pS[bs, 0:64], BKt[:, hs_], Ut[:, hs_], start=True, stop=False)
                            kb.mm(pS[bs, 0:64], BKt[:, 128 + hh * 64:128 + (hh + 1) * 64], VT[:, c, hs_], start=False, stop=True)
                        kb.stt(S.v(), S.v(), ETOT[:, c:c + 1], pS[:, 0:64], ALU.mult, ALU.add)
                        for hh in range(2):
                            kb.ts(SP[:, hh, :], S.v(), blk[:, hh * 64:hh * 64 + 1], None, op0=ALU.mult)
                for (t0, tw) in GROUPS:
                    g_ = slice(t0, t0 + tw)
                    ps = nextps()[:, 0:tw]
                    kb.mm(ps, blk, YACC[:, g_])
                    kb.stt(TMP[:, g_], ps, -1.0 / 64, YACC[:, g_], ALU.mult, ALU.add)
                kb.act(TMP2.v(), TMP.v(), AF.Square)
                for (t0, tw) in GROUPS:
                    g_ = slice(t0, t0 + tw)
                    ps = nextps()[:, 0:tw]
                    kb.mm(ps, blk, TMP2[:, g_])
                    kb.ts(LWt[:, g_], ps, 1.0 / 64, 64e-5, op0=ALU.mult, op1=ALU.add)
                kb.act(LWt.v(), LWt.v(), AF.Sqrt); kb.recip(LWt.v(), LWt.v())
                kb.tt(TMP.v(), TMP.v(), LWt.v(), ALU.mult)
                kb.ts(TMP.v(), TMP.v(), col("lnw"), col("lnb"), op0=ALU.mult, op1=ALU.add)
                for (t0, tw) in GROUPS:
                    g_ = slice(t0, t0 + tw)
                    ps = nextps()[:, 0:tw]
                    kb.mm(ps, blk, BON[:, g_])
                    kb.tt(TMP2[:, g_], ps, Vv[:, g_], ALU.mult)
                kb.tt(TMP.v(), TMP.v(), TMP2.v(), ALU.add)
                kb.dma(TMP2.v(), GG[pp * 128:(pp + 1) * 128, :])
                kb.tt(TMP.v(), TMP.v(), TMP2.v(), ALU.mult)
                kb.dma(YA[pp * 128:(pp + 1) * 128, :], TMP.v())

        PHASES["rwkv"] = phase_rwkv

        def bcmid(v2, n):
            t_ = v2.ap.shape[1]
            return V(v2.buf, v2.ap.rearrange("p (o t) -> p o t", o=1).to_broadcast([v2.ap.shape[0], n, t_]))

        def cview(off):
            return CST[:, off:off + 128]

        DIRC = {0: (CS_UI, CS_LS), 1: (CS_LI, CS_US)}

        def conv_silu(x, acc, wname, bname, ti, scale=None):
            def wc(k):
                o = CP_OFF["%s%d" % (wname, k)] + ti
                return CP[:, o:o + 1]
            bo = CP_OFF[bname] + ti
            kb.ts(acc.v(), x.v(), wc(2), CP[:, bo:bo + 1], op0=ALU.mult, op1=ALU.add)
            for k in (0, 1, 3, 4):
                off = k - 2
                for (a, e_) in SEGS:
                    lo = max(a, a - off); hi = min(e_, e_ - off)
                    kb.stt(acc[:, lo:hi], x[:, lo + off:hi + off], wc(k), acc[:, lo:hi], ALU.mult, ALU.add)
            kb.act(acc.v(), acc.v(), AF.Silu)
            if scale is not None:
                kb.ts(acc.v(), acc.v(), scale, None, op0=ALU.mult)

        def phase_mlstm(b, l):
            kb.release(0)
            HACC = kb.A([128, 18, 512])
            m0 = kb.aoff
            xin = kb.A([128, T])
            QM = [[kb.A([128, T]) for _ in range(2)] for _ in range(2)]
            KT = [kb.A([128, T]) for _ in range(2)]
            V1 = kb.A([128, 18, 4 * 129])
            GI = kb.A([128, 18, 8]); GF = kb.A([128, 18, 8]); GR = kb.A([128, 18, 16]); GB = kb.A([128, 16])
            CnS = [kb.A([128, 129]) for _ in range(4)]
            rhsF = kb.A([128, 4, 128]); Dm = kb.A([128, 4, 128]); Sc = kb.A([128, 4, 128]); TOTt = kb.A([128, 4, 129])
            KW = kb.A([128, 4, 128]); KTOK = kb.A([128, 256]); EX = kb.A([128, 12]); T12 = kb.A([128, 12])
            dn = kb.A([128, 4]); Hc = kb.A([128, 4, 128])
            acc = kb.A([128, T])
            for ti in range(4):
                kb.dma(xin.v(), UF[C_MLQK + ti * 128:C_MLQK + (ti + 1) * 128, :])
                if ti < 2:
                    conv_silu(xin, acc, "mlcw", "mlcb", ti)
                    for hh in range(2):
                        kb.ts(QM[ti][hh].v(), acc.v(), blk[:, hh * 64:hh * 64 + 1], None, op0=ALU.mult)
                else:
                    conv_silu(xin, KT[ti - 2], "mlcw", "mlcb", ti, scale=0.125)
            V14 = V1.v().re("p c (h x) -> p c h x", h=4)
            kb.memset(V1.v(), 1.0)
            for c in range(18):
                kb.dma(V14[:, c, :, 0:128], VML[c * 128:(c + 1) * 128, :].re("p (h v) -> p h v", h=4), q=("sp" if c % 2 else "pool"))
            kb.dma(GR.v(), SMALL[:, 0:16].re("(c p) g -> p c g", p=128), allow_slow_non_contiguous=True)
            kb.dma(GB.v(), V(rowp, rowp.t[l, RP_MLGB:RP_MLGB + 16].partition_broadcast(128)))
            kb.tt(GR.v(), GR.v(), bcmid(GB.v(), 18), ALU.add)
            GR4 = GR.v().re("p c (d g h) -> p c d g h", d=2, g=2)
            for d in range(2):
                kb.copy(GI[:, :, d * 4:(d + 1) * 4], GR4[:, :, d, 0, :])
                kb.act(GF[:, :, d * 4:(d + 1) * 4], GR4[:, :, d, 1, :], AF.Sigmoid)
            kb.act(GF.v(), GF.v(), AF.Ln)
            for d in range(2):
                tri, smat = DIRC[d]
                for h in range(4):
                    kb.memset(CnS[h].v(), 0.0)
                for c in CHUNK_ORDER[d]:
                    cs = slice(c * 128, (c + 1) * 128)
                    gi = GI[:, c, d * 4:(d + 1) * 4]; gf = GF[:, c, d * 4:(d + 1) * 4]
                    kb.tt(rhsF.v(), bcmid(cview(tri), 4), bc3(gf, 128), ALU.mult)
                    pSeg = nextps()
                    kb.mm(pSeg[:, 0:512], cview(smat), rhsF.v().re("p h t -> p (h t)"))
                    pB2 = nextps()
                    kb.mm(pB2[:, 0:4], cview(tri), gf)
                    kb.mm(pB2[:, 4:8], cview(smat), gf)
                    kb.mm(pB2[:, 8:12], ones, gf)
                    kb.copy(T12.v(), pB2[:, 0:12])
                    kb.tt(T12[:, 4:8], T12[:, 4:8], gi, ALU.add)
                    kb.act(EX.v(), T12.v(), AF.Exp)
                    kb.tt(Dm.v(), pSeg[:, 0:512].re("p (h t) -> p h t", h=4), bc3(gi, 128), ALU.add)
                    kb.act(Dm.v(), Dm.v(), AF.Exp)
                    kb.tt(Dm.v(), Dm.v(), bcmid(cview(tri), 4), ALU.mult)
                    pQK = nextps()
                    for h in range(4):
                        kb.mm(pQK[:, h * 128:(h + 1) * 128], KT[h // 2][:, cs], QM[h // 2][h % 2][:, cs])
                    kb.tt(Sc.v(), pQK[:, 0:512].re("p (h t) -> p h t", h=4), Dm.v(), ALU.mult)
                    pI = nextps(); pE = nextps()
                    for h in range(4):
                        o = (h // 2) * 512 + (h % 2) * 129
                        kb.mm(pI[:, o:o + 129], Sc[:, h, :], V14[:, c, h, :])
                        kb.mm(pE[:, o:o + 129], QM[h // 2][h % 2][:, cs], CnS[h].v())
                    for h in range(4):
                        o = (h // 2) * 512 + (h % 2) * 129
                        kb.ts(TOTt[:, h, :], pE[:, o:o + 129], EX[:, h:h + 1], None, op0=ALU.mult)
                    for half in range(2):
                        kb.tt(TOTt[:, 2 * half:2 * half + 2, :], TOTt[:, 2 * half:2 * half + 2, :],
                              pI[:, half * 512:half * 512 + 258].re("p (h x) -> p h x", h=2), ALU.add)
                    kb.act(dn.v(), TOTt[:, :, 128], AF.Abs)
                    kb.ts(dn.v(), dn.v(), 1.0, None, op0=ALU.max)
                    kb.recip(dn.v(), dn.v())
                    hv = HACC[:, c, :].re("p (h v) -> p h v", h=4)
                    if d == 0:
                        kb.tt(hv, TOTt[:, :, 0:128], bc3(dn.v(), 128), ALU.mult)
                    else:
                        kb.tt(Hc.v(), TOTt[:, :, 0:128], bc3(dn.v(), 128), ALU.mult)
                        kb.tt(hv, hv, Hc.v(), ALU.add)
                    pT = nextps()
                    kb.tr(pT[:, 0:128], KT[0][:, cs], ident)
                    kb.tr(pT[:, 128:256], KT[1][:, cs], ident)
                    evac(KTOK.v(), pT[:, 0:256])
                    pSt = nextps()
                    for h in range(4):
                        o = (h // 2) * 512 + (h % 2) * 129
                        kb.ts(KW[:, h, :], KTOK[:, (h // 2) * 128:(h // 2 + 1) * 128], EX[:, 4 + h:5 + h], None, op0=ALU.mult)
                        kb.mm(pSt[:, o:o + 129], KW[:, h, :], V14[:, c, h, :])
                    for h in range(4):
                        o = (h // 2) * 512 + (h % 2) * 129
                        kb.stt(CnS[h].v(), CnS[h].v(), EX[:, 8 + h:9 + h], pSt[:, o:o + 129], ALU.mult, ALU.add)
            kb.release(m0)
            YR = [kb.A([128, T]) for _ in range(4)]; SO = [kb.A([128, T]) for _ in range(4)]
            sq = kb.A([128, 4, 128]); ssq = kb.A([128, 4]); hn = kb.A([128, 4, 128])
            for h in range(4):
                kb.dma(SO[h].v(), UF[C_MLO + h * 128:C_MLO + (h + 1) * 128, :], q=("sp" if h % 2 else "pool"))
            for c in range(18):
                cs = slice(c * 128, (c + 1) * 128)
                hv = HACC[:, c, :].re("p (h v) -> p h v", h=4)
                kb.act(sq.v(), hv, AF.Square)
                kb.reduce(ssq.v(), sq.v(), ALU.add)
                kb.ts(ssq.v(), ssq.v(), 1.0 / 128, EPS, op0=ALU.mult, op1=ALU.add)
                kb.act(ssq.v(), ssq.v(), AF.Sqrt); kb.recip(ssq.v(), ssq.v())
                kb.tt(hn.v(), hv, bc3(ssq.v(), 128), ALU.mult)
                pT = nextps()
                for h in range(4):
                    kb.tr(pT[:, h * 128:(h + 1) * 128], hn[:, h, :], ident)
                for h in range(4):
                    o = CP_OFF["mlng"] + h
                    kb.stt(YR[h][:, cs], pT[:, h * 128:(h + 1) * 128], CP[:, o:o + 1], SO[h][:, cs], ALU.mult, ALU.mult)
            for h in range(4):
                kb.dma(YB[h * 128:(h + 1) * 128, :], YR[h].v())

        PHASES["mlstm"] = phase_mlstm

        XST = scratch("XST", [T, 1024]); YS0 = scratch("YS0", [T, 1024])

        def phase_ssd(b, l):
            kb.release(0)
            BT = [kb.A([128, T]) for _ in range(4)]; CT = [kb.A([128, T]) for _ in range(4)]
            m0 = kb.aoff
            xin = kb.A([128, T]); acc = kb.A([128, T]); XO = [kb.A([128, 4, 128]) for _ in range(2)]
            for ti in range(16):
                kb.dma(xin.v(), UF[C_XBC + ti * 128:C_XBC + (ti + 1) * 128, :], q=("sp" if ti % 2 else "pool"))
                if ti < 8:
                    conv_silu(xin, acc, "mbcw", "mbcb", ti)
                    for c0 in range(0, 18, 4):
                        n = min(4, 18 - c0)
                        pT = nextps()
                        for j in range(n):
                            kb.tr(pT[:, j * 128:(j + 1) * 128], acc[:, (c0 + j) * 128:(c0 + j + 1) * 128], ident)
                        xo = XO[(c0 // 4) % 2]
                        evac(xo[:, 0:n, :], pT[:, 0:n * 128].re("p (c f) -> p c f", c=n))
                        kb.dma(XST[c0 * 128:(c0 + n) * 128, ti * 128:(ti + 1) * 128].re("(c p) f -> p c f", p=128), xo[:, 0:n, :])
                elif ti < 12:
                    conv_silu(xin, BT[ti - 8], "mbcw", "mbcb", ti)
                else:
                    conv_silu(xin, CT[ti - 12], "mbcw", "mbcb", ti)
            kb.release(m0)
            DTt = kb.A([128, 18, 32]); DA = kb.A([128, 18, 32]); RB_ = kb.A([128, 32]); AN = kb.A([128, 32])
            DROW = kb.A([128, 1024])
            rhsF = kb.A([128, 16, 128]); Dm = kb.A([128, 16, 128]); Mt = kb.A([128, 16, 128]); CBs = kb.A([128, 4, 128])
            XC = kb.A([128, 16, 64]); XD = kb.A([128, 16, 64]); XW = kb.A([128, 16, 64]); Yc = kb.A([128, 16, 64])
            Y0 = kb.A([128, 16, 64]); HST = kb.A([128, 16, 64]); BK = kb.A([128, 4, 128]); tmp = kb.A([128, 1024])
            ZC = kb.A([128, 1024]); YO = kb.A([128, 8, 128]); EX = kb.A([128, 48]); ssq = kb.A([128, 4])
            kb.dma(DTt.v(), SMALL[:, 16:48].re("(c p) g -> p c g", p=128), allow_slow_non_contiguous=True)
            kb.dma(RB_.v(), V(rowp, rowp.t[l, RP_DTB:RP_DTB + 32].partition_broadcast(128)))
            kb.dma(AN.v(), V(rowp, rowp.t[l, RP_ALOG:RP_ALOG + 32].partition_broadcast(128)))
            kb.dma(DROW.v(), V(rowp, rowp.t[l, RP_MBD:RP_MBD + 1024].partition_broadcast(128)))
            kb.tt(DTt.v(), DTt.v(), bcmid(RB_.v(), 18), ALU.add)
            kb.act(DTt.v(), DTt.v(), AF.Exp)
            kb.act(DTt.v(), DTt.v(), AF.Ln, bias=1.0)
            kb.act(AN.v(), AN.v(), AF.Exp)
            kb.ts(AN.v(), AN.v(), -1.0, None, op0=ALU.mult)
            kb.tt(DA.v(), DTt.v(), bcmid(AN.v(), 18), ALU.mult)
            for d in range(2):
                tri, smat = DIRC[d]
                kb.memset(HST.v(), 0.0)
                for c in CHUNK_ORDER[d]:
                    cs = slice(c * 128, (c + 1) * 128)
                    da = DA[:, c, d * 16:(d + 1) * 16]; dt = DTt[:, c, d * 16:(d + 1) * 16]
                    kb.dma(XC.v().re("p h x -> p (h x)"), XST[c * 128:(c + 1) * 128, :])
                    kb.tt(rhsF.v(), bcmid(cview(tri), 16), bc3(da, 128), ALU.mult)
                    pS = [nextps(), nextps()]
                    for q in range(4):
                        kb.mm(pS[q // 2][:, (q % 2) * 512:(q % 2 + 1) * 512], cview(smat),
                              rhsF[:, q * 4:(q + 1) * 4, :].re("p h t -> p (h t)"))
                    pB2 = nextps()
                    kb.mm(pB2[:, 0:16], cview(tri), da)
                    kb.mm(pB2[:, 16:32], cview(smat), da)
                    kb.mm(pB2[:, 32:48], ones, da)
                    kb.act(EX.v(), pB2[:, 0:48], AF.Exp)
                    for half in range(2):
                        kb.act(Dm[:, half * 8:(half + 1) * 8, :], pS[half].v().re("p (h t) -> p h t", h=8), AF.Exp)
                    kb.tt(Dm.v(), Dm.v(), bcmid(cview(tri), 16), ALU.mult)
                    pCB = nextps()
                    for g in range(4):
                        kb.mm(pCB[:, g * 128:(g + 1) * 128], BT[g][:, cs], CT[g][:, cs])
                    evac(CBs.v(), pCB[:, 0:512].re("p (g t) -> p g t", g=4))
                    cb4 = V(CBs, CBs.t.rearrange("p g (o t) -> p g o t", o=1).to_broadcast([128, 4, 4, 128]))
                    kb.tt(Mt.v().re("p (g j) t -> p g j t", g=4), Dm.v().re("p (g j) t -> p g j t", g=4), cb4, ALU.mult)
                    kb.tt(XD.v(), XC.v(), bc3(dt, 64), ALU.mult)
                    pY = nextps(); pE = nextps()
                    for hd in range(16):
                        kb.mm(pY[:, hd * 64:(hd + 1) * 64], Mt[:, hd, :], XD[:, hd, :])
                    for hd in range(16):
                        kb.mm(pE[:, hd * 64:(hd + 1) * 64], CT[hd // 4][:, cs], HST[:, hd, :])
                    kb.tt(Yc.v(), pE.v().re("p (h x) -> p h x", h=16), bc3(EX[:, 0:16], 64), ALU.mult)
                    kb.tt(Yc.v(), Yc.v(), pY.v().re("p (h x) -> p h x", h=16), ALU.add)
                    pT = nextps()
                    for g in range(4):
                        kb.tr(pT[:, g * 128:(g + 1) * 128], BT[g][:, cs], ident)
                    evac(BK.v(), pT[:, 0:512].re("p (g t) -> p g t", g=4))
                    kb.tt(XW.v(), XD.v(), bc3(EX[:, 16:32], 64), ALU.mult)
                    pH = nextps()
                    for hd in range(16):
                        kb.mm(pH[:, hd * 64:(hd + 1) * 64], BK[:, hd // 4, :], XW[:, hd, :])
                    kb.tt(HST.v(), HST.v(), bc3(EX[:, 32:48], 64), ALU.mult)
                    kb.tt(HST.v(), HST.v(), pH.v().re("p (h x) -> p h x", h=16), ALU.add)
                    Ycf = Yc.v().re("p h x -> p (h x)")
                    if d == 0:
                        kb.dma(YS0[c * 128:(c + 1) * 128, :], Ycf, q="pool")
                        continue
                    kb.dma(Y0.v().re("p h x -> p (h x)"), YS0[c * 128:(c + 1) * 128, :], q="pool")
                    kb.dma(ZC.v(), ZT[c * 128:(c + 1) * 128, :], q="pool")
                    kb.tt(Yc.v(), Yc.v(), Y0.v(), ALU.add)
                    kb.tt(tmp.v(), XC.v().re("p h x -> p (h x)"), DROW.v(), ALU.mult)
                    kb.tt(Ycf, Ycf, tmp.v(), ALU.add)
                    kb.tt(Ycf, Ycf, ZC.v(), ALU.mult)
                    kb.act(tmp.v(), Ycf, AF.Square)
                    kb.reduce(ssq.v(), tmp.v().re("p (g x) -> p g x", g=4), ALU.add)
                    kb.ts(ssq.v(), ssq.v(), 1.0 / 256, EPS, op0=ALU.mult, op1=ALU.add)
                    kb.act(ssq.v(), ssq.v(), AF.Sqrt); kb.recip(ssq.v(), ssq.v())
                    y4 = Yc.v().re("p (g j) x -> p g (j x)", g=4)
                    kb.tt(y4, y4, bc3(ssq.v(), 256), ALU.mult)
                    pTs = [nextps(), nextps()]
                    for kt in range(8):
                        kb.tr(pTs[kt // 4][:, (kt % 4) * 128:(kt % 4 + 1) * 128], Ycf[:, kt * 128:(kt + 1) * 128], ident)
                    for kt in range(8):
                        o = CP_OFF["mbng"] + kt
                        kb.ts(YO[:, kt, :], pTs[kt // 4][:, (kt % 4) * 128:(kt % 4 + 1) * 128], CP[:, o:o + 1], None, op0=ALU.mult)
                    kb.dma(YC.v().re("(k p) t -> p k t", p=128)[:, :, cs], YO.v())

        PHASES["ssd"] = phase_ssd

        def phase_merge(b, l):
            kb.release(0)
            PA = kb.Ab([128, 4, 1024]); PB = kb.Ab([128, 4, 1024]); PC = kb.Ab([128, 8, 1024]); WO = kb.Ab([128, 8, 1024])
            ST = [kb.A([128, 4, 1024])] * 2
            for j, (dst, src) in enumerate([(PA[:, 0:4, :], p_a[l]), (PB[:, 0:4, :], p_b[l]), (PC[:, 0:4, :], p_c[l, 0:512, :]),
                                            (PC[:, 4:8, :], p_c[l, 512:1024, :]), (WO[:, 0:4, :], w_out[l, 0:512, :]), (WO[:, 4:8, :], w_out[l, 512:1024, :])]):
                st = ST[j % 2]
                kb.dma(st.v(), src.re("(k p) n -> p k n", p=128), q=("sp" if j % 2 else "pool"))
                castb(dst, st.v())
            G1 = [kb.A([128, 1024]) for _ in range(2)]
            for which in range(2):
                kb.dma(G1[which].v(), V(MOD, MOD.t[which, 2048:3072].partition_broadcast(128)))
            YAg = kb.A([128, 4, 512]); YBg = kb.A([128, 4, 512]); YCg = kb.A([128, 8, 512])
            YAb = kb.Ab([128, 4, 512]); YBb = kb.Ab([128, 4, 512]); YCb = kb.Ab([128, 8, 512]); mTb = kb.Ab([128, 8, 512])
            GT = kb.A([128, 3, 512]); mT = kb.A([128, 8, 512]); tmp = kb.A([128, 512])
            xt = kb.A([128, 1024]); ol = kb.A([128, 1024])
            for (t0, tw) in GROUPS:
                kb.dma(YAg[:, :, 0:tw], YA.v().re("(k p) t -> p k t", p=128)[:, :, t0:t0 + tw])
                kb.dma(YBg[:, :, 0:tw], YB.v().re("(k p) t -> p k t", p=128)[:, :, t0:t0 + tw], q="pool")
                kb.dma(YCg[:, :, 0:tw], YC.v().re("(k p) t -> p k t", p=128)[:, :, t0:t0 + tw])
                for Yg_, Yb_ in ((YAg, YAb), (YBg, YBb), (YCg, YCb)):
                    castb(Yb_[:, :, 0:tw], Yg_[:, :, 0:tw])
                for dt_ in range(8):
                    ds_ = slice(dt_ * 128, (dt_ + 1) * 128)
                    for br in range(3):
                        r0 = C_G + br * 1024 + dt_ * 128
                        kb.dma(GT[:, br, 0:tw], UF[r0:r0 + 128, t0:t0 + tw], q=("pool" if br % 2 else "sp"))
                    for br, (Wt, Yg, nk) in enumerate([(PA, YAb, 4), (PB, YBb, 4), (PC, YCb, 8)]):
                        ps = nextps()[:, 0:tw]
                        for k in range(nk):
                            kb.mm(ps, Wt[:, k, ds_], Yg[:, k, 0:tw], start=(k == 0), stop=(k == nk - 1))
                        if br == 0:
                            kb.tt(mT[:, dt_, 0:tw], ps, GT[:, br, 0:tw], ALU.mult)
                        else:
                            kb.tt(tmp[:, 0:tw], ps, GT[:, br, 0:tw], ALU.mult)
                            kb.tt((mTb if br == 2 else mT)[:, dt_, 0:tw], mT[:, dt_, 0:tw], tmp[:, 0:tw], ALU.add)
                for tile in range(tw // 128):
                    i = t0 // 128 + tile
                    load_x(b, l, i, xt)
                    for half in range(2):
                        ps = nextps()[:, 0:512]
                        for k in range(8):
                            kb.mm(ps, mTb[:, k, tile * 128:(tile + 1) * 128], WO[:, k, half * 512:(half + 1) * 512],
                                  start=(k == 0), stop=(k == 7))
                        kb.tt(ol[:, half * 512:(half + 1) * 512], ps, G1[1 if i < 2 else 0][:, half * 512:(half + 1) * 512], ALU.mult)
                    kb.tt(xt.v(), xt.v(), ol.v(), ALU.add)
                    store_x(b, l, i, xt)

        PHASES["merge"] = phase_merge

        def phase_moe(b, l):
            kb.release(0)
            G2 = [kb.A([128, 1024]) for _ in range(2)]
            for which in range(2):
                kb.dma(G2[which].v(), V(MOD, MOD.t[which, 5120:6144].partition_broadcast(128)))
            mA = kb.aoff
            gs = {}
            for which in (0, 1):
                gs[which] = mod_tiles(l, which, 4096, 3072, RP_N2)
            xts = [kb.A([128, D]) for _ in range(2)]; hs = [kb.A([128, D]) for _ in range(2)]
            junk = kb.A([128, D]); ssq = kb.A([128, 1]); rstd = kb.A([128, 1])
            hTt = kb.A([128, 8, 128]); RW = kb.A([128, 8, 16]); AFFT = kb.A([16, T])
            lg = kb.A([128, 16]); mx = kb.A([128, 1]); sm = kb.A([128, 1])
            kb.dma(RW.v(), router_w[l].re("(k p) e -> p k e", p=128), allow_slow_non_contiguous=True)
            for i in range(NT):
                xt = xts[i % 2]; h = hs[i % 2]
                kb.dma(xt.v(), XS[b, i * 128:(i + 1) * 128, :])
                gsc, sh = gs[1 if i < 2 else 0]
                rmsnorm(xt.v(), h.v(), gsc.v(), sh.v(), junk.v(), ssq.v(), rstd.v())
                kb.dma(H2[i * 128:(i + 1) * 128, :], h.v(), q="pool")
                transpose_tile(h, lambda k0: hTt[:, k0:k0 + 4, :])
                ps = nextps()
                for k in range(8):
                    kb.mm(ps[:, 0:16], hTt[:, k, :], RW[:, k, :], start=(k == 0), stop=(k == 7))
                kb.reduce(mx.v(), ps[:, 0:16], ALU.max)
                kb.ts(mx.v(), mx.v(), -1.0, None, op0=ALU.mult)
                kb.act(lg.v(), ps[:, 0:16], AF.Exp, bias=mx.v(), accum=sm.v())
                kb.recip(sm.v(), sm.v())
                kb.ts(lg.v(), lg.v(), sm.v(), None, op0=ALU.mult)
                ps2 = nextps()
                kb.tr(ps2[0:16, 0:128], lg.v(), ident)
                evac(AFFT[:, i * 128:(i + 1) * 128], ps2[0:16, 0:128])
            kb.dma(AFS.v(), AFFT.v())
            for (tile0, ntile, cap) in [(0, 2, 32), (2, 16, 256)]:
                kb.release(mA)
                n = ntile * 128
                cw = min(cap, 128); nct = cap // cw
                SELG_T = kb.A([128, ntile, 16]); RANK_T = kb.A([128, ntile, 16]); MASK_T = kb.A([128, ntile, 16])
                mB = kb.aoff
                work = kb.A([16, n]); selg = kb.A([16, n]); mask = kb.A([16, n]); rank = kb.A([16, n]); zer = kb.A([16, n])
                mx8 = kb.A([16, 8])
                kb.dma(work.v(), AFS[0:16, tile0 * 128:tile0 * 128 + n])
                kb.dma(selg.v(), AFS[0:16, tile0 * 128:tile0 * 128 + n], q="pool")
                for it in range(cap // 8):
                    kb.op("dve", lambda: nc.vector.max(out=mx8.t[:], in_=work.t[:]), [work.v()], [mx8.v()])
                    kb.op("dve", lambda: nc.vector.match_replace(out=work.t[:], in_to_replace=mx8.t[:], in_values=work.t[:], imm_value=0.0),
                          [work.v(), mx8.v()], [work.v()])
                kb.tt(selg.v(), selg.v(), work.v(), ALU.subtract)
                kb.ts(mask.v(), selg.v(), 0.0, None, op0=ALU.is_gt)
                kb.memset(zer.v(), 0.0)
                kb.scan(rank.v(), mask.v(), zer.v(), 0.0, ALU.add, ALU.add)
                for i in range(ntile):
                    ps = nextps()
                    kb.tr(ps[:, 0:16], selg[:, i * 128:(i + 1) * 128], ident[0:16, 0:16])
                    kb.tr(ps[:, 16:32], rank[:, i * 128:(i + 1) * 128], ident[0:16, 0:16])
                    kb.copy(SELG_T[:, i, :], ps[:, 0:16])
                    kb.copy(RANK_T[:, i, :], ps[:, 16:32])
                kb.ts(MASK_T.v(), SELG_T.v(), 0.0, None, op0=ALU.is_gt)
                kb.release(mB)
                OUT = kb.A([128, ntile, 1024]); SelE = kb.Ab([128, ntile, cap]); SelGT = kb.Ab([128, nct, n])
                H2T = [kb.A([128, 1024]) for _ in range(2)]; W = [kb.A([128, 8, 512]) for _ in range(2)]
                H2B = [kb.Ab([128, 1024]) for _ in range(2)]; WBb = [kb.Ab([128, 8, 512]) for _ in range(2)]
                xeT = kb.Ab([128, 8, cap]); actT = kb.Ab([128, 8, cap]); sa = kb.A([128, cap]); ye = kb.Ab([128, nct, 1024])
                SG_ = [kb.A([128, cap]) for _ in range(2)]; xt = H2T[0]
                iota = CST[:, CS_IOTA:CS_IOTA + cap]

                wcnt = {"w": 0}

                def load_w(src):
                    wb = W[wcnt["w"] % 2]; wbb = WBb[wcnt["w"] % 2]
                    q = "sp" if wcnt["w"] % 2 else "pool"
                    wcnt["w"] += 1
                    kb.dma(wb.v(), src.re("(k p) n -> p k n", p=128), q=q)
                    castb(wbb.v(), wb.v())
                    return wbb

                for e in range(16):
                    for i in range(ntile):
                        kb.ts(SelE[:, i, :], iota, RANK_T[:, i, e:e + 1], MASK_T[:, i, e:e + 1], op0=ALU.is_equal, op1=ALU.mult)
                        sg = SG_[i % 2]
                        kb.ts(sg.v(), iota, RANK_T[:, i, e:e + 1], SELG_T[:, i, e:e + 1], op0=ALU.is_equal, op1=ALU.mult)
                        ps = nextps()
                        for ct in range(nct):
                            kb.tr(ps[0:cw, ct * 128:(ct + 1) * 128], sg[:, ct * cw:(ct + 1) * cw], ident)
                        evac(SelGT[0:cw, :, i * 128:(i + 1) * 128], ps[0:cw, 0:nct * 128].re("p (c t) -> p c t", c=nct))
                    for i in range(ntile):
                        h2t = H2T[i % 2]
                        kb.dma(h2t.v(), H2[(tile0 + i) * 128:(tile0 + i + 1) * 128, :], q=("sp" if i % 2 else "pool"))
                        h2b = H2B[i % 2]
                        castb(h2b.v(), h2t.v())
                        for k in range(8):
                            pg = PS[k // 2][:, (k % 2) * 512:(k % 2) * 512 + cap]
                            kb.mm(pg, h2b[:, k * 128:(k + 1) * 128], SelE[:, i, :], start=(i == 0), stop=(i == ntile - 1))
                    for k in range(8):
                        evac(xeT[:, k, :], PS[k // 2][:, (k % 2) * 512:(k % 2) * 512 + cap])
                    for half in range(2):
                        wg = load_w(e_wg[l, e, :, half * 512:(half + 1) * 512])
                        wu = load_w(e_wu[l, e, :, half * 512:(half + 1) * 512])
                        for ft in range(4):
                            p1 = nextps()[:, 0:cap]; p2 = nextps()[:, 0:cap]
                            for k in range(8):
                                kb.mm(p1, wg[:, k, ft * 128:(ft + 1) * 128], xeT[:, k, :], start=(k == 0), stop=(k == 7))
                            for k in range(8):
                                kb.mm(p2, wu[:, k, ft * 128:(ft + 1) * 128], xeT[:, k, :], start=(k == 0), stop=(k == 7))
                            kb.act(sa.v(), p1, AF.Silu)
                            kb.tt(actT[:, half * 4 + ft, :], sa.v(), p2, ALU.mult)
                    for ch in range(2):
                        wd = load_w(e_wd[l, e, :, ch * 512:(ch + 1) * 512])
                        for ct in range(nct):
                            ps = nextps()
                            for k in range(8):
                                kb.mm(ps[0:cw, 0:512], actT[:, k, ct * cw:(ct + 1) * cw], wd[:, k, :], start=(k == 0), stop=(k == 7))
                            evac(ye[0:cw, ct, ch * 512:(ch + 1) * 512], ps[0:cw, 0:512])
                    for i in range(ntile):
                        for ch in range(2):
                            ps = nextps()
                            for ct in range(nct):
                                kb.mm(ps[:, 0:512], SelGT[0:cw, ct, i * 128:(i + 1) * 128], ye[0:cw, ct, ch * 512:(ch + 1) * 512],
                                      start=(ct == 0), stop=(ct == nct - 1))
                            o_ = OUT[:, i, ch * 512:(ch + 1) * 512]
                            if e == 0:
                                evac(o_, ps[:, 0:512])
                            else:
                                kb.tt(o_, o_, ps[:, 0:512], ALU.add)
                which = 1 if tile0 == 0 else 0
                for i in range(ntile):
                    rows = XS[b, (tile0 + i) * 128:(tile0 + i + 1) * 128, :]
                    kb.dma(xt.v(), rows)
                    kb.tt(OUT[:, i, :], OUT[:, i, :], G2[which].v(), ALU.mult)
                    kb.tt(xt.v(), xt.v(), OUT[:, i, :], ALU.add)
                    kb.dma(rows, xt.v(), q="pool")

        PHASES["moe"] = phase_moe

        AFS = scratch("AFS", [16, T])

        def phase_final():
            kb.release(0)
            gsc = kb.A([128, D]); xts = [kb.A([128, D]) for _ in range(2)]; hs = [kb.A([128, D]) for _ in range(2)]
            junk = kb.A([128, D]); ssq = kb.A([128, 1]); rstd = kb.A([128, 1])
            kb.dma(gsc.v(), V(final_g, final_g.t[0, :].partition_broadcast(128)))
            for b in range(NB):
                for j in range(16):
                    xt = xts[j % 2]; h = hs[j % 2]
                    kb.dma(xt.v(), XS[b, 256 + j * 128:256 + (j + 1) * 128, :])
                    rmsnorm(xt.v(), h.v(), gsc.v(), None, junk.v(), ssq.v(), rstd.v())
                    kb.dma(out[b, j * 128:(j + 1) * 128, :], h.v(), q="pool")

        for b in range(NB):
            kb.dma(XS[b, 0:256, :], ctx_in[b], q="sp")
            kb.dma(XS[b, 256:T, :], x_in[b], q="pool")
        seq = stop if stop is not None else ["mod", "in", "rwkv", "mlstm", "ssd", "merge", "moe"]
        for b in range(NB):
            for l in range(NL):
                for ph in seq:
                    PHASES[ph](b, l)
        if stop is None:
            phase_final()
        kb.release(0)
        kb.finish([out])
        print("instructions:", kb.nins, flush=True)
    return nc


WNAMES = ["ada_w", "ada_b", "w_in", "rw_w2", "rw_a2", "rw_g2", "p_a", "p_b", "p_c", "w_out", "router_w", "e_wg", "e_wu", "e_wd"]


def make_in_maps(inputs, ncores=8, nb=2, LW=4, EW=16):
    P = {k: np.asarray(v) for k, v in inputs.items()}
    shared = {k: np.ascontiguousarray(P[k][:LW, :EW] if k.startswith("e_w") else P[k][:LW], dtype=np.float32) for k in WNAMES}
    shared["final_g"] = np.ascontiguousarray(P["final_g"].reshape(1, D), dtype=np.float32)
    shared["colp"] = np.stack([host_colpack(P, l) for l in range(LW)])
    shared["rowp"] = np.stack([host_rowpack(P, l) for l in range(LW)])
    shared["cst"] = host_consts()
    shared["cctx"] = np.ascontiguousarray(P["c_ctx"].reshape(8, 128).T, dtype=np.float32)
    maps = []
    for c in range(ncores):
        m = dict(shared)
        sl = slice(c * nb, (c + 1) * nb)
        m["x"] = np.ascontiguousarray(P["x"][sl], dtype=np.float32)
        m["ctx"] = np.ascontiguousarray(P["ctx"][sl], dtype=np.float32)
        m["ccol"] = np.ascontiguousarray(P["c"][sl].reshape(nb, 8, 128).transpose(0, 2, 1), dtype=np.float32)
        maps.append(m)
    return maps


def kernel(**inputs):
    nc = build(NB=2, NL=4)
    maps = make_in_maps(inputs, 8, 2)
    res = run_bass_kernel_spmd(nc, maps, core_ids=list(range(8)))
    return np.concatenate([r["out"] for r in res.results], axis=0).astype(np.float32)
```
